# Optimizing a Trainium2 kernel written in Bass

```python
import math
import jax, jax.numpy as jnp
from jax import lax
import numpy as np

D_MODEL = 1024
BATCH = 8
SEQ = 2048
DEPTH = 4

D_MIX = D_MODEL
EPS = 1e-6
GLA_HEADS = 4
GLA_DV_TOTAL = D_MIX // 2
GLA_DK_TOTAL = GLA_DV_TOTAL // 2
GLA_DK = GLA_DK_TOTAL // GLA_HEADS
GLA_DV = GLA_DV_TOTAL // GLA_HEADS
GLA_LOW_RANK = 16
GLA_GATE_NORM = 16.0
GLA_CHUNK = 64
S5_WIDTH = D_MIX // 4
S5_GROUP = 16
S5_GROUPS = S5_WIDTH // S5_GROUP
S5_STATE = 64
S5_DT_MIN = 1e-3
S5_DT_MAX = 1e-1
RG_WIDTH = D_MIX // 4
RG_HEADS = 4
RG_HEAD_DIM = RG_WIDTH // RG_HEADS
RG_CONV = 4
RG_C = 8.0
D_FF = 2816
FFN_CONV = 3
IN_SIZES = (GLA_DK_TOTAL, GLA_DK_TOTAL, GLA_DV_TOTAL, GLA_DV_TOTAL, GLA_LOW_RANK, S5_WIDTH, RG_WIDTH, RG_WIDTH)
D_IN = 2 * GLA_DK_TOTAL + 2 * GLA_DV_TOTAL + GLA_LOW_RANK + S5_WIDTH + 2 * RG_WIDTH

kernel_name = "hymba_style_gla_s5_rglru_hybrid"


def rmsnorm(x, g):
    xf = x.astype(jnp.float32)
    y = xf * lax.rsqrt(jnp.mean(xf * xf, axis=-1, keepdims=True) + EPS)
    return y.astype(x.dtype) * g


def causal_dwconv(x, w, b):
    k, c = w.shape
    y = lax.conv_general_dilated(x, w[:, None, :].astype(x.dtype), window_strides=(1,),
                                 padding=[(k - 1, 0)], dimension_numbers=('NWC', 'WIO', 'NWC'),
                                 feature_group_count=c)
    return y + b


def linear_rec_combine(e1, e2):
    a1, x1 = e1
    a2, x2 = e2
    return a2 * a1, a2 * x1 + x2


def gla_mixer(q, k, v, g, gk_low, w_gk_up, b_gk, norm_g):
    B, L, _ = q.shape
    H, DK, DV, C = GLA_HEADS, GLA_DK, GLA_DV, GLA_CHUNK
    n = L // C
    f32 = jnp.float32
    gk = jax.nn.log_sigmoid((gk_low @ w_gk_up + b_gk).astype(f32)) / GLA_GATE_NORM

    def chunks(t, d):
        return t.reshape(B, n, C, H, d).transpose(0, 3, 1, 2, 4)

    qc = chunks(q.astype(f32) * (DK ** -0.5), DK)
    kc = chunks(k.astype(f32), DK)
    vc = chunks(v.astype(f32), DV)
    bc = jnp.cumsum(chunks(gk, DK), axis=3)
    b_mid = bc[:, :, :, C // 2 - 1:C // 2]
    scores = jnp.einsum('bhncd,bhnsd->bhncs', qc * jnp.exp(bc - b_mid), kc * jnp.exp(b_mid - bc))
    causal = jnp.tril(jnp.ones((C, C), dtype=bool))
    scores = jnp.where(causal, scores, 0.0)
    o_intra = jnp.einsum('bhncs,bhnsv->bhncv', scores, vc)
    b_last = bc[:, :, :, -1]
    kv = jnp.einsum('bhnsd,bhnsv->bhndv', kc * jnp.exp(b_last[:, :, :, None] - bc), vc)
    decay = jnp.exp(b_last)

    def step(S, inp):
        dec, kv_n = inp
        return dec[..., None] * S + kv_n, S

    S0 = jnp.zeros((B, H, DK, DV), f32)
    _, S_prev = lax.scan(step, S0, (jnp.moveaxis(decay, 2, 0), jnp.moveaxis(kv, 2, 0)))
    S_prev = jnp.moveaxis(S_prev, 0, 2)
    o_inter = jnp.einsum('bhncd,bhndv->bhncv', qc * jnp.exp(bc), S_prev)
    o = (o_intra + o_inter).transpose(0, 2, 3, 1, 4).reshape(B, L, H, DV)
    o = rmsnorm(o, norm_g).reshape(B, L, H * DV).astype(q.dtype)
    return o * jax.nn.silu(g)


def s5_mixer(u, lam_re, lam_im, log_dt, b_re, b_im, c_re, c_im, d, w_glu, b_glu):
    B, L, _ = u.shape
    f32 = jnp.float32
    uf = u.astype(f32)
    ug = uf.reshape(B, L, S5_GROUPS, S5_GROUP).astype(jnp.complex64)
    lam = lax.complex(lam_re.astype(f32), lam_im.astype(f32))
    dt = jnp.exp(log_dt.astype(f32))[:, None]
    lam_bar = jnp.exp(lam * dt)
    b = lax.complex(b_re.astype(f32), b_im.astype(f32))
    b_bar = ((lam_bar - 1.0) / lam)[..., None] * b
    bu = jnp.einsum('gph,blgh->blgp', b_bar, ug)
    a = jnp.broadcast_to(lam_bar, bu.shape)
    _, states = lax.associative_scan(linear_rec_combine, (a, bu), axis=1)
    c = lax.complex(c_re.astype(f32), c_im.astype(f32))
    y = jnp.einsum('ghp,blgp->blgh', c, states).real.reshape(B, L, S5_WIDTH) + d * uf
    y = jax.nn.gelu(y)
    y = y * jax.nn.sigmoid(y @ w_glu + b_glu)
    return y.astype(u.dtype)


def rglru_mixer(xr, gate, conv_w, conv_b, w_a, b_a, w_x, b_x, lam):
    B, L, _ = xr.shape
    f32 = jnp.float32
    xc = causal_dwconv(xr, conv_w, conv_b)
    xh = xc.reshape(B, L, RG_HEADS, RG_HEAD_DIM)
    r = jax.nn.sigmoid(jnp.einsum('blhi,hij->blhj', xh, w_a).reshape(B, L, RG_WIDTH) + b_a)
    i = jax.nn.sigmoid(jnp.einsum('blhi,hij->blhj', xh, w_x).reshape(B, L, RG_WIDTH) + b_x)
    log_a = -RG_C * r.astype(f32) * jax.nn.softplus(-lam.astype(f32))
    a = jnp.exp(log_a)
    xin = jnp.sqrt(-jnp.expm1(2.0 * log_a)) * (i * xc).astype(f32)
    _, h = lax.associative_scan(linear_rec_combine, (a, xin), axis=1)
    return h.astype(xr.dtype) * jax.nn.gelu(gate)


def conv_ffn(h, w_up, conv_w, conv_b, w_down):
    up, gv = jnp.split(h @ w_up, 2, axis=-1)
    up = causal_dwconv(up, conv_w, conv_b)
    return (jax.nn.gelu(up) * gv) @ w_down


def setup_inputs(seed: int = 0) -> dict:
    key = jax.random.key(seed)
    ks = jax.random.split(key, 32)
    f32 = jnp.float32
    L = DEPTH
    G, P = S5_GROUPS, S5_STATE

    def nrm(k, shape, scale):
        return scale * jax.random.normal(k, shape, f32)

    def gain(k, shape):
        return 1.0 + 0.02 * jax.random.normal(k, shape, f32)

    a0 = jax.random.uniform(ks[23], (L, RG_WIDTH), f32, 0.9, 0.999)
    return {
        "x": jax.random.normal(ks[0], (BATCH, SEQ, D_MODEL), f32),
        "attn_norm": gain(ks[1], (L, D_MODEL)),
        "w_in": nrm(ks[2], (L, D_MODEL, D_IN), D_MODEL ** -0.5),
        "gla_w_gk_up": nrm(ks[3], (L, GLA_LOW_RANK, GLA_DK_TOTAL), GLA_LOW_RANK ** -0.5),
        "gla_b_gk": nrm(ks[4], (L, GLA_DK_TOTAL), 0.1),
        "gla_norm": gain(ks[5], (L, GLA_DV)),
        "s5_lambda_re": -0.5 + nrm(ks[6], (L, G, P), 0.01),
        "s5_lambda_im": jnp.pi * jnp.arange(P, dtype=f32) + nrm(ks[7], (L, G, P), 0.01),
        "s5_log_dt": jax.random.uniform(ks[8], (L, G), f32, math.log(S5_DT_MIN), math.log(S5_DT_MAX)),
        "s5_b_re": nrm(ks[9], (L, G, P, S5_GROUP), (2 * S5_GROUP) ** -0.5),
        "s5_b_im": nrm(ks[10], (L, G, P, S5_GROUP), (2 * S5_GROUP) ** -0.5),
        "s5_c_re": nrm(ks[11], (L, G, S5_GROUP, P), (2 * P) ** -0.5),
        "s5_c_im": nrm(ks[12], (L, G, S5_GROUP, P), (2 * P) ** -0.5),
        "s5_d": nrm(ks[13], (L, S5_WIDTH), 1.0),
        "s5_w_glu": nrm(ks[14], (L, S5_WIDTH, S5_WIDTH), S5_WIDTH ** -0.5),
        "s5_b_glu": nrm(ks[15], (L, S5_WIDTH), 0.01),
        "s5_norm": gain(ks[16], (L, S5_WIDTH)),
        "rg_conv_w": nrm(ks[17], (L, RG_CONV, RG_WIDTH), RG_CONV ** -0.5),
        "rg_conv_b": nrm(ks[18], (L, RG_WIDTH), 0.01),
        "rg_w_a": nrm(ks[19], (L, RG_HEADS, RG_HEAD_DIM, RG_HEAD_DIM), RG_HEAD_DIM ** -0.5),
        "rg_b_a": nrm(ks[20], (L, RG_WIDTH), 0.01),
        "rg_w_x": nrm(ks[21], (L, RG_HEADS, RG_HEAD_DIM, RG_HEAD_DIM), RG_HEAD_DIM ** -0.5),
        "rg_b_x": nrm(ks[22], (L, RG_WIDTH), 0.01),
        "rg_lambda": jnp.log(a0) - jnp.log1p(-a0),
        "rg_norm": gain(ks[24], (L, RG_WIDTH)),
        "w_out": nrm(ks[25], (L, D_MIX, D_MODEL), D_MIX ** -0.5),
        "mlp_norm": gain(ks[26], (L, D_MODEL)),
        "w_up": nrm(ks[27], (L, D_MODEL, 2 * D_FF), D_MODEL ** -0.5),
        "mlp_conv_w": nrm(ks[28], (L, FFN_CONV, D_FF), FFN_CONV ** -0.5),
        "mlp_conv_b": nrm(ks[29], (L, D_FF), 0.01),
        "w_down": nrm(ks[30], (L, D_FF, D_MODEL), D_FF ** -0.5),
        "final_norm": gain(ks[31], (D_MODEL,)),
    }


def reference(x, attn_norm, w_in, gla_w_gk_up, gla_b_gk, gla_norm,
              s5_lambda_re, s5_lambda_im, s5_log_dt, s5_b_re, s5_b_im, s5_c_re, s5_c_im,
              s5_d, s5_w_glu, s5_b_glu, s5_norm,
              rg_conv_w, rg_conv_b, rg_w_a, rg_b_a, rg_w_x, rg_b_x, rg_lambda, rg_norm,
              w_out, mlp_norm, w_up, mlp_conv_w, mlp_conv_b, w_down, final_norm):
    splits = np.cumsum(np.array(IN_SIZES))[:-1].tolist()
    for l in range(DEPTH):
        h = rmsnorm(x, attn_norm[l])
        q, k, v, g, gk_low, s5_u, rg_x, rg_gate = jnp.split(h @ w_in[l], splits, axis=-1)
        y_gla = gla_mixer(q, k, v, g, gk_low, gla_w_gk_up[l], gla_b_gk[l], gla_norm[l])
        y_s5 = rmsnorm(s5_mixer(s5_u, s5_lambda_re[l], s5_lambda_im[l], s5_log_dt[l], s5_b_re[l], s5_b_im[l],
                                s5_c_re[l], s5_c_im[l], s5_d[l], s5_w_glu[l], s5_b_glu[l]), s5_norm[l])
        y_rg = rmsnorm(rglru_mixer(rg_x, rg_gate, rg_conv_w[l], rg_conv_b[l], rg_w_a[l], rg_b_a[l],
                                   rg_w_x[l], rg_b_x[l], rg_lambda[l]), rg_norm[l])
        x = x + jnp.concatenate([y_gla, y_s5, y_rg], axis=-1) @ w_out[l]
        h = rmsnorm(x, mlp_norm[l])
        x = x + conv_ffn(h, w_up[l], mlp_conv_w[l], mlp_conv_b[l], w_down[l])
    return rmsnorm(x, final_norm)
```

```python
import math
import numpy as np
import concourse.bass as bass
import concourse.mybir as mybir
from concourse.bass_utils import run_bass_kernel_spmd

F32 = mybir.dt.float32
BF16 = mybir.dt.bfloat16
I32 = mybir.dt.int32
ALU = mybir.AluOpType
AF = mybir.ActivationFunctionType

ENG = ['pe', 'act', 'dve', 'pool', 'sp']
DEPTH = 4
NT = 2048
HALF = 1024
DM = 1024
DFF = 2816
NJ = 22
EPS = 1e-6
PI = math.pi

CC_ID, CC_MASK, CC_SEG, CC_CIDX, CC_IDX9, CC_MASKT, NCC = 0, 128, 256, 1280, 1536, 1545, 1548
PP_AN, PP_MN, PP_FN, PP_BGK, PP_GN = 0, 8, 16, 24, 26
PP_S5D, PP_BGLU, PP_S5N = 27, 29, 31
PP_RCW, PP_RCB, PP_RBA, PP_RBX, PP_RLAM, PP_RN = 33, 41, 43, 45, 47, 49
PP_MCW, PP_MCB = 51, 117
PP_LRE, PP_LIM, PP_LDT = 139, 147, 155
PP_LRET, PP_LIMT, PP_LDTT = 163, 291, 419
PP_BRE, PP_BIM, PP_BRET, PP_BIMT, PP_CRE, PP_CIM, NPP = 547, 675, 803, 931, 1059, 1187, 1316


class Prog:
    def __init__(self, nc):
        self.nc = nc
        self.streams = {e: [] for e in ENG}
        self.cnt = {e: 0 for e in ENG}
        self.known = {e: {} for e in ENG}
        self.snap = {}
        self.lastw = {}
        self.readers = {}
        self.dcnt = {}
        self.pe_pending = []
        self.pe_reads = []
        self.pe_writes = []
        self.ekeys = set()

    def _need(self, E, ev, waits):
        s, v = ev
        if self.known[E].get(s, 0) >= v:
            return
        if '#' in s:
            e0, ep = s.split('#')
            for s2 in self.known[E]:
                if s2.startswith(e0 + '#') and int(s2.split('#')[1]) > int(ep):
                    return
        waits[s] = max(waits.get(s, 0), v)

    def _collect(self, E, reads, writes):
        waits = {}
        for k in reads:
            w = self.lastw.get(k)
            if w is not None:
                self._need(E, w, waits)
        for k in writes:
            w = self.lastw.get(k)
            if w is not None and (E != 'pe' or w[0].split('#')[0] != E):
                self._need(E, w, waits)
            for r in self.readers.get(k, ()):
                if E != 'pe' or r[0].split('#')[0] != E:
                    self._need(E, r, waits)
        kn = self.known[E]
        for s, v in waits.items():
            self.streams[E].append(('wait', s, v))
            if kn.get(s, 0) < v:
                kn[s] = v
            sn = self.snap.get((s, v))
            if sn:
                for s2, v2 in sn.items():
                    if kn.get(s2, 0) < v2:
                        kn[s2] = v2

    def _register(self, ev, reads, writes):
        for k in writes:
            self.lastw[k] = ev
            self.readers[k] = []
        for k in reads:
            self.readers.setdefault(k, []).append(ev)

    EPOCH = 1500

    def _tick(self, E):
        self.cnt[E] += 1
        ep = (self.cnt[E] - 1) // self.EPOCH
        key = '%s#%d' % (E, ep)
        self.ekeys.add(key)
        return (key, self.cnt[E] - ep * self.EPOCH)

    def op(self, E, fn, reads=(), writes=()):
        self._collect(E, reads, writes)
        ev = self._tick(E)
        self.snap[ev] = dict(self.known[E])
        self.streams[E].append(('op', fn, ev[0], 1))
        self._register(ev, reads, writes)

    def mm(self, fn, reads=(), writes=(), last=True):
        self.pe_pending.append(fn)
        self.pe_reads += list(reads)
        self.pe_writes += list(writes)
        if last:
            self._collect('pe', self.pe_reads, self.pe_writes)
            ev = self._tick('pe')
            self.snap[ev] = dict(self.known['pe'])
            for f in self.pe_pending[:-1]:
                self.streams['pe'].append(('op', f, None, 0))
            self.streams['pe'].append(('op', self.pe_pending[-1], ev[0], 1))
            self._register(ev, self.pe_reads, self.pe_writes)
            self.pe_pending, self.pe_reads, self.pe_writes = [], [], []

    def dma(self, Q, sem, out, in_, reads=(), writes=()):
        self._collect(Q, reads, writes)
        self.dcnt[sem] = self.dcnt.get(sem, 0) + 16
        ev = ('d:' + sem, self.dcnt[sem])
        self.snap[ev] = dict(self.known[Q])
        self.streams[Q].append(('op', lambda e: e.dma_start(out=out, in_=in_), 'd:' + sem, 16))
        self._register(ev, reads, writes)

    def barrier(self):
        assert not self.pe_pending
        evs = []
        for e in ENG:
            if self.cnt[e] > 0:
                ep = (self.cnt[e] - 1) // self.EPOCH
                evs.append(('%s#%d' % (e, ep), self.cnt[e] - ep * self.EPOCH))
        evs += [('d:' + s, v) for s, v in self.dcnt.items()]
        for E in ENG:
            waits = {}
            for ev in evs:
                if ev[0].split('#')[0] != E:
                    self._need(E, ev, waits)
            for s, v in waits.items():
                self.streams[E].append(('wait', s, v))
                self.known[E][s] = v

    def emit(self):
        import contextlib
        nc = self.nc
        self.barrier()
        names = sorted(self.ekeys) + ['d:' + s for s in self.dcnt]
        with contextlib.ExitStack() as st:
            sems = {}
            for i, n in enumerate(names):
                sems[n] = st.enter_context(nc.semaphore('s%d' % i))
            block = st.enter_context(nc.Block())

            def run(E):
                def body(eng):
                    for it in self.streams[E]:
                        if it[0] == 'wait':
                            eng.wait_ge(sems[it[1]], it[2])
                        else:
                            ins = it[1](eng)
                            if it[2] is not None:
                                ins.then_inc(sems[it[2]], it[3])
                return body

            block.tensor(run('pe'))
            block.scalar(run('act'))
            block.vector(run('dve'))
            block.gpsimd(run('pool'))
            block.sync(run('sp'))


def build_program(nlayers=DEPTH, stop=None, dbg_names=()):
    nc = bass.Bass("TRN2", target_bir_lowering=False)
    P = Prog(nc)

    def dram(name, shape, dt=F32, kind="ExternalInput"):
        return nc.dram_tensor(name, shape, dt, kind=kind).ap()

    x_d = dram("x", [NT, DM])
    out_d = dram("out", [NT, DM], kind="ExternalOutput")
    cc_d = dram("cc", [128, NCC])
    pp_d = dram("pp", [DEPTH, 128, NPP])
    win_d = dram("w_in", [DEPTH, DM, 2320])
    wout_d = dram("w_out", [DEPTH, DM, DM])
    wup_d = dram("w_up", [DEPTH, DM, 2 * DFF])
    wdn_d = dram("w_down", [DEPTH, DFF, DM])
    wgk_d = dram("wgk", [DEPTH, 16, 256])
    rwa_d = dram("rwa", [DEPTH, 4, 64, 64])
    rwx_d = dram("rwx", [DEPTH, 4, 64, 64])
    wglu_d = dram("wglu", [DEPTH, 256, 256])

    cur = [16512]
    SB_TOP = 229376
    uid = [0]

    def alloc(shape, dt, at=None):
        size = int(np.prod(shape[1:])) * (4 if dt in (F32, I32) else 2)
        off = ((cur[0] if at is None else at) + 31) // 32 * 32
        assert off + size <= SB_TOP, (shape, off, size)
        uid[0] += 1
        t = nc.alloc_sbuf_tensor_at("t%d" % uid[0], list(shape), dt, offset=off)
        if at is None:
            cur[0] = off + size
        return t, off + size

    def A_(shape, dt):
        return alloc(shape, dt)[0]

    xT = A_([128, 8, NT], F32)
    A = A_([128, 8, HALF], BF16)
    cst = A_([128, NCC], F32)
    pp = A_([128, NPP], F32)
    identb = A_([128, 128], BF16)
    onesb = A_([128, 128], BF16)
    segb = A_([128, HALF], BF16)
    gains = A_([128, 40], F32)
    rgc = A_([128, 8], F32)
    E5 = A_([128, 8, 9, 2, 32], BF16)
    WH = A_([128, 2, 8, 2, 128], BF16)
    KT = A_([128, 2, 8, 128], BF16)
    s5sm = A_([128, 24], F32)
    S5S = A_([128, 8, 2, 129], BF16)
    Gsl = A_([128, 8, 2], F32)
    wgk = A_([128, 256], BF16)
    rgw = A_([128, 2, 2, 64], BF16)
    wglu = A_([128, 2, 256], BF16)
    Sst = A_([128, 2, 128], F32)
    hlast = A_([128, 2], F32)
    rxhist = A_([128, 2, 3], F32)
    carry = A_([128, NJ, 2], F32)
    gsc = A_([128, 3, 2, 16], F32)
    wb0 = cur[0]
    upgv = [A_([128, 8, 256], BF16) for _ in range(2)]
    wdn = [A_([128, NJ, 128], BF16) for _ in range(2)]
    wb1 = cur[0]
    wct = []
    o = wb0
    for _ in range(3):
        t, o = alloc([128, 8, 128], BF16, at=o)
        wct.append(t)
    wv, o = alloc([128, 8, 512], BF16, at=o)
    yrg, o = alloc([128, 2, HALF], BF16, at=o)
    assert o <= wb1
    r0 = cur[0]
    qT = A_([128, 2, HALF], BF16)
    kT = A_([128, 2, HALF], BF16)
    gT = A_([128, 4, HALF], BF16)
    vtok = A_([128, 8, 512], BF16)
    gkl = A_([128, HALF], BF16)
    uT = A_([128, 2, HALF], BF16)
    rx = A_([128, 2, 3 + HALF], F32)
    rgT = A_([128, 2, HALF], BF16)
    ZT = A_([128, 10, 512], F32)
    r_end = cur[0]
    print("SBUF used", r_end, "of", SB_TOP, "R size", r_end - r0)
    o = r0
    interm, o = alloc([128, NJ, HALF], BF16, at=o)
    upsb = []
    for _ in range(3):
        t, o = alloc([128, 2 + 512], F32, at=o)
        upsb.append(t)
    fcv = []
    for _ in range(3):
        t, o = alloc([128, 512], F32, at=o)
        fcv.append(t)
    assert o <= r_end, (o, r_end)
    o = r0
    xs = []
    for _ in range(2):
        t, o = alloc([128, DM], F32, at=o)
        xs.append(t)
    xo, o = alloc([128, 8, 512], F32, at=o)
    assert o <= r_end
    o = r0
    st_ = {}

    def SA(name, shape, dt=F32):
        nonlocal o
        t, o2 = alloc(shape, dt, at=o)
        o = o2
        st_[name] = t
        return t

    for n in ['dt', 'ar', 'th', 'nr', 'ni', 'den', 'kr', 'ki', 'w8a', 'w8b']:
        SA(n, [128, 8])
    for n in ['tA', 'tB', 'sn9', 'cs9', 'Pr', 'Pi', 'w9a', 'w9b']:
        SA(n, [128, 8, 9])
    SA('w9i', [128, 8, 9], I32)
    SA('w8i', [128, 8], I32)
    for n in ['Bbr', 'Bbi', 'w16a', 'w16b']:
        SA(n, [128, 8, 16])
    SA('t1', [128, 8, 9, 16]); SA('t2', [128, 8, 9, 16])
    SA('Bw', [128, 8, 2, 128], BF16)
    for n in ['dtT', 'arT', 'thT', 'nrT', 'niT', 'denT', 'krT', 'kiT', 'wTa', 'wTb', 'BbrT', 'BbiT']:
        SA(n, [128, 2, 64])
    for n in ['angT', 'wkf', 'snT', 'csT', 'mgT', 'WHr', 'WHi', 'wk2']:
        SA(n, [128, 2, 8, 64])
    SA('wki', [128, 2, 8, 64], I32)
    assert o <= r_end, (o, r_end)

    ps = nc.alloc_psum_tensor("ps", [128, 8, 512], F32)
    psb = ps[:, 7, :].bitcast(BF16).rearrange("p (s c) -> p s c", c=128)

    def PS(b):
        return 'ps%d' % b

    def ACT(out, in_, func, r, w, bias=None, scale=None):
        kw = {}
        if bias is not None:
            kw['bias'] = bias
        if scale is not None:
            kw['scale'] = scale
        P.op('act', lambda e: e.activation(out=out, in_=in_, func=func, **kw), r, w)

    def TT(out, in0, in1, op, r, w, eng='dve'):
        P.op(eng, lambda e: e.tensor_tensor(out=out, in0=in0, in1=in1, op=op), r, w)

    def TS(out, in0, s1, s2, op0, op1, r, w, eng='dve'):
        if s2 is None:
            P.op(eng, lambda e: e.tensor_scalar(out=out, in0=in0, scalar1=s1, scalar2=None, op0=op0), r, w)
        else:
            P.op(eng, lambda e: e.tensor_scalar(out=out, in0=in0, scalar1=s1, scalar2=s2, op0=op0, op1=op1), r, w)

    def STT(out, in0, scalar, in1, op0, op1, r, w, eng='dve'):
        P.op(eng, lambda e: e.scalar_tensor_tensor(out=out, in0=in0, scalar=scalar, in1=in1, op0=op0, op1=op1), r, w)

    def CP(out, in_, r, w, eng='dve'):
        if eng == 'act':
            P.op('act', lambda e: e.copy(out=out, in_=in_), r, w)
        else:
            P.op(eng, lambda e: e.tensor_copy(out=out, in_=in_), r, w)

    def MSET(ap, val, w, eng='dve'):
        P.op(eng, lambda e: e.memset(ap, val), (), w)

    def SCAN(out, d0, d1, init, r, w):
        P.op('dve', lambda e: e.tensor_tensor_scan(out=out, data0=d0, data1=d1, initial=init, op0=ALU.mult, op1=ALU.add), r, w)

    def MM(out, lhsT, rhs, start, stop, r=(), w=(), last=True, tp=None, sgc=False):
        kw = {}
        if tp is not None:
            kw['tile_position'] = tp
        if sgc:
            kw['skip_group_check'] = True
        P.mm(lambda e: e.matmul(out, lhsT=lhsT, rhs=rhs, start=start, stop=stop, **kw), r, w, last)

    def TR(out, in_, ident, r, w, last=True):
        P.mm(lambda e: e.transpose(out, in_, ident), r, w, last)

    def bc(ap, shape):
        return ap.broadcast_to(list(shape))

    def rsqrt_from_sum(out, ss_ps, n, r, w):
        ACT(out, ss_ps, AF.Ln, r, w, bias=epsb[n])
        ACT(out, out, AF.Exp, w, w, scale=-0.5)

    dbg = {}

    P.dma('sp', 'cst', cst[:], cc_d, writes=['cst'])
    P.dma('pool', 'idb', identb[:], cc_d[:, CC_ID:CC_ID + 128], writes=['identb'])
    P.dma('pool', 'sgb', segb[:], cc_d[:, CC_SEG:CC_SEG + HALF], writes=['segb'])
    MSET(onesb[:], 1.0, ['onesb'])
    MSET(E5[:], 0.0, ['E5'], eng='pool')
    epst = A_([128, 4], F32)
    MSET(epst[:, 0:1], 128 * EPS, ['epst'])
    MSET(epst[:, 1:2], 256 * EPS, ['epst'])
    MSET(epst[:, 2:3], 1024 * EPS, ['epst'])
    epsb = {128: epst[:, 0:1], 256: epst[:, 1:2], 1024: epst[:, 2:3]}
    upgv.append(A_([128, 8, 256], BF16))
    hpi = A_([128, 1], F32)
    lneighth = A_([128, 1], F32)
    MSET(hpi[:], PI / 2, ['hpi'])
    MSET(lneighth[:], math.log(0.125), ['hpi'])
    ident = cst[:, CC_ID:CC_ID + 128]
    cmask = cst[:, CC_MASK:CC_MASK + 128]

    xs_in = [alloc([128, DM], F32, at=r_end - 10 * 2048 + i_ * 4096)[0] for i_ in range(2)]

    def load_x(tts):
        for tt in tts:
            s = xs_in[tt % 2]
            sk = 'xsi%d' % (tt % 2)
            P.dma('sp', sk, s[:], x_d[tt * 128:(tt + 1) * 128, :], writes=[sk])
            for kq in range(2):
                b = 4 + (tt * 2 + kq) % 4
                for kk in range(4):
                    k = kq * 4 + kk
                    TR(ps[:, b, kk * 128:(kk + 1) * 128], s[:, k * 128:(k + 1) * 128], ident,
                       [sk, 'cst'], [PS(b)], last=(kk == 3))
                CP(xT[:, kq * 4:(kq + 1) * 4, tt * 128:(tt + 1) * 128],
                   ps[:, b, :].rearrange("p (k t) -> p k t", k=4),
                   [PS(b)], ['x%d_%d' % (kq * 4 + kk_, tt // 4) for kk_ in range(4)], eng=('act' if kq == 0 else 'dve'))

    def rmsnorm_to_A(l, hf, gcol):
        for sb in range(2):
            t0 = hf * HALF + sb * 512
            sqv = sqbuf[:]
            for k in range(8):
                ACT(sqv[:, k, :], xT[:, k, t0:t0 + 512], AF.Square, ['x%d_%d' % (k, t0 // 512)], ['z%d' % (k // 2)])
            for k in range(8):
                MM(ps[:, 6, :], onesb[:], sqv[:, k, :], k == 0, k == 7, ['onesb', 'z%d' % (k // 2)], [PS(6)], last=(k == 7))
            rsqrt_from_sum(rstd_n, ps[:, 6, :], 1024, [PS(6), 'epst'], ['z4'])
            for k in range(8):
                STT(A[:, k, sb * 512:(sb + 1) * 512], xT[:, k, t0:t0 + 512], gains[:, gcol + k:gcol + k + 1],
                    rstd_n, ALU.mult, ALU.mult, ['x%d_%d' % (k, t0 // 512), 'gains', 'z4'], ['A%d_%d' % (k, sb)])

    sqbuf_t, _ = alloc([128, 8, 512], BF16, at=r_end - 10 * 2048)
    sqbuf = sqbuf_t
    rstd_n = ZT[:, 4, :]
    ktok_t, _ = alloc([128, 8, 2, 128], BF16, at=r_end - 2 * 2048)

    def layer_setup(l):
        P.dma('sp', 'pp', pp[:], pp_d[l], writes=['pp'])
        P.dma('pool', 'wgk', wgk[0:16, :], wgk_d[l], writes=['wgk'])
        for gi, wd in enumerate((rwa_d, rwx_d)):
            src = wd[l].rearrange("(k hl) i j -> hl i k j", hl=2)
            for hl in range(2):
                P.dma('pool', 'rgw', rgw[hl * 64:(hl + 1) * 64, gi, :, :], src[hl], writes=['rgw'])
        P.dma('pool', 'wglu', wglu[:], wglu_d[l].rearrange("(k p) c -> p k c", p=128), writes=['wglu'])
        TS(gains[:, 0:24], pp[:, PP_AN:PP_AN + 24], 32.0, None, ALU.mult, None, ['pp'], ['gains'])
        TS(gains[:, 24:25], pp[:, PP_GN:PP_GN + 1], math.sqrt(128.0), None, ALU.mult, None, ['pp'], ['gains'])
        TS(gains[:, 25:27], pp[:, PP_S5N:PP_S5N + 2], 16.0, None, ALU.mult, None, ['pp'], ['gains'])
        TS(gains[:, 27:29], pp[:, PP_RN:PP_RN + 2], 16.0, None, ALU.mult, None, ['pp'], ['gains'])
        ACT(rgc[:, 0:2], pp[:, PP_RLAM:PP_RLAM + 2], AF.Exp, ['pp'], ['rgc'], scale=-1.0)
        ACT(rgc[:, 0:2], rgc[:, 0:2], AF.Ln, ['rgc'], ['rgc'], bias=1.0)
        TS(rgc[:, 2:4], rgc[:, 0:2], -16.0, None, ALU.mult, None, ['rgc'], ['rgc'])
        TS(rgc[:, 0:2], rgc[:, 0:2], -8.0, None, ALU.mult, None, ['rgc'], ['rgc'])
        TS(rgc[:, 4:6], pp[:, PP_BGK:PP_BGK + 2], -1.0, None, ALU.mult, None, ['pp'], ['rgc'])
        MSET(Sst[:], 0.0, ['Sst'])
        MSET(hlast[:], 0.0, ['hlast'])
        MSET(carry[:], 0.0, ['carry'])
        MSET(S5S[:, :, :, 0:1], 0.0, ['S5S'])
        MSET(Gsl[:], 0.0, ['Gsl'])

    def inproj(l, hf):
        W = win_d[l]

        def load_ct(slot, c0, ncols=128):
            P.dma('pool', 'wct%dL%d' % (slot, l), wct[slot][:, :, 0:ncols],
                  W[:, c0:c0 + ncols].rearrange("(k p) c -> p k c", p=128), writes=['wct%d' % slot])

        tiles = []
        for t in range(2):
            tiles.append((lambda sb, t=t: rx[:, t, 3 + sb * 512:3 + (sb + 1) * 512], 1808 + t * 128, 'rx%d' % t))
        for t in range(2):
            tiles.append((lambda sb, t=t: rgT[:, t, sb * 512:(sb + 1) * 512], 2064 + t * 128, 'rgT%d' % t))
        for t in range(2):
            tiles.append((lambda sb, t=t: qT[:, t, sb * 512:(sb + 1) * 512], 0 + t * 128, 'qT%d' % t))
        for t in range(2):
            tiles.append((lambda sb, t=t: kT[:, t, sb * 512:(sb + 1) * 512], 256 + t * 128, 'kT%d' % t))
        for t in range(4):
            tiles.append((lambda sb, t=t: gT[:, t, sb * 512:(sb + 1) * 512], 1024 + t * 128, 'gT%d' % t))
        for t in range(2):
            tiles.append((lambda sb, t=t: uT[:, t, sb * 512:(sb + 1) * 512], 1552 + t * 128, 'uT%d' % t))
        cnt = 0
        P.dma('pool', 'wvL%d' % l, wv[:], W[:, 512:1024].rearrange("(k p) c -> p k c", p=128), writes=['wv'])
        load_ct(0, tiles[0][1])
        load_ct(1, tiles[1][1])
        for i, (dst, c0, key) in enumerate(tiles):
            slot = i % 3
            if i + 2 < len(tiles):
                load_ct((i + 2) % 3, tiles[i + 2][1])
            elif i + 2 == len(tiles):
                load_ct((i + 2) % 3, 1536, 16)
            for sb in range(2):
                b = cnt % 4
                for k in range(8):
                    MM(ps[:, b, :], wct[slot][:, k, :], A[:, k, sb * 512:(sb + 1) * 512], k == 0, k == 7,
                       ['wct%d' % slot, 'A%d_%d' % (k, sb)], [PS(b)], last=(k == 7))
                CP(dst(sb), ps[:, b, :], [PS(b)], ['%s_%d' % (key, sb)], eng=('act' if cnt % 2 == 0 else 'dve'))
                cnt += 1
            if i == 3:
                rg_pre(l, hf)
                chain = rg_chain(l, hf)
            elif i > 3:
                next(chain, None)
        gs = len(tiles) % 3
        for sb in range(2):
            b = cnt % 4
            for k in range(8):
                MM(ps[0:16, b, :], wct[gs][:, k, 0:16], A[:, k, sb * 512:(sb + 1) * 512], k == 0, k == 7,
                   ['wct%d' % gs, 'A%d_%d' % (k, sb)], [PS(b)], last=(k == 7))
            CP(gkl[0:16, sb * 512:(sb + 1) * 512], ps[0:16, b, :], [PS(b)], ['gkl_%d' % sb], eng='act')
            cnt += 1
            next(chain, None)
        for tb in range(8):
            b = cnt % 4
            sb = tb // 4
            for k in range(8):
                MM(ps[:, b, :], A[:, k, tb * 128:(tb + 1) * 128], wv[:, k, :], k == 0, k == 7,
                   ['wv', 'A%d_%d' % (k, sb)], [PS(b)], last=(k == 7))
            CP(vtok[:, tb, :], ps[:, b, :], [PS(b)], ['vtok%d' % tb], eng=('act' if cnt % 2 == 0 else 'dve'))
            cnt += 1
            next(chain, None)
        for _ in chain:
            pass

    def rg_pre(l, hf):
        for k in range(2):
            CP(rx[:, k, 0:3], rxhist[:, k, :], ['rxhist'], ['rxh'], eng='pool')
        for k in range(2):
            for sb in range(2):
                gk_ = 'rgT%d_%d' % (k, sb)
                ACT(rgT[:, k, sb * 512:(sb + 1) * 512], rgT[:, k, sb * 512:(sb + 1) * 512], AF.Gelu_apprx_tanh, [gk_], [gk_])

    def rg_iter(l, hf, sb, k):
        z = lambda i: ZT[:, i, :]
        lo = sb * 512
        xc, rb, ib, ab, hb = z(0), z(1), z(2), z(3), z(4)
        xcb = ZT[:, 5, 0:256].bitcast(BF16)
        rxk = ['rx%d_%d' % (k, sb), 'rxh'] + (['rx%d_%d' % (k, sb - 1)] if sb else [])
        w = lambda tap: pp[:, PP_RCW + k * 4 + tap:PP_RCW + k * 4 + tap + 1]
        TS(xc, rx[:, k, 3 + lo:3 + lo + 512], w(3), pp[:, PP_RCB + k:PP_RCB + k + 1], ALU.mult, ALU.add,
           rxk + ['pp'], ['z0'])
        for tap in range(3):
            STT(xc, rx[:, k, tap + lo:tap + lo + 512], w(tap), xc, ALU.mult, ALU.add, rxk + ['pp', 'z0'], ['z0'])
        CP(xcb, xc, ['z0'], ['z5'], eng='act')
        yield
        for gi in range(2):
            b = 4 + gi
            for hl in range(2):
                MM(ps[hl * 64:(hl + 1) * 64, b, :], rgw[hl * 64:(hl + 1) * 64, gi, k, :],
                   xcb[hl * 64:(hl + 1) * 64, :], True, True, ['rgw', 'z5'], [PS(b)], last=(hl == 1))
        yield
        for gi, (dst, bcol) in enumerate(((rb, PP_RBA), (ib, PP_RBX))):
            b = 4 + gi
            ACT(dst, ps[:, b, :], AF.Sigmoid, [PS(b), 'pp'], ['z%d' % (1 + gi)], bias=pp[:, bcol + k:bcol + k + 1])
        ACT(ab, rb, AF.Exp, ['z1', 'rgc'], ['z3'], scale=rgc[:, k:k + 1])
        ACT(rb, rb, AF.Exp, ['z1', 'rgc'], ['z1'], scale=rgc[:, 2 + k:3 + k])
        TS(rb, rb, 0.999999, None, ALU.min, None, ['z1'], ['z1'])
        ACT(rb, rb, AF.Ln, ['z1'], ['z1'], scale=-1.0, bias=1.0)
        ACT(rb, rb, AF.Exp, ['z1'], ['z1'], scale=0.5)
        yield
        TT(ib, ib, xc, ALU.mult, ['z2', 'z0'], ['z2'])
        TT(ib, ib, rb, ALU.mult, ['z2', 'z1'], ['z2'])
        SCAN(hb, ab, ib, hlast[:, k:k + 1], ['z3', 'z2', 'hlast'], ['z4'])
        CP(hlast[:, k:k + 1], hb[:, 511:512], ['z4'], ['hlast'], eng='pool')
        TT(ZT[:, 6 + k, :], hb, rgT[:, k, lo:lo + 512], ALU.mult, ['z4', 'rgT%d_%d' % (k, sb)], ['z%d' % (6 + k)])
        ACT(ZT[:, 8, k * 256:(k + 1) * 256].bitcast(BF16), ZT[:, 6 + k, :], AF.Square, ['z%d' % (6 + k)], ['z8'])
        yield

    def rg_chain(l, hf):
        for sb in range(2):
            for k in range(2):
                yield from rg_iter(l, hf, sb, k)
            rg_norm(l, hf, sb)
            yield

    def rg_norm(l, hf, sb):
        lo = sb * 512
        sqv = ZT[:, 8, :].bitcast(BF16)
        for k in range(2):
            MM(ps[:, 6, :], onesb[:], sqv[:, k * 512:(k + 1) * 512], k == 0, k == 1, ['onesb', 'z8'], [PS(6)], last=(k == 1))
        rsqrt_from_sum(ZT[:, 9, :], ps[:, 6, :], 256, [PS(6), 'epst'], ['z9'])
        for k in range(2):
            STT(yrg[:, k, lo:lo + 512], ZT[:, 6 + k, :], gains[:, 27 + k:28 + k], ZT[:, 9, :], ALU.mult, ALU.mult,
                ['z%d' % (6 + k), 'gains', 'z9'], ['yrg%d_%d' % (k, sb)])

    def rg_post(l, hf):
        for k in range(2):
            CP(rxhist[:, k, :], rx[:, k, HALF:HALF + 3], ['rx%d_1' % k], ['rxhist'], eng='pool')
            for sb in range(2):
                CP(A[:, 6 + k, sb * 512:(sb + 1) * 512], yrg[:, k, sb * 512:(sb + 1) * 512], ['yrg%d_%d' % (k, sb)],
                   ['A%d_%d' % (6 + k, sb)], eng='pool')


    TWO_PI = 2.0 * PI

    def bcl(ap, n):
        return ap.unsqueeze(2).broadcast_to([ap.shape[0], ap.shape[1], n])

    def sincos(X, sn, cs, wf, wi, r, w):
        TS(wi, X, 1.0 / TWO_PI, None, ALU.mult, None, r, w)
        CP(wf, wi, w, w)
        STT(wf, wf, -TWO_PI, X, ALU.mult, ALU.add, r + w, w)
        TS(wf, wf, -3.14159, 3.14159, ALU.max, ALU.min, w, w)
        ACT(sn, wf, AF.Sin, w, w)
        STT(wf, wf, -1.0, wf, ALU.mult, ALU.max, w, w)
        ACT(cs, wf, AF.Sin, w, w, scale=-1.0, bias=hpi[:])

    def kappa(P1r, P1i, lre, lim, nr, den, kr, ki, wa, r, w):
        TS(nr, P1r, -1.0, None, ALU.add, None, r, w)
        TT(den, lre, lre, ALU.mult, r, w)
        TT(wa, lim, lim, ALU.mult, r, w)
        TT(den, den, wa, ALU.add, w, w)
        P.op('dve', lambda e: e.reciprocal(out=den, in_=den), w, w)
        TT(kr, nr, lre, ALU.mult, r + w, w)
        TT(wa, P1i, lim, ALU.mult, r + w, w)
        TT(kr, kr, wa, ALU.add, w, w)
        TT(kr, kr, den, ALU.mult, w, w)
        TT(ki, P1i, lre, ALU.mult, r + w, w)
        TT(wa, nr, lim, ALU.mult, r + w, w)
        TT(ki, ki, wa, ALU.subtract, w, w)
        TT(ki, ki, den, ALU.mult, w, w)

    def s5_setup(l, hooks=()):
        hooks = list(hooks)

        def hook():
            if hooks:
                hooks.pop(0)()
        T = st_
        hook()
        r_ = ['pp', 'cst']
        w_ = ['s5t']
        idx9 = cst[:, CC_IDX9:CC_IDX9 + 9]
        idx8 = cst[:, CC_IDX9:CC_IDX9 + 8]
        lre, lim = pp[:, PP_LRE:PP_LRE + 8], pp[:, PP_LIM:PP_LIM + 8]
        ACT(T['dt'][:], pp[:, PP_LDT:PP_LDT + 8], AF.Exp, r_, w_)
        TT(T['ar'][:], lre, T['dt'][:], ALU.mult, r_ + w_, w_)
        TT(T['th'][:], lim, T['dt'][:], ALU.mult, r_ + w_, w_)
        i9b = idx9.unsqueeze(1).broadcast_to([128, 8, 9])
        TT(T['tA'][:], bcl(T['ar'][:], 9), i9b, ALU.mult, r_ + w_, w_)
        TT(T['tB'][:], bcl(T['th'][:], 9), i9b, ALU.mult, r_ + w_, w_)
        ACT(T['tA'][:], T['tA'][:], AF.Exp, w_, w_)
        sincos(T['tB'][:], T['sn9'][:], T['cs9'][:], T['w9a'][:], T['w9i'][:], w_, w_)
        TT(T['Pr'][:], T['tA'][:], T['cs9'][:], ALU.mult, w_, w_)
        TT(T['Pi'][:], T['tA'][:], T['sn9'][:], ALU.mult, w_, w_)
        hook()
        kappa(T['Pr'][:, :, 1], T['Pi'][:, :, 1], lre, lim, T['nr'][:], T['den'][:], T['kr'][:], T['ki'][:], T['w8a'][:], r_ + w_, w_)
        Bre = pp[:, PP_BRE:PP_BRE + 128].rearrange("p (a i) -> p a i", i=16)
        Bim = pp[:, PP_BIM:PP_BIM + 128].rearrange("p (a i) -> p a i", i=16)
        krb, kib = bcl(T['kr'][:], 16), bcl(T['ki'][:], 16)
        TT(T['Bbr'][:], krb, Bre, ALU.mult, r_ + w_, w_)
        TT(T['w16a'][:], kib, Bim, ALU.mult, r_ + w_, w_)
        TT(T['Bbr'][:], T['Bbr'][:], T['w16a'][:], ALU.subtract, w_, w_)
        TT(T['Bbi'][:], krb, Bim, ALU.mult, r_ + w_, w_)
        TT(T['w16a'][:], kib, Bre, ALU.mult, r_ + w_, w_)
        TT(T['Bbi'][:], T['Bbi'][:], T['w16a'][:], ALU.add, w_, w_)
        MSET(T['Bw'][:], 0.0, w_, eng='pool')
        hook()
        for gl in range(2):
            for ri, Bb in enumerate((T['Bbr'], T['Bbi'])):
                for k in range(2):
                    dst = bass.AP(T['Bw'], (gl * 64) * 2048 + (4 * k) * 256 + ri * 128 + gl * 16,
                                  [[2048, 64], [256 + 32, 4], [1, 16]])
                    CP(dst, Bb[gl * 64:(gl + 1) * 64, 4 * k:4 * k + 4, :], w_, w_)
        Cre = pp[:, PP_CRE:PP_CRE + 128].rearrange("p (a j) -> p a j", j=16)
        Cim = pp[:, PP_CIM:PP_CIM + 128].rearrange("p (a j) -> p a j", j=16)
        Cb = lambda C: C.unsqueeze(2).broadcast_to([128, 8, 9, 16])
        Pb = lambda Pt: Pt[:].unsqueeze(3).broadcast_to([128, 8, 9, 16])
        TT(T['t1'][:], Cb(Cre), Pb(T['Pr']), ALU.mult, r_ + w_, w_)
        TT(T['t2'][:], Cb(Cim), Pb(T['Pi']), ALU.mult, r_ + w_, w_)
        TT(T['t1'][:], T['t1'][:], T['t2'][:], ALU.subtract, w_, w_)
        for gl in range(2):
            CP(E5[gl * 64:(gl + 1) * 64, :, :, 0, gl * 16:(gl + 1) * 16], T['t1'][gl * 64:(gl + 1) * 64], w_, ['E5'])
        TT(T['t1'][:], Cb(Cre), Pb(T['Pi']), ALU.mult, r_ + w_, w_)
        TT(T['t2'][:], Cb(Cim), Pb(T['Pr']), ALU.mult, r_ + w_, w_)
        TT(T['t1'][:], T['t1'][:], T['t2'][:], ALU.add, w_, w_)
        for gl in range(2):
            TS(E5[gl * 64:(gl + 1) * 64, :, :, 1, gl * 16:(gl + 1) * 16], T['t1'][gl * 64:(gl + 1) * 64], -1.0, None,
               ALU.mult, None, w_, ['E5'])
        hook()
        while hooks:
            hook()
        for k in range(2):
            for a in range(4):
                pr = 4 * k + a
                for tau in range(8):
                    out = ps[:, 2 * k + tau // 4, (tau % 4) * 128 + 32 * a:(tau % 4) * 128 + 32 * a + 32]
                    MM(out, T['Bw'][:, pr, 0, :], E5[:, pr, tau, 0, :], True, False, ['s5t', 'E5'],
                       [PS(2 * k + tau // 4)], last=False, sgc=True)
                    MM(out, T['Bw'][:, pr, 1, :], E5[:, pr, tau, 1, :], False, True, [], [], last=(tau == 7), sgc=True)
            CP(KT[:, k, :, :], ps[:, 2 * k:2 * k + 2, :].rearrange("p b (t c) -> p (b t) c", c=128),
               [PS(2 * k), PS(2 * k + 1)], ['KT'], eng='act')
        qo = lambda ap: ap.rearrange("p (k a) -> p a k", k=2)
        ACT(s5sm[:, 0:8].rearrange("p (a k) -> p a k", k=2), qo(T['ar'][:]), AF.Exp, w_, ['s5sm'], scale=8.0)
        TS(T['w8i'][:], T['th'][:], 8.0 / TWO_PI, None, ALU.mult, None, w_, w_)
        CP(T['w8a'][:], T['w8i'][:], w_, w_)
        TS(T['w8b'][:], T['th'][:], 8.0, None, ALU.mult, None, w_, w_)
        STT(s5sm[:, 8:16].rearrange("p (a k) -> p a k", k=2), qo(T['w8a'][:]), -TWO_PI, qo(T['w8b'][:]), ALU.mult, ALU.add, w_, ['s5sm'])
        wT_ = ['s5tT']
        v3 = lambda c0: pp[:, c0:c0 + 128].rearrange("p (k q) -> p k q", q=64)
        lreT, limT = v3(PP_LRET), v3(PP_LIMT)
        ACT(T['dtT'][:], v3(PP_LDTT), AF.Exp, r_, wT_)
        TT(T['arT'][:], lreT, T['dtT'][:], ALU.mult, r_ + wT_, wT_)
        TT(T['thT'][:], limT, T['dtT'][:], ALU.mult, r_ + wT_, wT_)
        i8b = idx8.unsqueeze(1).unsqueeze(3).broadcast_to([128, 2, 8, 64])
        eb = lambda t: t.unsqueeze(2).broadcast_to([128, 2, 8, 64])
        TT(T['angT'][:], eb(T['thT'][:]), i8b, ALU.mult, r_ + wT_, wT_)
        TT(T['mgT'][:], eb(T['arT'][:]), i8b, ALU.mult, r_ + wT_, wT_)
        ACT(T['mgT'][:], T['mgT'][:], AF.Exp, wT_, wT_)
        sincos(T['angT'][:], T['snT'][:], T['csT'][:], T['wkf'][:], T['wki'][:], wT_, wT_)
        TT(T['csT'][:], T['csT'][:], T['mgT'][:], ALU.mult, wT_, wT_)
        TT(T['snT'][:], T['snT'][:], T['mgT'][:], ALU.mult, wT_, wT_)
        kappa(T['csT'][:, :, 1, :], T['snT'][:, :, 1, :], lreT, limT, T['nrT'][:], T['denT'][:], T['krT'][:], T['kiT'][:],
              T['wTa'][:], r_ + wT_, wT_)
        BreT, BimT = v3(PP_BRET), v3(PP_BIMT)
        TT(T['BbrT'][:], T['krT'][:], BreT, ALU.mult, r_ + wT_, wT_)
        TT(T['wTa'][:], T['kiT'][:], BimT, ALU.mult, r_ + wT_, wT_)
        TT(T['BbrT'][:], T['BbrT'][:], T['wTa'][:], ALU.subtract, wT_, wT_)
        TT(T['BbiT'][:], T['krT'][:], BimT, ALU.mult, r_ + wT_, wT_)
        TT(T['wTa'][:], T['kiT'][:], BreT, ALU.mult, r_ + wT_, wT_)
        TT(T['BbiT'][:], T['BbiT'][:], T['wTa'][:], ALU.add, wT_, wT_)
        TT(T['WHr'][:], T['csT'][:], eb(T['BbrT'][:]), ALU.mult, wT_, wT_)
        TT(T['wk2'][:], T['snT'][:], eb(T['BbiT'][:]), ALU.mult, wT_, wT_)
        TT(T['WHr'][:], T['WHr'][:], T['wk2'][:], ALU.subtract, wT_, wT_)
        TT(T['WHi'][:], T['csT'][:], eb(T['BbiT'][:]), ALU.mult, wT_, wT_)
        TT(T['wk2'][:], T['snT'][:], eb(T['BbrT'][:]), ALU.mult, wT_, wT_)
        TT(T['WHi'][:], T['WHi'][:], T['wk2'][:], ALU.add, wT_, wT_)
        mT = cst[:, CC_MASKT:CC_MASKT + 2].unsqueeze(1).unsqueeze(3).broadcast_to([128, 8, 2, 64])
        for k in range(2):
            for ri, Wx in enumerate((T['WHr'], T['WHi'])):
                out = WH[:, k, :, ri, :].rearrange("p e (g q) -> p e g q", g=2)
                TT(out, Wx[:, k, :, :].unsqueeze(2).broadcast_to([128, 8, 2, 64]), mT, ALU.mult, r_ + wT_, ['WH'])

    def s5_mixer(l, hf):
        c0 = hf * 128
        v8 = lambda ap: ap.rearrange("p (a c) -> p a c", c=128)
        Tc = ZT[:, 0:2, :].rearrange("p a (b c) -> p (a b) c", c=128)
        Ts = ZT[:, 2:4, :].rearrange("p a (b c) -> p (a b) c", c=128)
        Gr = ZT[:, 4:6, :].rearrange("p a (b c) -> p (a b) c", c=128)
        Gi = ZT[:, 6:8, :].rearrange("p a (b c) -> p (a b) c", c=128)
        Gsi = ZT[:, 8:10, :].rearrange("p a (b c) -> p (a b) c", c=128)
        tmp = v8(rx[:, 0, 3:3 + HALF])
        Gsr = v8(rx[:, 1, 3:3 + HALF])
        wi = rgT[:].rearrange("p a t -> p (a t)").bitcast(I32).rearrange("p (a c) -> p a c", c=128)
        kTc, kTs, kGr, kGi, kGsi = ['z0', 'z1'], ['z2', 'z3'], ['z4', 'z5'], ['z6', 'z7'], ['z8', 'z9']
        ktmp, kGsr = ['rx0_0', 'rx0_1'], ['rx1_0', 'rx1_1']
        kwi = ['rgT0_0', 'rgT0_1', 'rgT1_0', 'rgT1_1']
        cidx = cst[:, CC_CIDX + c0:CC_CIDX + c0 + 128].unsqueeze(1).broadcast_to([128, 8, 128])
        TT(Tc, bcl(s5sm[:, 8:16], 128), cidx, ALU.mult, ['s5sm', 'cst'], kTc)
        TS(wi, Tc, 1.0 / TWO_PI, None, ALU.mult, None, kTc, kwi)
        CP(Gr, wi, kwi, kGr)
        STT(Gr, Gr, -TWO_PI, Tc, ALU.mult, ALU.add, kGr + kTc, kGr)
        TS(Gr, Gr, -3.14159, 3.14159, ALU.max, ALU.min, kGr, kGr)
        ACT(Ts, Gr, AF.Sin, kGr, kTs)
        STT(Gr, Gr, -1.0, Gr, ALU.mult, ALU.max, kGr, kGr)
        ACT(Tc, Gr, AF.Sin, kGr, kTc, scale=-1.0, bias=hpi[:])
        for k in range(2):
            uv = uT[:, k, :].rearrange("p (c s) -> p s c", s=8)
            for a in range(4):
                pr = 4 * k + a
                for ri in range(2):
                    off = k * 256 + ri * 128
                    for e in range(8):
                        MM(ps[:, a, off:off + 128], WH[32 * a:32 * a + 32, k, e, ri, :], uv[32 * a:32 * a + 32, 7 - e, :],
                           e == 0, e == 7, ['WH', 'uT%d_0' % k, 'uT%d_1' % k], [PS(a)], last=(e == 7),
                           tp=(32 * a, 0), sgc=True)
        import os
        cut = int(os.environ.get('S5CUT', '99'))
        if cut <= 0:
            return
        hv = ps[:, 0:4, :].rearrange("p b (q r c) -> p (b q) r c", r=2, c=128)
        hr, hi = hv[:, :, 0, :], hv[:, :, 1, :]
        kh = [PS(0), PS(1), PS(2), PS(3)]
        TT(tmp, hr, Tc, ALU.mult, kh + kTc, ktmp)
        TT(Gr, hi, Ts, ALU.mult, kh + kTs, kGr)
        TT(Gr, Gr, tmp, ALU.add, kGr + ktmp, kGr)
        TT(tmp, hr, Ts, ALU.mult, kh + kTs, ktmp)
        TT(Gi, hi, Tc, ALU.mult, kh + kTc, kGi)
        TT(Gi, Gi, tmp, ALU.subtract, kGi + ktmp, kGi)
        for pr in range(8):
            d0 = s5sm[:, pr:pr + 1].broadcast_to([128, 128])
            SCAN(Gsr[:, pr, :], d0, Gr[:, pr, :], Gsl[:, pr, 0:1], kGr + ['s5sm', 'Gsl'], kGsr)
            SCAN(Gsi[:, pr, :], d0, Gi[:, pr, :], Gsl[:, pr, 1:2], kGi + ['s5sm', 'Gsl'], kGsi)
        CP(Gsl[:, :, 0], Gsr[:, :, 127], kGsr, ['Gsl'], eng='pool')
        CP(Gsl[:, :, 1], Gsi[:, :, 127], kGsi, ['Gsl'], eng='pool')
        TT(tmp, Gsr, Tc, ALU.mult, kGsr + kTc, ktmp)
        TT(Gr, Gsi, Ts, ALU.mult, kGsi + kTs, kGr)
        TT(S5S[:, :, 0, 1:129], tmp, Gr, ALU.subtract, ktmp + kGr, ['S5S'])
        TT(tmp, Gsr, Ts, ALU.mult, kGsr + kTs, ktmp)
        TT(Gr, Gsi, Tc, ALU.mult, kGsi + kTc, kGr)
        TT(S5S[:, :, 1, 1:129], tmp, Gr, ALU.add, ktmp + kGr, ['S5S'])
        if cut <= 1:
            return
        ybank = {0: (4, 5), 1: (0, 1)}
        for k in range(2):
            uv = uT[:, k, :].rearrange("p (c s) -> p s c", s=8)
            bks = ybank[k]
            for tau in range(8):
                for b2 in range(2):
                    t_lo, t_hi = max(tau, 4 * b2), 4 * b2 + 4
                    if t_lo >= t_hi:
                        continue
                    out = ps[:, bks[b2], (t_lo - 4 * b2) * 128:(t_hi - 4 * b2) * 128]
                    MM(out, KT[:, k, tau, :], uv[:, t_lo - tau:t_hi - tau, :], tau == 0, False,
                       ['KT', 'uT%d_0' % k, 'uT%d_1' % k], [PS(bks[b2])], last=False, sgc=True)
            for a in range(4):
                pr = 4 * k + a
                for t in range(8):
                    for ri in range(2):
                        lastmm = (a == 3 and t == 7 and ri == 1)
                        out = ps[32 * a:32 * a + 32, bks[t // 4], (t % 4) * 128:(t % 4) * 128 + 128]
                        MM(out, E5[:, pr, t + 1, ri, :], S5S[:, 2 * a + k, ri, 0:128], False, lastmm,
                           ['E5', 'S5S'], [PS(bks[0]), PS(bks[1])], last=lastmm, tp=(0, 32 * a), sgc=True)
        if cut <= 2:
            return
        CP(S5S[:, :, :, 0:1], S5S[:, :, :, 128:129], ['S5S'], ['S5S'], eng='pool')
        for sb in range(2):
            lo = sb * 512
            y2b = ZT[:, 6, :].bitcast(BF16).rearrange("p (k t) -> p k t", k=2)
            for k in range(2):
                bks = ybank[k]
                yv = ps[:, bks[0]:bks[0] + 2, :].rearrange("p b (t c) -> p (b t) c", c=128)[:, :, 64 * sb:64 * sb + 64]
                yv = yv.rearrange("p t c -> p c t")
                y1 = ZT[:, 4 + k, :]
                STT(y1.rearrange("p (c t) -> p c t", t=8), uT[:, k, lo:lo + 512].rearrange("p (c t) -> p c t", t=8),
                    pp[:, PP_S5D + k:PP_S5D + k + 1], yv, ALU.mult, ALU.add,
                    ['uT%d_%d' % (k, sb), 'pp', PS(bks[0]), PS(bks[1])], ['z%d' % (4 + k)])
                ACT(y1, y1, AF.Gelu_apprx_tanh, ['z%d' % (4 + k)], ['z%d' % (4 + k)])
                CP(y2b[:, k, :], y1, ['z%d' % (4 + k)], ['z6'], eng='pool')
            for kk in range(2):
                for k in range(2):
                    MM(ps[:, 2 + kk, :], wglu[:, k, kk * 128:(kk + 1) * 128], y2b[:, k, :], k == 0, k == 1, ['wglu', 'z6'],
                       [PS(2 + kk)], last=(k == 1))
                ACT(ZT[:, 7, :], ps[:, 2 + kk, :], AF.Sigmoid, [PS(2 + kk), 'pp'], ['z7'], bias=pp[:, PP_BGLU + kk:PP_BGLU + kk + 1])
                TT(ZT[:, 4 + kk, :], ZT[:, 4 + kk, :], ZT[:, 7, :], ALU.mult, ['z%d' % (4 + kk), 'z7'], ['z%d' % (4 + kk)])
                ACT(ZT[:, 8, kk * 256:(kk + 1) * 256].bitcast(BF16), ZT[:, 4 + kk, :], AF.Square, ['z%d' % (4 + kk)], ['z8'])
            sqv = ZT[:, 8, :].bitcast(BF16)
            for kk in range(2):
                MM(ps[:, 6, :], onesb[:], sqv[:, kk * 512:(kk + 1) * 512], kk == 0, kk == 1, ['onesb', 'z8'], [PS(6)], last=(kk == 1))
            rsqrt_from_sum(ZT[:, 9, :], ps[:, 6, :], 256, [PS(6), 'epst'], ['z9'])
            for kk in range(2):
                STT(A[:, 4 + kk, lo:lo + 512], ZT[:, 4 + kk, :], gains[:, 25 + kk:26 + kk], ZT[:, 9, :], ALU.mult, ALU.mult,
                    ['z%d' % (4 + kk), 'gains', 'z9'], ['A%d_%d' % (4 + kk, sb)])

    def gla_mixer(l, hf):
        Lb = ZT[:, 0:4, :].rearrange("p (a b) t -> p a (b t)", a=2)
        kL = [['z0', 'z1'], ['z2', 'z3']]
        kq = lambda p2: ['qT%d_0' % p2, 'qT%d_1' % p2]
        kk_ = lambda p2: ['kT%d_0' % p2, 'kT%d_1' % p2]
        ktok = ktok_t
        PT = uT[:, 0, :].rearrange("p (s c) -> p s c", c=128)
        Sbf = uT[:, 1, :].rearrange("p (s q c) -> p s q c", q=2, c=128)
        osq = rgT[:].rearrange("p a (s t) -> p (a s) t", t=512)
        cnt = 0
        for h in range(4):
            for sb in range(2):
                gk_ = 'gT%d_%d' % (h, sb)
                ACT(gT[:, h, sb * 512:(sb + 1) * 512], gT[:, h, sb * 512:(sb + 1) * 512], AF.Silu, [gk_], [gk_])
        for p2 in range(2):
            for sb in range(2):
                b = cnt % 4
                cnt += 1
                MM(ps[:, b, :], wgk[0:16, p2 * 128:(p2 + 1) * 128], gkl[0:16, sb * 512:(sb + 1) * 512], True, True,
                   ['wgk', 'gkl_%d' % sb], [PS(b)])
                Lv = Lb[:, p2, sb * 512:(sb + 1) * 512]
                ACT(Lv, ps[:, b, :], AF.Exp, [PS(b), 'rgc'], [kL[p2][sb]], scale=-1.0, bias=rgc[:, 4 + p2:5 + p2])
                ACT(Lv, Lv, AF.Ln, [kL[p2][sb]], [kL[p2][sb]], bias=1.0)
            SCAN(Lb[:, p2, :], segb[:], Lb[:, p2, :], 0.0, kL[p2] + ['segb'], kL[p2])
            Lc = Lb[:, p2, :].rearrange("p (n c) -> p n c", c=64)
            Lmid, Llast = Lc[:, :, 31], Lc[:, :, 63]
            ACT(gsc[:, 0, p2, :], Llast, AF.Exp, kL[p2], ['gsc'], scale=-1.0 / 16)
            ACT(gsc[:, 2, p2, :], Lmid, AF.Exp, kL[p2], ['gsc'], scale=-1.0 / 16)
            TT(gsc[:, 1, p2, :], Llast, Lmid, ALU.subtract, kL[p2], ['gsc'])
            ACT(gsc[:, 1, p2, :], gsc[:, 1, p2, :], AF.Exp, ['gsc'], ['gsc'], scale=-1.0 / 16)
            import os
            gcut = int(os.environ.get('GLACUT', '99'))
            if gcut <= 0:
                continue
            D1 = rx[:, p2, 3:3 + HALF]
            kD = ['rx%d_0' % p2, 'rx%d_1' % p2]
            TT(D1.rearrange("p (n c) -> p n c", c=64), Lc, Lmid.unsqueeze(2).broadcast_to([128, 16, 64]), ALU.subtract, kL[p2], kD)
            eq = ZT[:, 4:6, :].rearrange("p a t -> p (a t)")
            ek = ZT[:, 6:8, :].rearrange("p a t -> p (a t)")
            ACT(eq, D1, AF.Exp, kD, ['z4', 'z5'], scale=-1.0 / 16, bias=lneighth[:])
            ACT(ek, D1, AF.Exp, kD, ['z6', 'z7'], scale=1.0 / 16)
            TT(qT[:, p2, :], qT[:, p2, :], eq, ALU.mult, kq(p2) + ['z4', 'z5'], kq(p2))
            TT(ek, kT[:, p2, :], ek, ALU.mult, kk_(p2) + ['z6', 'z7'], ['z6', 'z7'])
            CP(kT[:, p2, :], ek, ['z6', 'z7'], kk_(p2), eng='pool')
            if gcut <= 1:
                continue
            for g4 in range(2):
                bk = 4 + g4
                tks = ['scb%d' % g4]
                for i_ in range(4):
                    blk = 4 * g4 + i_
                    TR(ps[:, bk, i_ * 128:(i_ + 1) * 128], ek[:, blk * 128:(blk + 1) * 128], ident, ['z6', 'z7', 'cst'], tks,
                       last=(i_ == 3))
                CP(ktok[:, 4 * g4:4 * g4 + 4, p2, :], ps[:, bk, :].rearrange("p (b c) -> p b c", c=128), tks, ['z8', 'z9'],
                   eng=('act' if g4 else 'dve'))
        scn = 0
        if gcut <= 2:
            return
        for sbk in range(2):
            for blk in range(4):
                bg = 4 * sbk + blk
                for h in range(4):
                    p2, hl = h // 2, h % 2
                    hs = slice(hl * 64, (hl + 1) * 64)
                    MM(ps[:, 4 + hl, p2 * 128:(p2 + 1) * 128], kT[hs, p2, bg * 128:(bg + 1) * 128], qT[hs, p2, bg * 128:(bg + 1) * 128],
                       True, True, kk_(p2) + kq(p2), ['scb%d' % hl])
                pbase = (bg % 2) * 4
                for hl in range(2):
                    TT(PT[:, pbase + hl:pbase + hl + 3:2, :], ps[:, 4 + hl, 0:256].rearrange("p (q c) -> p q c", c=128),
                       cmask.unsqueeze(1).broadcast_to([128, 2, 128]), ALU.mult, ['scb%d' % hl, 'cst'],
                       ['PT%d' % (pbase + hl), 'PT%d' % (pbase + hl + 2)])
                for h in range(4):
                    pl = pbase + h
                    MM(ps[:, h, blk * 128:(blk + 1) * 128], vtok[:, bg, h * 128:(h + 1) * 128], PT[:, pl, :], blk == 0, False,
                       ['vtok%d' % bg, 'PT%d' % pl], [PS(h)], sgc=True)
            if gcut <= 3:
                return
            for n in range(8):
                ng = 8 * sbk + n
                bg, hh = ng // 2, ng % 2
                ts_ = slice(hh * 64, (hh + 1) * 64)
                ssl = ng % 4
                kvs = ng % 2
                for p2 in range(2):
                    for hl in range(2):
                        h = 2 * p2 + hl
                        MM(ps[hl * 64:(hl + 1) * 64, 6 + kvs, p2 * 128:(p2 + 1) * 128],
                           ktok[ts_, bg, p2, hl * 64:(hl + 1) * 64], vtok[ts_, bg, h * 128:(h + 1) * 128], True, True,
                           ['z8', 'z9', 'vtok%d' % bg], ['kv%d' % kvs], last=(p2 == 1 and hl == 1))
                TT(Sbf[:, ssl, :, :], Sst[:], gsc[:, 2, :, ng].unsqueeze(2).broadcast_to([128, 2, 128]), ALU.mult,
                   ['Sst', 'gsc'], ['Sbf%d' % ssl])
                for h in range(4):
                    p2, hl = h // 2, h % 2
                    hs = slice(hl * 64, (hl + 1) * 64)
                    tcol = (4 * sbk) * 128 + n * 64
                    MM(ps[:, h, n * 64:(n + 1) * 64], Sbf[hs, ssl, p2, :], qT[hs, p2, tcol:tcol + 64], False, n == 7,
                       ['Sbf%d' % ssl] + kq(p2), [PS(h)], sgc=True)
                kvv = ps[:, 6 + kvs, 0:256].rearrange("p (q c) -> p q c", c=128)
                tkv = ZT[:, 4, 0:256].rearrange("p (q c) -> p q c", c=128)
                TT(tkv, kvv, gsc[:, 1, :, ng].unsqueeze(2).broadcast_to([128, 2, 128]), ALU.mult, ['kv%d' % kvs, 'gsc'], ['z4'])
                TT(Sst[:], Sst[:], gsc[:, 0, :, ng].unsqueeze(2).broadcast_to([128, 2, 128]), ALU.mult, ['Sst', 'gsc'], ['Sst'])
                TT(Sst[:], Sst[:], tkv, ALU.add, ['Sst', 'z4'], ['Sst'])
            if gcut <= 4:
                return
            lo = sbk * 512
            for h in range(4):
                oq = osq[:, h % 4, :]
                ACT(oq, ps[:, h, :], AF.Square, [PS(h)], ['rgT%d_%d' % (h // 2, h % 2)])
                MM(ps[:, 4 + h % 2, :], onesb[:], oq, True, True, ['onesb', 'rgT%d_%d' % (h // 2, h % 2)], ['scb%d' % (h % 2)])
                za, zb = (5, 6) if h % 2 == 0 else (7, 0)
                rsqrt_from_sum(ZT[:, za, :], ps[:, 4 + h % 2, :], 128, ['scb%d' % (h % 2), 'epst'], ['z%d' % za])
                STT(ZT[:, zb, :], ps[:, h, :], gains[:, 24:25], ZT[:, za, :], ALU.mult, ALU.mult, [PS(h), 'gains', 'z%d' % za], ['z%d' % zb])
                TT(A[:, h, lo:lo + 512], ZT[:, zb, :], gT[:, h, lo:lo + 512], ALU.mult, ['z%d' % zb, 'gT%d_%d' % (h, sbk)],
                   ['A%d_%d' % (h, sbk)])

    def outproj(l, hf):
        W = wout_d[l]
        cnt = 0

        def ld(i):
            P.dma('pool', 'wct%dL%d' % (i % 3, l), wct[i % 3][:], W[:, i * 128:(i + 1) * 128].rearrange("(k p) c -> p k c", p=128),
                  writes=['wct%d' % (i % 3)])
        ld(0)
        ld(1)
        for i in range(8):
            slot = i % 3
            if i + 2 < 8:
                ld(i + 2)
            for sb in range(2):
                b = cnt % 4
                cnt += 1
                t0 = hf * HALF + sb * 512
                for k in range(8):
                    MM(ps[:, b, :], wct[slot][:, k, :], A[:, k, sb * 512:(sb + 1) * 512], k == 0, k == 7,
                       ['wct%d' % slot, 'A%d_%d' % (k, sb)], [PS(b)], last=(k == 7))
                xk = 'x%d_%d' % (i, t0 // 512)
                TT(xT[:, i, t0:t0 + 512], xT[:, i, t0:t0 + 512], ps[:, b, :], ALU.add, [xk, PS(b)], [xk])

    def ffn_prefetch(l, hf):
        Wu = wup_d[l]
        P.dma('pool', 'upgv2L%d' % l, upgv[2][:, :, 0:128], Wu[:, 0:128].rearrange("(k p) c -> p k c", p=128), writes=['upgv2'])
        P.dma('pool', 'upgv2L%d' % l, upgv[2][:, :, 128:256], Wu[:, DFF:DFF + 128].rearrange("(k p) c -> p k c", p=128), writes=['upgv2'])

    def ffn(l, hf):
        Wu, Wd = wup_d[l], wdn_d[l]
        cnt = 0
        def ldu(j):
            sk_ = 'upgv%d' % ((j + 2) % 3)
            P.dma('pool', sk_ + 'L%d' % l, upgv[(j + 2) % 3][:, :, 0:128], Wu[:, j * 128:(j + 1) * 128].rearrange("(k p) c -> p k c", p=128), writes=[sk_])
            P.dma('pool', sk_ + 'L%d' % l, upgv[(j + 2) % 3][:, :, 128:256],
                  Wu[:, DFF + j * 128:DFF + (j + 1) * 128].rearrange("(k p) c -> p k c", p=128), writes=[sk_])

        def ldd(i):
            P.dma('pool', 'wdn%dL%d' % (i % 2, l), wdn[i % 2][:], Wd[:, i * 128:(i + 1) * 128].rearrange("(j p) c -> p j c", p=128),
                  writes=['wdn%d' % (i % 2)])
        ldu(1)
        pend = []
        import os
        FFN_TS_ENG = os.environ.get('FFN_TS_ENG', 'pool')
        for j in range(NJ):
            slot = (j + 2) % 3
            sk = 'upgv%d' % slot
            if j + 2 < NJ:
                ldu(j + 2)
            elif j + 2 == NJ:
                ldd(0)
            elif j + 1 == NJ:
                ldd(1)
            for sb in range(2):
                q = cnt % 3
                qp = (cnt - 1) % 3
                cnt += 1
                bu, bg_ = 2 * q, 2 * q + 1
                for k in range(8):
                    MM(ps[:, bu, :], upgv[slot][:, k, 0:128], A[:, k, sb * 512:(sb + 1) * 512], k == 0, k == 7,
                       [sk, 'A%d_%d' % (k, sb)], [PS(bu)], last=(k == 7))
                for k in range(8):
                    MM(ps[:, bg_, :], upgv[slot][:, k, 128:256], A[:, k, sb * 512:(sb + 1) * 512], k == 0, k == 7,
                       [sk, 'A%d_%d' % (k, sb)], [PS(bg_)], last=(k == 7))
                us, uk = upsb[q], 'us%d' % q
                if sb == 0:
                    CP(us[:, 0:2], carry[:, j, :], ['carry'], [uk], eng='act')
                else:
                    CP(us[:, 0:2], upsb[qp][:, 512:514], ['us%d' % qp], [uk], eng='act')
                CP(us[:, 2:514], ps[:, bu, :], [PS(bu)], [uk], eng='act')
                if sb == 1:
                    CP(carry[:, j, :], us[:, 512:514], [uk], ['carry'], eng='act')
                cw = lambda tap: pp[:, PP_MCW + j * 3 + tap:PP_MCW + j * 3 + tap + 1]
                cv, ck = fcv[q], 'cv%d' % q
                TS(cv[:], us[:, 2:514], cw(2), pp[:, PP_MCB + j:PP_MCB + j + 1], ALU.mult, ALU.add, [uk, 'pp'], [ck], eng=FFN_TS_ENG)
                STT(cv[:], us[:, 1:513], cw(1), cv[:], ALU.mult, ALU.add, [uk, 'pp', ck], [ck])
                STT(cv[:], us[:, 0:512], cw(0), cv[:], ALU.mult, ALU.add, [uk, 'pp', ck], [ck])
                if pend:
                    pend.pop()()
                ACT(cv[:], cv[:], AF.Gelu_apprx_tanh, [ck], [ck])
                pend.append(lambda j=j, sb=sb, cv=cv, ck=ck, bg_=bg_: TT(interm[:, j, sb * 512:(sb + 1) * 512], cv[:], ps[:, bg_, :],
                                                                      ALU.mult, [ck, PS(bg_)], ['im%d_%d' % (j, sb)]))
        while pend:
            pend.pop()()
        cnt = 0
        for i in range(8):
            slot = i % 2
            sk = 'wdn%d' % slot
            if i >= 1 and i + 1 < 8:
                ldd(i + 1)
            for sb in range(2):
                b = 6 + cnt % 2
                cnt += 1
                t0 = hf * HALF + sb * 512
                for j in range(NJ):
                    MM(ps[:, b, :], wdn[slot][:, j, :], interm[:, j, sb * 512:(sb + 1) * 512], j == 0, j == NJ - 1,
                       [sk, 'im%d_%d' % (j, sb)], [PS(b)], last=(j == NJ - 1))
                xk = 'x%d_%d' % (i, t0 // 512)
                TT(xT[:, i, t0:t0 + 512], xT[:, i, t0:t0 + 512], ps[:, b, :], ALU.add, [xk, PS(b)], [xk])

    def final_out():
        for blk in range(4):
            t0 = blk * 512
            sqv = sqbuf[:]
            for k in range(8):
                ACT(sqv[:, k, :], xT[:, k, t0:t0 + 512], AF.Square, ['x%d_%d' % (k, blk)], ['z%d' % (k // 2)])
            for k in range(8):
                MM(ps[:, 6, :], onesb[:], sqv[:, k, :], k == 0, k == 7, ['onesb', 'z%d' % (k // 2)], [PS(6)], last=(k == 7))
            rsqrt_from_sum(rstd_n, ps[:, 6, :], 1024, [PS(6), 'epst'], ['z4'])
            for k in range(8):
                STT(xo[:, k, :], xT[:, k, t0:t0 + 512], gains[:, 16 + k:17 + k], rstd_n, ALU.mult, ALU.mult,
                    ['x%d_%d' % (k, blk), 'gains', 'z4'], ['xo%d' % k])
            for t4 in range(4):
                tt = blk * 4 + t4
                s, sk = xs[tt % 2], 'xs%d' % (tt % 2)
                for kq_ in range(2):
                    b = (tt * 2 + kq_) % 4
                    for kk in range(4):
                        k = kq_ * 4 + kk
                        TR(ps[:, b, kk * 128:(kk + 1) * 128], xo[:, k, t4 * 128:(t4 + 1) * 128], ident, ['xo%d' % k, 'cst'], [PS(b)],
                           last=(kk == 3))
                    CP(s[:, kq_ * 512:(kq_ + 1) * 512], ps[:, b, :], [PS(b)], [sk], eng=('act' if kq_ == 0 else 'dve'))
                P.dma('sp', 'outd%d' % (tt % 2), out_d[tt * 128:(tt + 1) * 128, :], s[:], reads=[sk])

    import os
    stop_hf = int(os.environ.get('STOPHF', '0'))
    done = False
    for l in range(nlayers):
        layer_setup(l)
        hooks = [lambda q_=q_: load_x(range(4 * q_, 4 * q_ + 4)) for q_ in range(4)] if l == 0 else []
        s5_setup(l, hooks)
        P.barrier()
        MSET(rxhist[:], 0.0, ['rxhist'])
        for hf in range(2):
            rmsnorm_to_A(l, hf, 0)
            if stop == 'norm1' and hf == stop_hf:
                done = True
                break
            inproj(l, hf)
            if stop == 'inproj' and hf == stop_hf:
                done = True
                break
            rg_post(l, hf)
            if stop == 'rg' and hf == stop_hf:
                done = True
                break
            s5_mixer(l, hf)
            if stop == 's5' and hf == stop_hf:
                done = True
                break
            gla_mixer(l, hf)
            if stop == 'gla' and hf == stop_hf:
                done = True
                break
            outproj(l, hf)
            if stop == 'outproj' and hf == stop_hf:
                done = True
                break
            ffn_prefetch(l, hf)
            rmsnorm_to_A(l, hf, 8)
            P.barrier()
            ffn(l, hf)
            P.barrier()
            if stop == 'ffn' and hf == stop_hf:
                done = True
                break
        if done:
            break
    if not done:
        P.barrier()
        final_out()

    avail = dict(A=(A, [128, 8, HALF], BF16), qT=(qT, [128, 2, HALF], BF16), kT=(kT, [128, 2, HALF], BF16),
                 gT=(gT, [128, 4, HALF], BF16), uT=(uT, [128, 2, HALF], BF16), rx=(rx, [128, 2, 3 + HALF], F32),
                 rgT=(rgT, [128, 2, HALF], BF16), vtok=(vtok, [128, 8, 512], BF16), gkl=(gkl, [128, HALF], BF16),
                 xT=(xT, [128, 8, NT], F32), ZT=(ZT, [128, 10, 512], F32))
    for n in dbg_names:
        t, shp, dt = avail[n]
        d = nc.dram_tensor("dbg_" + n, shp, dt, kind="ExternalOutput").ap()
        P.barrier()
        P.dma('sp', 'dbg', d, t[:])
    P.emit()
    return nc


def host_prep(inputs):
    f = lambda n: np.ascontiguousarray(np.asarray(inputs[n], dtype=np.float32))
    cc = np.zeros((128, NCC), np.float32)
    cc[:, CC_ID:CC_ID + 128] = np.eye(128, dtype=np.float32)
    s = np.arange(128)[:, None]
    c = np.arange(128)[None, :]
    cc[:, CC_MASK:CC_MASK + 128] = ((s // 64 == c // 64) & (s <= c)).astype(np.float32)
    cc[:, CC_SEG:CC_SEG + HALF] = (np.arange(HALF) % 64 != 0).astype(np.float32)[None, :]
    cc[:, CC_CIDX:CC_CIDX + 256] = np.arange(256, dtype=np.float32)[None, :]
    cc[:, CC_IDX9:CC_IDX9 + 9] = np.arange(9, dtype=np.float32)[None, :]
    q = np.arange(128)
    glp = (q // 16) % 2
    cc[:, CC_MASKT + 0] = (glp == 0)
    cc[:, CC_MASKT + 1] = (glp == 1)
    pp = np.zeros((DEPTH, 128, NPP), np.float32)

    def tile_vec(v, nt):
        return v.reshape(DEPTH, nt, 128).transpose(0, 2, 1)

    pp[:, :, PP_AN:PP_AN + 8] = tile_vec(f('attn_norm'), 8)
    pp[:, :, PP_MN:PP_MN + 8] = tile_vec(f('mlp_norm'), 8)
    pp[:, :, PP_FN:PP_FN + 8] = np.broadcast_to(f('final_norm').reshape(8, 128).T[None], (DEPTH, 128, 8))
    pp[:, :, PP_BGK:PP_BGK + 2] = tile_vec(f('gla_b_gk'), 2)
    pp[:, :, PP_GN] = f('gla_norm')
    pp[:, :, PP_S5D:PP_S5D + 2] = tile_vec(f('s5_d'), 2)
    pp[:, :, PP_BGLU:PP_BGLU + 2] = tile_vec(f('s5_b_glu'), 2)
    pp[:, :, PP_S5N:PP_S5N + 2] = tile_vec(f('s5_norm'), 2)
    rcw = f('rg_conv_w').reshape(DEPTH, 4, 2, 128).transpose(0, 3, 2, 1)
    pp[:, :, PP_RCW:PP_RCW + 8] = rcw.reshape(DEPTH, 128, 8)
    pp[:, :, PP_RCB:PP_RCB + 2] = tile_vec(f('rg_conv_b'), 2)
    pp[:, :, PP_RBA:PP_RBA + 2] = tile_vec(f('rg_b_a'), 2)
    pp[:, :, PP_RBX:PP_RBX + 2] = tile_vec(f('rg_b_x'), 2)
    pp[:, :, PP_RLAM:PP_RLAM + 2] = tile_vec(f('rg_lambda'), 2)
    pp[:, :, PP_RN:PP_RN + 2] = tile_vec(f('rg_norm'), 2)
    mcw = f('mlp_conv_w').reshape(DEPTH, 3, NJ, 128).transpose(0, 3, 2, 1)
    pp[:, :, PP_MCW:PP_MCW + 66] = mcw.reshape(DEPTH, 128, 66)
    pp[:, :, PP_MCB:PP_MCB + NJ] = tile_vec(f('mlp_conv_b'), NJ)
    def SL(a):
        sh = a.shape
        a = a.reshape((DEPTH, 8, 2, 64) + sh[3:])
        a = np.moveaxis(a, 1, 3)
        return a.reshape((DEPTH, 128, 8) + sh[3:])
    lre, lim = f('s5_lambda_re'), f('s5_lambda_im')
    ldt = np.broadcast_to(f('s5_log_dt')[:, :, None], (DEPTH, 16, 64))
    pp[:, :, PP_LRE:PP_LRE + 8] = SL(lre)
    pp[:, :, PP_LIM:PP_LIM + 8] = SL(lim)
    pp[:, :, PP_LDT:PP_LDT + 8] = SL(np.ascontiguousarray(ldt))
    pp[:, :, PP_BRE:PP_BRE + 128] = SL(f('s5_b_re')).reshape(DEPTH, 128, 128)
    pp[:, :, PP_BIM:PP_BIM + 128] = SL(f('s5_b_im')).reshape(DEPTH, 128, 128)
    pp[:, :, PP_CRE:PP_CRE + 128] = SL(f('s5_c_re').transpose(0, 1, 3, 2)).reshape(DEPTH, 128, 128)
    pp[:, :, PP_CIM:PP_CIM + 128] = SL(f('s5_c_im').transpose(0, 1, 3, 2)).reshape(DEPTH, 128, 128)
    def TL(a, per_i):
        if not per_i:
            a = np.broadcast_to(a[..., None], a.shape + (16,))
        a = a.reshape(DEPTH, 2, 4, 2, 64, 16)
        a = a.transpose(0, 2, 3, 5, 1, 4)
        return a.reshape(DEPTH, 128, 128)
    pp[:, :, PP_LRET:PP_LRET + 128] = TL(lre, False)
    pp[:, :, PP_LIMT:PP_LIMT + 128] = TL(lim, False)
    pp[:, :, PP_LDTT:PP_LDTT + 128] = TL(np.ascontiguousarray(ldt), False)
    pp[:, :, PP_BRET:PP_BRET + 128] = TL(f('s5_b_re'), True)
    pp[:, :, PP_BIMT:PP_BIMT + 128] = TL(f('s5_b_im'), True)
    shared = dict(cc=cc, pp=pp, w_in=f('w_in'), w_out=f('w_out'), w_up=f('w_up'), w_down=f('w_down'),
                  wgk=f('gla_w_gk_up'), rwa=f('rg_w_a'), rwx=f('rg_w_x'), wglu=f('s5_w_glu'))
    return shared


_CACHE = {}


def kernel(**inputs):
    x = np.ascontiguousarray(np.asarray(inputs['x'], dtype=np.float32))
    shared = host_prep(inputs)
    if 'nc' not in _CACHE:
        _CACHE['nc'] = build_program()
    nc = _CACHE['nc']
    in_maps = [dict(shared, x=x[b]) for b in range(8)]
    res = run_bass_kernel_spmd(nc, in_maps, core_ids=list(range(8)))
    return np.stack([res.results[b]['out'] for b in range(8)], axis=0).astype(np.float32)
```

```python
import math
import numpy as np
import concourse.bass as bass
import concourse.mybir as mybir
from concourse.bass_utils import run_bass_kernel_spmd

F32 = mybir.dt.float32
BF16 = mybir.dt.bfloat16
I32 = mybir.dt.int32
ALU = mybir.AluOpType
AF = mybir.ActivationFunctionType

ENG = ['pe', 'act', 'dve', 'pool', 'sp']
DEPTH = 4
NT = 2048
HALF = 1024
DM = 1024
DFF = 2816
NJ = 22
EPS = 1e-6
PI = math.pi

CC_ID, CC_MASK, CC_SEG, CC_CIDX, CC_IDX9, CC_MASKT, NCC = 0, 128, 256, 1280, 1536, 1545, 1548
PP_AN, PP_MN, PP_FN, PP_BGK, PP_GN = 0, 8, 16, 24, 26
PP_S5D, PP_BGLU, PP_S5N = 27, 29, 31
PP_RCW, PP_RCB, PP_RBA, PP_RBX, PP_RLAM, PP_RN = 33, 41, 43, 45, 47, 49
PP_MCW, PP_MCB = 51, 117
PP_LRE, PP_LIM, PP_LDT = 139, 147, 155
PP_LRET, PP_LIMT, PP_LDTT = 163, 291, 419
PP_BRE, PP_BIM, PP_BRET, PP_BIMT, PP_CRE, PP_CIM, NPP = 547, 675, 803, 931, 1059, 1187, 1316


class Prog:
    def __init__(self, nc):
        self.nc = nc
        self.streams = {e: [] for e in ENG}
        self.cnt = {e: 0 for e in ENG}
        self.known = {e: {} for e in ENG}
        self.snap = {}
        self.lastw = {}
        self.readers = {}
        self.dcnt = {}
        self.pe_pending = []
        self.pe_reads = []
        self.pe_writes = []
        self.ekeys = set()

    def _need(self, E, ev, waits):
        s, v = ev
        if self.known[E].get(s, 0) >= v:
            return
        if '#' in s:
            e0, ep = s.split('#')
            for s2 in self.known[E]:
                if s2.startswith(e0 + '#') and int(s2.split('#')[1]) > int(ep):
                    return
        waits[s] = max(waits.get(s, 0), v)

    def _collect(self, E, reads, writes):
        waits = {}
        for k in reads:
            w = self.lastw.get(k)
            if w is not None:
                self._need(E, w, waits)
        for k in writes:
            w = self.lastw.get(k)
            if w is not None and (E != 'pe' or w[0].split('#')[0] != E):
                self._need(E, w, waits)
            for r in self.readers.get(k, ()):
                if E != 'pe' or r[0].split('#')[0] != E:
                    self._need(E, r, waits)
        kn = self.known[E]
        for s, v in waits.items():
            self.streams[E].append(('wait', s, v))
            if kn.get(s, 0) < v:
                kn[s] = v
            sn = self.snap.get((s, v))
            if sn:
                for s2, v2 in sn.items():
                    if kn.get(s2, 0) < v2:
                        kn[s2] = v2

    def _register(self, ev, reads, writes):
        for k in writes:
            self.lastw[k] = ev
            self.readers[k] = []
        for k in reads:
            self.readers.setdefault(k, []).append(ev)

    EPOCH = 1500

    def _tick(self, E):
        self.cnt[E] += 1
        ep = (self.cnt[E] - 1) // self.EPOCH
        key = '%s#%d' % (E, ep)
        self.ekeys.add(key)
        return (key, self.cnt[E] - ep * self.EPOCH)

    def op(self, E, fn, reads=(), writes=()):
        self._collect(E, reads, writes)
        ev = self._tick(E)
        self.snap[ev] = dict(self.known[E])
        self.streams[E].append(('op', fn, ev[0], 1))
        self._register(ev, reads, writes)

    def mm(self, fn, reads=(), writes=(), last=True):
        self.pe_pending.append(fn)
        self.pe_reads += list(reads)
        self.pe_writes += list(writes)
        if last:
            self._collect('pe', self.pe_reads, self.pe_writes)
            ev = self._tick('pe')
            self.snap[ev] = dict(self.known['pe'])
            for f in self.pe_pending[:-1]:
                self.streams['pe'].append(('op', f, None, 0))
            self.streams['pe'].append(('op', self.pe_pending[-1], ev[0], 1))
            self._register(ev, self.pe_reads, self.pe_writes)
            self.pe_pending, self.pe_reads, self.pe_writes = [], [], []

    def dma(self, Q, sem, out, in_, reads=(), writes=()):
        self._collect(Q, reads, writes)
        self.dcnt[sem] = self.dcnt.get(sem, 0) + 16
        ev = ('d:' + sem, self.dcnt[sem])
        self.snap[ev] = dict(self.known[Q])
        self.streams[Q].append(('op', lambda e: e.dma_start(out=out, in_=in_), 'd:' + sem, 16))
        self._register(ev, reads, writes)

    def barrier(self):
        assert not self.pe_pending
        evs = []
        for e in ENG:
            if self.cnt[e] > 0:
                ep = (self.cnt[e] - 1) // self.EPOCH
                evs.append(('%s#%d' % (e, ep), self.cnt[e] - ep * self.EPOCH))
        evs += [('d:' + s, v) for s, v in self.dcnt.items()]
        for E in ENG:
            waits = {}
            for ev in evs:
                if ev[0].split('#')[0] != E:
                    self._need(E, ev, waits)
            for s, v in waits.items():
                self.streams[E].append(('wait', s, v))
                self.known[E][s] = v

    def emit(self):
        import contextlib
        nc = self.nc
        self.barrier()
        names = sorted(self.ekeys) + ['d:' + s for s in self.dcnt]
        with contextlib.ExitStack() as st:
            sems = {}
            for i, n in enumerate(names):
                sems[n] = st.enter_context(nc.semaphore('s%d' % i))
            block = st.enter_context(nc.Block())

            def run(E):
                def body(eng):
                    for it in self.streams[E]:
                        if it[0] == 'wait':
                            eng.wait_ge(sems[it[1]], it[2])
                        else:
                            ins = it[1](eng)
                            if it[2] is not None:
                                ins.then_inc(sems[it[2]], it[3])
                return body

            block.tensor(run('pe'))
            block.scalar(run('act'))
            block.vector(run('dve'))
            block.gpsimd(run('pool'))
            block.sync(run('sp'))


def build_program(nlayers=DEPTH, stop=None, dbg_names=()):
    nc = bass.Bass("TRN2", target_bir_lowering=False)
    P = Prog(nc)

    def dram(name, shape, dt=F32, kind="ExternalInput"):
        return nc.dram_tensor(name, shape, dt, kind=kind).ap()

    x_d = dram("x", [NT, DM])
    out_d = dram("out", [NT, DM], kind="ExternalOutput")
    cc_d = dram("cc", [128, NCC])
    pp_d = dram("pp", [DEPTH, 128, NPP])
    win_d = dram("w_in", [DEPTH, DM, 2320])
    wout_d = dram("w_out", [DEPTH, DM, DM])
    wup_d = dram("w_up", [DEPTH, DM, 2 * DFF])
    wdn_d = dram("w_down", [DEPTH, DFF, DM])
    wgk_d = dram("wgk", [DEPTH, 16, 256])
    rwa_d = dram("rwa", [DEPTH, 4, 64, 64])
    rwx_d = dram("rwx", [DEPTH, 4, 64, 64])
    wglu_d = dram("wglu", [DEPTH, 256, 256])

    cur = [16512]
    SB_TOP = 229376
    uid = [0]

    def alloc(shape, dt, at=None):
        size = int(np.prod(shape[1:])) * (4 if dt in (F32, I32) else 2)
        off = ((cur[0] if at is None else at) + 31) // 32 * 32
        assert off + size <= SB_TOP, (shape, off, size)
        uid[0] += 1
        t = nc.alloc_sbuf_tensor_at("t%d" % uid[0], list(shape), dt, offset=off)
        if at is None:
            cur[0] = off + size
        return t, off + size

    def A_(shape, dt):
        return alloc(shape, dt)[0]

    xT = A_([128, 8, NT], F32)
    A = A_([128, 8, HALF], BF16)
    cst = A_([128, NCC], F32)
    pp = A_([128, NPP], F32)
    identb = A_([128, 128], BF16)
    onesb = A_([128, 128], BF16)
    segb = A_([128, HALF], BF16)
    gains = A_([128, 40], F32)
    rgc = A_([128, 8], F32)
    E5 = A_([128, 8, 9, 2, 32], BF16)
    WH = A_([128, 2, 8, 2, 128], BF16)
    KT = A_([128, 2, 8, 128], BF16)
    s5sm = A_([128, 24], F32)
    S5S = A_([128, 8, 2, 129], BF16)
    Gsl = A_([128, 8, 2], F32)
    wgk = A_([128, 256], BF16)
    rgw = A_([128, 2, 2, 64], BF16)
    wglu = A_([128, 2, 256], BF16)
    Sst = A_([128, 2, 128], F32)
    hlast = A_([128, 2], F32)
    rxhist = A_([128, 2, 3], F32)
    carry = A_([128, NJ, 2], F32)
    gsc = A_([128, 3, 2, 16], F32)
    wb0 = cur[0]
    upgv = [A_([128, 8, 256], BF16) for _ in range(2)]
    wdn = [A_([128, NJ, 128], BF16) for _ in range(2)]
    wb1 = cur[0]
    wct = []
    o = wb0
    for _ in range(3):
        t, o = alloc([128, 8, 128], BF16, at=o)
        wct.append(t)
    wv, o = alloc([128, 8, 512], BF16, at=o)
    yrg, o = alloc([128, 2, HALF], BF16, at=o)
    assert o <= wb1
    r0 = cur[0]
    qT = A_([128, 2, HALF], BF16)
    kT = A_([128, 2, HALF], BF16)
    gT = A_([128, 4, HALF], BF16)
    vtok = A_([128, 8, 512], BF16)
    gkl = A_([128, HALF], BF16)
    uT = A_([128, 2, HALF], BF16)
    rx = A_([128, 2, 3 + HALF], F32)
    rgT = A_([128, 2, HALF], BF16)
    ZT = A_([128, 10, 512], F32)
    r_end = cur[0]
    print("SBUF used", r_end, "of", SB_TOP, "R size", r_end - r0)
    o = r0
    interm, o = alloc([128, NJ, HALF], BF16, at=o)
    upsb = []
    for _ in range(3):
        t, o = alloc([128, 2 + 512], F32, at=o)
        upsb.append(t)
    fcv = []
    for _ in range(3):
        t, o = alloc([128, 512], F32, at=o)
        fcv.append(t)
    assert o <= r_end, (o, r_end)
    o = r0
    xs = []
    for _ in range(2):
        t, o = alloc([128, DM], F32, at=o)
        xs.append(t)
    xo, o = alloc([128, 8, 512], F32, at=o)
    assert o <= r_end
    o = r0
    st_ = {}

    def SA(name, shape, dt=F32):
        nonlocal o
        t, o2 = alloc(shape, dt, at=o)
        o = o2
        st_[name] = t
        return t

    for n in ['dt', 'ar', 'th', 'nr', 'ni', 'den', 'kr', 'ki', 'w8a', 'w8b']:
        SA(n, [128, 8])
    for n in ['tA', 'tB', 'sn9', 'cs9', 'Pr', 'Pi', 'w9a', 'w9b']:
        SA(n, [128, 8, 9])
    SA('w9i', [128, 8, 9], I32)
    SA('w8i', [128, 8], I32)
    for n in ['Bbr', 'Bbi', 'w16a', 'w16b']:
        SA(n, [128, 8, 16])
    SA('t1', [128, 8, 9, 16]); SA('t2', [128, 8, 9, 16])
    SA('Bw', [128, 8, 2, 128], BF16)
    for n in ['dtT', 'arT', 'thT', 'nrT', 'niT', 'denT', 'krT', 'kiT', 'wTa', 'wTb', 'BbrT', 'BbiT']:
        SA(n, [128, 2, 64])
    for n in ['angT', 'wkf', 'snT', 'csT', 'mgT', 'WHr', 'WHi', 'wk2']:
        SA(n, [128, 2, 8, 64])
    SA('wki', [128, 2, 8, 64], I32)
    assert o <= r_end, (o, r_end)

    ps = nc.alloc_psum_tensor("ps", [128, 8, 512], F32)
    psb = ps[:, 7, :].bitcast(BF16).rearrange("p (s c) -> p s c", c=128)

    def PS(b):
        return 'ps%d' % b

    def ACT(out, in_, func, r, w, bias=None, scale=None):
        kw = {}
        if bias is not None:
            kw['bias'] = bias
        if scale is not None:
            kw['scale'] = scale
        P.op('act', lambda e: e.activation(out=out, in_=in_, func=func, **kw), r, w)

    def TT(out, in0, in1, op, r, w, eng='dve'):
        P.op(eng, lambda e: e.tensor_tensor(out=out, in0=in0, in1=in1, op=op), r, w)

    def TS(out, in0, s1, s2, op0, op1, r, w, eng='dve'):
        if s2 is None:
            P.op(eng, lambda e: e.tensor_scalar(out=out, in0=in0, scalar1=s1, scalar2=None, op0=op0), r, w)
        else:
            P.op(eng, lambda e: e.tensor_scalar(out=out, in0=in0, scalar1=s1, scalar2=s2, op0=op0, op1=op1), r, w)

    def STT(out, in0, scalar, in1, op0, op1, r, w, eng='dve'):
        P.op(eng, lambda e: e.scalar_tensor_tensor(out=out, in0=in0, scalar=scalar, in1=in1, op0=op0, op1=op1), r, w)

    def CP(out, in_, r, w, eng='dve'):
        if eng == 'act':
            P.op('act', lambda e: e.copy(out=out, in_=in_), r, w)
        else:
            P.op(eng, lambda e: e.tensor_copy(out=out, in_=in_), r, w)

    def MSET(ap, val, w, eng='dve'):
        P.op(eng, lambda e: e.memset(ap, val), (), w)

    def SCAN(out, d0, d1, init, r, w):
        P.op('dve', lambda e: e.tensor_tensor_scan(out=out, data0=d0, data1=d1, initial=init, op0=ALU.mult, op1=ALU.add), r, w)

    def MM(out, lhsT, rhs, start, stop, r=(), w=(), last=True, tp=None, sgc=False):
        kw = {}
        if tp is not None:
            kw['tile_position'] = tp
        if sgc:
            kw['skip_group_check'] = True
        P.mm(lambda e: e.matmul(out, lhsT=lhsT, rhs=rhs, start=start, stop=stop, **kw), r, w, last)

    def TR(out, in_, ident, r, w, last=True):
        P.mm(lambda e: e.transpose(out, in_, ident), r, w, last)

    def bc(ap, shape):
        return ap.broadcast_to(list(shape))

    def rsqrt_from_sum(out, ss_ps, n, r, w):
        ACT(out, ss_ps, AF.Ln, r, w, bias=epsb[n])
        ACT(out, out, AF.Exp, w, w, scale=-0.5)

    dbg = {}

    P.dma('sp', 'cst', cst[:], cc_d, writes=['cst'])
    P.dma('pool', 'idb', identb[:], cc_d[:, CC_ID:CC_ID + 128], writes=['identb'])
    P.dma('pool', 'sgb', segb[:], cc_d[:, CC_SEG:CC_SEG + HALF], writes=['segb'])
    MSET(onesb[:], 1.0, ['onesb'])
    MSET(E5[:], 0.0, ['E5'], eng='pool')
    epst = A_([128, 4], F32)
    MSET(epst[:, 0:1], 128 * EPS, ['epst'])
    MSET(epst[:, 1:2], 256 * EPS, ['epst'])
    MSET(epst[:, 2:3], 1024 * EPS, ['epst'])
    epsb = {128: epst[:, 0:1], 256: epst[:, 1:2], 1024: epst[:, 2:3]}
    upgv.append(A_([128, 8, 256], BF16))
    hpi = A_([128, 1], F32)
    lneighth = A_([128, 1], F32)
    MSET(hpi[:], PI / 2, ['hpi'])
    MSET(lneighth[:], math.log(0.125), ['hpi'])
    ident = cst[:, CC_ID:CC_ID + 128]
    cmask = cst[:, CC_MASK:CC_MASK + 128]

    xs_in = [alloc([128, DM], F32, at=r_end - 10 * 2048 + i_ * 4096)[0] for i_ in range(2)]

    def load_x(tts):
        for tt in tts:
            s = xs_in[tt % 2]
            sk = 'xsi%d' % (tt % 2)
            P.dma('sp', sk, s[:], x_d[tt * 128:(tt + 1) * 128, :], writes=[sk])
            for kq in range(2):
                b = 4 + (tt * 2 + kq) % 4
                for kk in range(4):
                    k = kq * 4 + kk
                    TR(ps[:, b, kk * 128:(kk + 1) * 128], s[:, k * 128:(k + 1) * 128], ident,
                       [sk, 'cst'], [PS(b)], last=(kk == 3))
                CP(xT[:, kq * 4:(kq + 1) * 4, tt * 128:(tt + 1) * 128],
                   ps[:, b, :].rearrange("p (k t) -> p k t", k=4),
                   [PS(b)], ['x%d_%d' % (kq * 4 + kk_, tt // 4) for kk_ in range(4)], eng=('act' if kq == 0 else 'dve'))

    def rmsnorm_to_A(l, hf, gcol):
        for sb in range(2):
            t0 = hf * HALF + sb * 512
            sqv = sqbuf[:]
            for k in range(8):
                ACT(sqv[:, k, :], xT[:, k, t0:t0 + 512], AF.Square, ['x%d_%d' % (k, t0 // 512)], ['z%d' % (k // 2)])
            for k in range(8):
                MM(ps[:, 6, :], onesb[:], sqv[:, k, :], k == 0, k == 7, ['onesb', 'z%d' % (k // 2)], [PS(6)], last=(k == 7))
            rsqrt_from_sum(rstd_n, ps[:, 6, :], 1024, [PS(6), 'epst'], ['z4'])
            for k in range(8):
                STT(A[:, k, sb * 512:(sb + 1) * 512], xT[:, k, t0:t0 + 512], gains[:, gcol + k:gcol + k + 1],
                    rstd_n, ALU.mult, ALU.mult, ['x%d_%d' % (k, t0 // 512), 'gains', 'z4'], ['A%d_%d' % (k, sb)])

    sqbuf_t, _ = alloc([128, 8, 512], BF16, at=r_end - 10 * 2048)
    sqbuf = sqbuf_t
    rstd_n = ZT[:, 4, :]
    ktok_t, _ = alloc([128, 8, 2, 128], BF16, at=r_end - 2 * 2048)

    def layer_setup(l):
        P.dma('sp', 'pp', pp[:], pp_d[l], writes=['pp'])
        P.dma('pool', 'wgk', wgk[0:16, :], wgk_d[l], writes=['wgk'])
        for gi, wd in enumerate((rwa_d, rwx_d)):
            src = wd[l].rearrange("(k hl) i j -> hl i k j", hl=2)
            for hl in range(2):
                P.dma('pool', 'rgw', rgw[hl * 64:(hl + 1) * 64, gi, :, :], src[hl], writes=['rgw'])
        P.dma('pool', 'wglu', wglu[:], wglu_d[l].rearrange("(k p) c -> p k c", p=128), writes=['wglu'])
        TS(gains[:, 0:24], pp[:, PP_AN:PP_AN + 24], 32.0, None, ALU.mult, None, ['pp'], ['gains'])
        TS(gains[:, 24:25], pp[:, PP_GN:PP_GN + 1], math.sqrt(128.0), None, ALU.mult, None, ['pp'], ['gains'])
        TS(gains[:, 25:27], pp[:, PP_S5N:PP_S5N + 2], 16.0, None, ALU.mult, None, ['pp'], ['gains'])
        TS(gains[:, 27:29], pp[:, PP_RN:PP_RN + 2], 16.0, None, ALU.mult, None, ['pp'], ['gains'])
        ACT(rgc[:, 0:2], pp[:, PP_RLAM:PP_RLAM + 2], AF.Exp, ['pp'], ['rgc'], scale=-1.0)
        ACT(rgc[:, 0:2], rgc[:, 0:2], AF.Ln, ['rgc'], ['rgc'], bias=1.0)
        TS(rgc[:, 2:4], rgc[:, 0:2], -16.0, None, ALU.mult, None, ['rgc'], ['rgc'])
        TS(rgc[:, 0:2], rgc[:, 0:2], -8.0, None, ALU.mult, None, ['rgc'], ['rgc'])
        TS(rgc[:, 4:6], pp[:, PP_BGK:PP_BGK + 2], -1.0, None, ALU.mult, None, ['pp'], ['rgc'])
        MSET(Sst[:], 0.0, ['Sst'])
        MSET(hlast[:], 0.0, ['hlast'])
        MSET(carry[:], 0.0, ['carry'])
        MSET(S5S[:, :, :, 0:1], 0.0, ['S5S'])
        MSET(Gsl[:], 0.0, ['Gsl'])

    def inproj(l, hf):
        W = win_d[l]

        def load_ct(slot, c0, ncols=128):
            P.dma('pool', 'wct%dL%d' % (slot, l), wct[slot][:, :, 0:ncols],
                  W[:, c0:c0 + ncols].rearrange("(k p) c -> p k c", p=128), writes=['wct%d' % slot])

        tiles = []
        for t in range(2):
            tiles.append((lambda sb, t=t: rx[:, t, 3 + sb * 512:3 + (sb + 1) * 512], 1808 + t * 128, 'rx%d' % t))
        for t in range(2):
            tiles.append((lambda sb, t=t: rgT[:, t, sb * 512:(sb + 1) * 512], 2064 + t * 128, 'rgT%d' % t))
        for t in range(2):
            tiles.append((lambda sb, t=t: qT[:, t, sb * 512:(sb + 1) * 512], 0 + t * 128, 'qT%d' % t))
        for t in range(2):
            tiles.append((lambda sb, t=t: kT[:, t, sb * 512:(sb + 1) * 512], 256 + t * 128, 'kT%d' % t))
        for t in range(4):
            tiles.append((lambda sb, t=t: gT[:, t, sb * 512:(sb + 1) * 512], 1024 + t * 128, 'gT%d' % t))
        for t in range(2):
            tiles.append((lambda sb, t=t: uT[:, t, sb * 512:(sb + 1) * 512], 1552 + t * 128, 'uT%d' % t))
        cnt = 0
        P.dma('pool', 'wvL%d' % l, wv[:], W[:, 512:1024].rearrange("(k p) c -> p k c", p=128), writes=['wv'])
        load_ct(0, tiles[0][1])
        load_ct(1, tiles[1][1])
        for i, (dst, c0, key) in enumerate(tiles):
            slot = i % 3
            if i + 2 < len(tiles):
                load_ct((i + 2) % 3, tiles[i + 2][1])
            elif i + 2 == len(tiles):
                load_ct((i + 2) % 3, 1536, 16)
            for sb in range(2):
                b = cnt % 4
                for k in range(8):
                    MM(ps[:, b, :], wct[slot][:, k, :], A[:, k, sb * 512:(sb + 1) * 512], k == 0, k == 7,
                       ['wct%d' % slot, 'A%d_%d' % (k, sb)], [PS(b)], last=(k == 7))
                CP(dst(sb), ps[:, b, :], [PS(b)], ['%s_%d' % (key, sb)], eng=('act' if cnt % 2 == 0 else 'dve'))
                cnt += 1
            if i == 3:
                rg_pre(l, hf)
                chain = rg_chain(l, hf)
            elif i > 3:
                next(chain, None)
        gs = len(tiles) % 3
        for sb in range(2):
            b = cnt % 4
            for k in range(8):
                MM(ps[0:16, b, :], wct[gs][:, k, 0:16], A[:, k, sb * 512:(sb + 1) * 512], k == 0, k == 7,
                   ['wct%d' % gs, 'A%d_%d' % (k, sb)], [PS(b)], last=(k == 7))
            CP(gkl[0:16, sb * 512:(sb + 1) * 512], ps[0:16, b, :], [PS(b)], ['gkl_%d' % sb], eng='act')
            cnt += 1
            next(chain, None)
        for tb in range(8):
            b = cnt % 4
            sb = tb // 4
            for k in range(8):
                MM(ps[:, b, :], A[:, k, tb * 128:(tb + 1) * 128], wv[:, k, :], k == 0, k == 7,
                   ['wv', 'A%d_%d' % (k, sb)], [PS(b)], last=(k == 7))
            CP(vtok[:, tb, :], ps[:, b, :], [PS(b)], ['vtok%d' % tb], eng=('act' if cnt % 2 == 0 else 'dve'))
            cnt += 1
            next(chain, None)
        for _ in chain:
            pass

    def rg_pre(l, hf):
        for k in range(2):
            CP(rx[:, k, 0:3], rxhist[:, k, :], ['rxhist'], ['rxh'], eng='pool')
        for k in range(2):
            for sb in range(2):
                gk_ = 'rgT%d_%d' % (k, sb)
                ACT(rgT[:, k, sb * 512:(sb + 1) * 512], rgT[:, k, sb * 512:(sb + 1) * 512], AF.Gelu_apprx_tanh, [gk_], [gk_])

    def rg_iter(l, hf, sb, k):
        z = lambda i: ZT[:, i, :]
        lo = sb * 512
        xc, rb, ib, ab, hb = z(0), z(1), z(2), z(3), z(4)
        xcb = ZT[:, 5, 0:256].bitcast(BF16)
        rxk = ['rx%d_%d' % (k, sb), 'rxh'] + (['rx%d_%d' % (k, sb - 1)] if sb else [])
        w = lambda tap: pp[:, PP_RCW + k * 4 + tap:PP_RCW + k * 4 + tap + 1]
        TS(xc, rx[:, k, 3 + lo:3 + lo + 512], w(3), pp[:, PP_RCB + k:PP_RCB + k + 1], ALU.mult, ALU.add,
           rxk + ['pp'], ['z0'])
        for tap in range(3):
            STT(xc, rx[:, k, tap + lo:tap + lo + 512], w(tap), xc, ALU.mult, ALU.add, rxk + ['pp', 'z0'], ['z0'])
        CP(xcb, xc, ['z0'], ['z5'], eng='act')
        yield
        for gi in range(2):
            b = 4 + gi
            for hl in range(2):
                MM(ps[hl * 64:(hl + 1) * 64, b, :], rgw[hl * 64:(hl + 1) * 64, gi, k, :],
                   xcb[hl * 64:(hl + 1) * 64, :], True, True, ['rgw', 'z5'], [PS(b)], last=(hl == 1))
        yield
        for gi, (dst, bcol) in enumerate(((rb, PP_RBA), (ib, PP_RBX))):
            b = 4 + gi
            ACT(dst, ps[:, b, :], AF.Sigmoid, [PS(b), 'pp'], ['z%d' % (1 + gi)], bias=pp[:, bcol + k:bcol + k + 1])
        ACT(ab, rb, AF.Exp, ['z1', 'rgc'], ['z3'], scale=rgc[:, k:k + 1])
        ACT(rb, rb, AF.Exp, ['z1', 'rgc'], ['z1'], scale=rgc[:, 2 + k:3 + k])
        TS(rb, rb, 0.999999, None, ALU.min, None, ['z1'], ['z1'])
        ACT(rb, rb, AF.Ln, ['z1'], ['z1'], scale=-1.0, bias=1.0)
        ACT(rb, rb, AF.Exp, ['z1'], ['z1'], scale=0.5)
        yield
        TT(ib, ib, xc, ALU.mult, ['z2', 'z0'], ['z2'])
        TT(ib, ib, rb, ALU.mult, ['z2', 'z1'], ['z2'])
        SCAN(hb, ab, ib, hlast[:, k:k + 1], ['z3', 'z2', 'hlast'], ['z4'])
        CP(hlast[:, k:k + 1], hb[:, 511:512], ['z4'], ['hlast'], eng='pool')
        TT(ZT[:, 6 + k, :], hb, rgT[:, k, lo:lo + 512], ALU.mult, ['z4', 'rgT%d_%d' % (k, sb)], ['z%d' % (6 + k)])
        ACT(ZT[:, 8, k * 256:(k + 1) * 256].bitcast(BF16), ZT[:, 6 + k, :], AF.Square, ['z%d' % (6 + k)], ['z8'])
        yield

    def rg_chain(l, hf):
        for sb in range(2):
            for k in range(2):
                yield from rg_iter(l, hf, sb, k)
            rg_norm(l, hf, sb)
            yield

    def rg_norm(l, hf, sb):
        lo = sb * 512
        sqv = ZT[:, 8, :].bitcast(BF16)
        for k in range(2):
            MM(ps[:, 6, :], onesb[:], sqv[:, k * 512:(k + 1) * 512], k == 0, k == 1, ['onesb', 'z8'], [PS(6)], last=(k == 1))
        rsqrt_from_sum(ZT[:, 9, :], ps[:, 6, :], 256, [PS(6), 'epst'], ['z9'])
        for k in range(2):
            STT(yrg[:, k, lo:lo + 512], ZT[:, 6 + k, :], gains[:, 27 + k:28 + k], ZT[:, 9, :], ALU.mult, ALU.mult,
                ['z%d' % (6 + k), 'gains', 'z9'], ['yrg%d_%d' % (k, sb)])

    def rg_post(l, hf):
        for k in range(2):
            CP(rxhist[:, k, :], rx[:, k, HALF:HALF + 3], ['rx%d_1' % k], ['rxhist'], eng='pool')
            for sb in range(2):
                CP(A[:, 6 + k, sb * 512:(sb + 1) * 512], yrg[:, k, sb * 512:(sb + 1) * 512], ['yrg%d_%d' % (k, sb)],
                   ['A%d_%d' % (6 + k, sb)], eng='pool')


    TWO_PI = 2.0 * PI

    def bcl(ap, n):
        return ap.unsqueeze(2).broadcast_to([ap.shape[0], ap.shape[1], n])

    def sincos(X, sn, cs, wf, wi, r, w):
        TS(wi, X, 1.0 / TWO_PI, None, ALU.mult, None, r, w)
        CP(wf, wi, w, w)
        STT(wf, wf, -TWO_PI, X, ALU.mult, ALU.add, r + w, w)
        TS(wf, wf, -3.14159, 3.14159, ALU.max, ALU.min, w, w)
        ACT(sn, wf, AF.Sin, w, w)
        STT(wf, wf, -1.0, wf, ALU.mult, ALU.max, w, w)
        ACT(cs, wf, AF.Sin, w, w, scale=-1.0, bias=hpi[:])

    def kappa(P1r, P1i, lre, lim, nr, den, kr, ki, wa, r, w):
        TS(nr, P1r, -1.0, None, ALU.add, None, r, w)
        TT(den, lre, lre, ALU.mult, r, w)
        TT(wa, lim, lim, ALU.mult, r, w)
        TT(den, den, wa, ALU.add, w, w)
        P.op('dve', lambda e: e.reciprocal(out=den, in_=den), w, w)
        TT(kr, nr, lre, ALU.mult, r + w, w)
        TT(wa, P1i, lim, ALU.mult, r + w, w)
        TT(kr, kr, wa, ALU.add, w, w)
        TT(kr, kr, den, ALU.mult, w, w)
        TT(ki, P1i, lre, ALU.mult, r + w, w)
        TT(wa, nr, lim, ALU.mult, r + w, w)
        TT(ki, ki, wa, ALU.subtract, w, w)
        TT(ki, ki, den, ALU.mult, w, w)

    def s5_setup(l, hooks=()):
        hooks = list(hooks)

        def hook():
            if hooks:
                hooks.pop(0)()
        T = st_
        hook()
        r_ = ['pp', 'cst']
        w_ = ['s5t']
        idx9 = cst[:, CC_IDX9:CC_IDX9 + 9]
        idx8 = cst[:, CC_IDX9:CC_IDX9 + 8]
        lre, lim = pp[:, PP_LRE:PP_LRE + 8], pp[:, PP_LIM:PP_LIM + 8]
        ACT(T['dt'][:], pp[:, PP_LDT:PP_LDT + 8], AF.Exp, r_, w_)
        TT(T['ar'][:], lre, T['dt'][:], ALU.mult, r_ + w_, w_)
        TT(T['th'][:], lim, T['dt'][:], ALU.mult, r_ + w_, w_)
        i9b = idx9.unsqueeze(1).broadcast_to([128, 8, 9])
        TT(T['tA'][:], bcl(T['ar'][:], 9), i9b, ALU.mult, r_ + w_, w_)
        TT(T['tB'][:], bcl(T['th'][:], 9), i9b, ALU.mult, r_ + w_, w_)
        ACT(T['tA'][:], T['tA'][:], AF.Exp, w_, w_)
        sincos(T['tB'][:], T['sn9'][:], T['cs9'][:], T['w9a'][:], T['w9i'][:], w_, w_)
        TT(T['Pr'][:], T['tA'][:], T['cs9'][:], ALU.mult, w_, w_)
        TT(T['Pi'][:], T['tA'][:], T['sn9'][:], ALU.mult, w_, w_)
        hook()
        kappa(T['Pr'][:, :, 1], T['Pi'][:, :, 1], lre, lim, T['nr'][:], T['den'][:], T['kr'][:], T['ki'][:], T['w8a'][:], r_ + w_, w_)
        Bre = pp[:, PP_BRE:PP_BRE + 128].rearrange("p (a i) -> p a i", i=16)
        Bim = pp[:, PP_BIM:PP_BIM + 128].rearrange("p (a i) -> p a i", i=16)
        krb, kib = bcl(T['kr'][:], 16), bcl(T['ki'][:], 16)
        TT(T['Bbr'][:], krb, Bre, ALU.mult, r_ + w_, w_)
        TT(T['w16a'][:], kib, Bim, ALU.mult, r_ + w_, w_)
        TT(T['Bbr'][:], T['Bbr'][:], T['w16a'][:], ALU.subtract, w_, w_)
        TT(T['Bbi'][:], krb, Bim, ALU.mult, r_ + w_, w_)
        TT(T['w16a'][:], kib, Bre, ALU.mult, r_ + w_, w_)
        TT(T['Bbi'][:], T['Bbi'][:], T['w16a'][:], ALU.add, w_, w_)
        MSET(T['Bw'][:], 0.0, w_, eng='pool')
        hook()
        for gl in range(2):
            for ri, Bb in enumerate((T['Bbr'], T['Bbi'])):
                for k in range(2):
                    dst = bass.AP(T['Bw'], (gl * 64) * 2048 + (4 * k) * 256 + ri * 128 + gl * 16,
                                  [[2048, 64], [256 + 32, 4], [1, 16]])
                    CP(dst, Bb[gl * 64:(gl + 1) * 64, 4 * k:4 * k + 4, :], w_, w_)
        Cre = pp[:, PP_CRE:PP_CRE + 128].rearrange("p (a j) -> p a j", j=16)
        Cim = pp[:, PP_CIM:PP_CIM + 128].rearrange("p (a j) -> p a j", j=16)
        Cb = lambda C: C.unsqueeze(2).broadcast_to([128, 8, 9, 16])
        Pb = lambda Pt: Pt[:].unsqueeze(3).broadcast_to([128, 8, 9, 16])
        TT(T['t1'][:], Cb(Cre), Pb(T['Pr']), ALU.mult, r_ + w_, w_)
        TT(T['t2'][:], Cb(Cim), Pb(T['Pi']), ALU.mult, r_ + w_, w_)
        TT(T['t1'][:], T['t1'][:], T['t2'][:], ALU.subtract, w_, w_)
        for gl in range(2):
            CP(E5[gl * 64:(gl + 1) * 64, :, :, 0, gl * 16:(gl + 1) * 16], T['t1'][gl * 64:(gl + 1) * 64], w_, ['E5'])
        TT(T['t1'][:], Cb(Cre), Pb(T['Pi']), ALU.mult, r_ + w_, w_)
        TT(T['t2'][:], Cb(Cim), Pb(T['Pr']), ALU.mult, r_ + w_, w_)
        TT(T['t1'][:], T['t1'][:], T['t2'][:], ALU.add, w_, w_)
        for gl in range(2):
            TS(E5[gl * 64:(gl + 1) * 64, :, :, 1, gl * 16:(gl + 1) * 16], T['t1'][gl * 64:(gl + 1) * 64], -1.0, None,
               ALU.mult, None, w_, ['E5'])
        hook()
        while hooks:
            hook()
        for k in range(2):
            for a in range(4):
                pr = 4 * k + a
                for tau in range(8):
                    out = ps[:, 2 * k + tau // 4, (tau % 4) * 128 + 32 * a:(tau % 4) * 128 + 32 * a + 32]
                    MM(out, T['Bw'][:, pr, 0, :], E5[:, pr, tau, 0, :], True, False, ['s5t', 'E5'],
                       [PS(2 * k + tau // 4)], last=False, sgc=True)
                    MM(out, T['Bw'][:, pr, 1, :], E5[:, pr, tau, 1, :], False, True, [], [], last=(tau == 7), sgc=True)
            CP(KT[:, k, :, :], ps[:, 2 * k:2 * k + 2, :].rearrange("p b (t c) -> p (b t) c", c=128),
               [PS(2 * k), PS(2 * k + 1)], ['KT'], eng='act')
        qo = lambda ap: ap.rearrange("p (k a) -> p a k", k=2)
        ACT(s5sm[:, 0:8].rearrange("p (a k) -> p a k", k=2), qo(T['ar'][:]), AF.Exp, w_, ['s5sm'], scale=8.0)
        TS(T['w8i'][:], T['th'][:], 8.0 / TWO_PI, None, ALU.mult, None, w_, w_)
        CP(T['w8a'][:], T['w8i'][:], w_, w_)
        TS(T['w8b'][:], T['th'][:], 8.0, None, ALU.mult, None, w_, w_)
        STT(s5sm[:, 8:16].rearrange("p (a k) -> p a k", k=2), qo(T['w8a'][:]), -TWO_PI, qo(T['w8b'][:]), ALU.mult, ALU.add, w_, ['s5sm'])
        wT_ = ['s5tT']
        v3 = lambda c0: pp[:, c0:c0 + 128].rearrange("p (k q) -> p k q", q=64)
        lreT, limT = v3(PP_LRET), v3(PP_LIMT)
        ACT(T['dtT'][:], v3(PP_LDTT), AF.Exp, r_, wT_)
        TT(T['arT'][:], lreT, T['dtT'][:], ALU.mult, r_ + wT_, wT_)
        TT(T['thT'][:], limT, T['dtT'][:], ALU.mult, r_ + wT_, wT_)
        i8b = idx8.unsqueeze(1).unsqueeze(3).broadcast_to([128, 2, 8, 64])
        eb = lambda t: t.unsqueeze(2).broadcast_to([128, 2, 8, 64])
        TT(T['angT'][:], eb(T['thT'][:]), i8b, ALU.mult, r_ + wT_, wT_)
        TT(T['mgT'][:], eb(T['arT'][:]), i8b, ALU.mult, r_ + wT_, wT_)
        ACT(T['mgT'][:], T['mgT'][:], AF.Exp, wT_, wT_)
        sincos(T['angT'][:], T['snT'][:], T['csT'][:], T['wkf'][:], T['wki'][:], wT_, wT_)
        TT(T['csT'][:], T['csT'][:], T['mgT'][:], ALU.mult, wT_, wT_)
        TT(T['snT'][:], T['snT'][:], T['mgT'][:], ALU.mult, wT_, wT_)
        kappa(T['csT'][:, :, 1, :], T['snT'][:, :, 1, :], lreT, limT, T['nrT'][:], T['denT'][:], T['krT'][:], T['kiT'][:],
              T['wTa'][:], r_ + wT_, wT_)
        BreT, BimT = v3(PP_BRET), v3(PP_BIMT)
        TT(T['BbrT'][:], T['krT'][:], BreT, ALU.mult, r_ + wT_, wT_)
        TT(T['wTa'][:], T['kiT'][:], BimT, ALU.mult, r_ + wT_, wT_)
        TT(T['BbrT'][:], T['BbrT'][:], T['wTa'][:], ALU.subtract, wT_, wT_)
        TT(T['BbiT'][:], T['krT'][:], BimT, ALU.mult, r_ + wT_, wT_)
        TT(T['wTa'][:], T['kiT'][:], BreT, ALU.mult, r_ + wT_, wT_)
        TT(T['BbiT'][:], T['BbiT'][:], T['wTa'][:], ALU.add, wT_, wT_)
        TT(T['WHr'][:], T['csT'][:], eb(T['BbrT'][:]), ALU.mult, wT_, wT_)
        TT(T['wk2'][:], T['snT'][:], eb(T['BbiT'][:]), ALU.mult, wT_, wT_)
        TT(T['WHr'][:], T['WHr'][:], T['wk2'][:], ALU.subtract, wT_, wT_)
        TT(T['WHi'][:], T['csT'][:], eb(T['BbiT'][:]), ALU.mult, wT_, wT_)
        TT(T['wk2'][:], T['snT'][:], eb(T['BbrT'][:]), ALU.mult, wT_, wT_)
        TT(T['WHi'][:], T['WHi'][:], T['wk2'][:], ALU.add, wT_, wT_)
        mT = cst[:, CC_MASKT:CC_MASKT + 2].unsqueeze(1).unsqueeze(3).broadcast_to([128, 8, 2, 64])
        for k in range(2):
            for ri, Wx in enumerate((T['WHr'], T['WHi'])):
                out = WH[:, k, :, ri, :].rearrange("p e (g q) -> p e g q", g=2)
                TT(out, Wx[:, k, :, :].unsqueeze(2).broadcast_to([128, 8, 2, 64]), mT, ALU.mult, r_ + wT_, ['WH'])

    def s5_mixer(l, hf):
        c0 = hf * 128
        v8 = lambda ap: ap.rearrange("p (a c) -> p a c", c=128)
        Tc = ZT[:, 0:2, :].rearrange("p a (b c) -> p (a b) c", c=128)
        Ts = ZT[:, 2:4, :].rearrange("p a (b c) -> p (a b) c", c=128)
        Gr = ZT[:, 4:6, :].rearrange("p a (b c) -> p (a b) c", c=128)
        Gi = ZT[:, 6:8, :].rearrange("p a (b c) -> p (a b) c", c=128)
        Gsi = ZT[:, 8:10, :].rearrange("p a (b c) -> p (a b) c", c=128)
        tmp = v8(rx[:, 0, 3:3 + HALF])
        Gsr = v8(rx[:, 1, 3:3 + HALF])
        wi = rgT[:].rearrange("p a t -> p (a t)").bitcast(I32).rearrange("p (a c) -> p a c", c=128)
        kTc, kTs, kGr, kGi, kGsi = ['z0', 'z1'], ['z2', 'z3'], ['z4', 'z5'], ['z6', 'z7'], ['z8', 'z9']
        ktmp, kGsr = ['rx0_0', 'rx0_1'], ['rx1_0', 'rx1_1']
        kwi = ['rgT0_0', 'rgT0_1', 'rgT1_0', 'rgT1_1']
        cidx = cst[:, CC_CIDX + c0:CC_CIDX + c0 + 128].unsqueeze(1).broadcast_to([128, 8, 128])
        TT(Tc, bcl(s5sm[:, 8:16], 128), cidx, ALU.mult, ['s5sm', 'cst'], kTc)
        TS(wi, Tc, 1.0 / TWO_PI, None, ALU.mult, None, kTc, kwi)
        CP(Gr, wi, kwi, kGr)
        STT(Gr, Gr, -TWO_PI, Tc, ALU.mult, ALU.add, kGr + kTc, kGr)
        TS(Gr, Gr, -3.14159, 3.14159, ALU.max, ALU.min, kGr, kGr)
        ACT(Ts, Gr, AF.Sin, kGr, kTs)
        STT(Gr, Gr, -1.0, Gr, ALU.mult, ALU.max, kGr, kGr)
        ACT(Tc, Gr, AF.Sin, kGr, kTc, scale=-1.0, bias=hpi[:])
        for k in range(2):
            uv = uT[:, k, :].rearrange("p (c s) -> p s c", s=8)
            for a in range(4):
                pr = 4 * k + a
                for ri in range(2):
                    off = k * 256 + ri * 128
                    for e in range(8):
                        MM(ps[:, a, off:off + 128], WH[32 * a:32 * a + 32, k, e, ri, :], uv[32 * a:32 * a + 32, 7 - e, :],
                           e == 0, e == 7, ['WH', 'uT%d_0' % k, 'uT%d_1' % k], [PS(a)], last=(e == 7),
                           tp=(32 * a, 0), sgc=True)
        import os
        cut = int(os.environ.get('S5CUT', '99'))
        if cut <= 0:
            return
        hv = ps[:, 0:4, :].rearrange("p b (q r c) -> p (b q) r c", r=2, c=128)
        hr, hi = hv[:, :, 0, :], hv[:, :, 1, :]
        kh = [PS(0), PS(1), PS(2), PS(3)]
        TT(tmp, hr, Tc, ALU.mult, kh + kTc, ktmp)
        TT(Gr, hi, Ts, ALU.mult, kh + kTs, kGr)
        TT(Gr, Gr, tmp, ALU.add, kGr + ktmp, kGr)
        TT(tmp, hr, Ts, ALU.mult, kh + kTs, ktmp)
        TT(Gi, hi, Tc, ALU.mult, kh + kTc, kGi)
        TT(Gi, Gi, tmp, ALU.subtract, kGi + ktmp, kGi)
        for pr in range(8):
            d0 = s5sm[:, pr:pr + 1].broadcast_to([128, 128])
            SCAN(Gsr[:, pr, :], d0, Gr[:, pr, :], Gsl[:, pr, 0:1], kGr + ['s5sm', 'Gsl'], kGsr)
            SCAN(Gsi[:, pr, :], d0, Gi[:, pr, :], Gsl[:, pr, 1:2], kGi + ['s5sm', 'Gsl'], kGsi)
        CP(Gsl[:, :, 0], Gsr[:, :, 127], kGsr, ['Gsl'], eng='pool')
        CP(Gsl[:, :, 1], Gsi[:, :, 127], kGsi, ['Gsl'], eng='pool')
        TT(tmp, Gsr, Tc, ALU.mult, kGsr + kTc, ktmp)
        TT(Gr, Gsi, Ts, ALU.mult, kGsi + kTs, kGr)
        TT(S5S[:, :, 0, 1:129], tmp, Gr, ALU.subtract, ktmp + kGr, ['S5S'])
        TT(tmp, Gsr, Ts, ALU.mult, kGsr + kTs, ktmp)
        TT(Gr, Gsi, Tc, ALU.mult, kGsi + kTc, kGr)
        TT(S5S[:, :, 1, 1:129], tmp, Gr, ALU.add, ktmp + kGr, ['S5S'])
        if cut <= 1:
            return
        ybank = {0: (4, 5), 1: (0, 1)}
        for k in range(2):
            uv = uT[:, k, :].rearrange("p (c s) -> p s c", s=8)
            bks = ybank[k]
            for tau in range(8):
                for b2 in range(2):
                    t_lo, t_hi = max(tau, 4 * b2), 4 * b2 + 4
                    if t_lo >= t_hi:
                        continue
                    out = ps[:, bks[b2], (t_lo - 4 * b2) * 128:(t_hi - 4 * b2) * 128]
                    MM(out, KT[:, k, tau, :], uv[:, t_lo - tau:t_hi - tau, :], tau == 0, False,
                       ['KT', 'uT%d_0' % k, 'uT%d_1' % k], [PS(bks[b2])], last=False, sgc=True)
            for a in range(4):
                pr = 4 * k + a
                for t in range(8):
                    for ri in range(2):
                        lastmm = (a == 3 and t == 7 and ri == 1)
                        out = ps[32 * a:32 * a + 32, bks[t // 4], (t % 4) * 128:(t % 4) * 128 + 128]
                        MM(out, E5[:, pr, t + 1, ri, :], S5S[:, 2 * a + k, ri, 0:128], False, lastmm,
                           ['E5', 'S5S'], [PS(bks[0]), PS(bks[1])], last=lastmm, tp=(0, 32 * a), sgc=True)
        if cut <= 2:
            return
        CP(S5S[:, :, :, 0:1], S5S[:, :, :, 128:129], ['S5S'], ['S5S'], eng='pool')
        for sb in range(2):
            lo = sb * 512
            y2b = ZT[:, 6, :].bitcast(BF16).rearrange("p (k t) -> p k t", k=2)
            for k in range(2):
                bks = ybank[k]
                yv = ps[:, bks[0]:bks[0] + 2, :].rearrange("p b (t c) -> p (b t) c", c=128)[:, :, 64 * sb:64 * sb + 64]
                yv = yv.rearrange("p t c -> p c t")
                y1 = ZT[:, 4 + k, :]
                STT(y1.rearrange("p (c t) -> p c t", t=8), uT[:, k, lo:lo + 512].rearrange("p (c t) -> p c t", t=8),
                    pp[:, PP_S5D + k:PP_S5D + k + 1], yv, ALU.mult, ALU.add,
                    ['uT%d_%d' % (k, sb), 'pp', PS(bks[0]), PS(bks[1])], ['z%d' % (4 + k)])
                ACT(y1, y1, AF.Gelu_apprx_tanh, ['z%d' % (4 + k)], ['z%d' % (4 + k)])
                CP(y2b[:, k, :], y1, ['z%d' % (4 + k)], ['z6'], eng='pool')
            for kk in range(2):
                for k in range(2):
                    MM(ps[:, 2 + kk, :], wglu[:, k, kk * 128:(kk + 1) * 128], y2b[:, k, :], k == 0, k == 1, ['wglu', 'z6'],
                       [PS(2 + kk)], last=(k == 1))
                ACT(ZT[:, 7, :], ps[:, 2 + kk, :], AF.Sigmoid, [PS(2 + kk), 'pp'], ['z7'], bias=pp[:, PP_BGLU + kk:PP_BGLU + kk + 1])
                TT(ZT[:, 4 + kk, :], ZT[:, 4 + kk, :], ZT[:, 7, :], ALU.mult, ['z%d' % (4 + kk), 'z7'], ['z%d' % (4 + kk)])
                ACT(ZT[:, 8, kk * 256:(kk + 1) * 256].bitcast(BF16), ZT[:, 4 + kk, :], AF.Square, ['z%d' % (4 + kk)], ['z8'])
            sqv = ZT[:, 8, :].bitcast(BF16)
            for kk in range(2):
                MM(ps[:, 6, :], onesb[:], sqv[:, kk * 512:(kk + 1) * 512], kk == 0, kk == 1, ['onesb', 'z8'], [PS(6)], last=(kk == 1))
            rsqrt_from_sum(ZT[:, 9, :], ps[:, 6, :], 256, [PS(6), 'epst'], ['z9'])
            for kk in range(2):
                STT(A[:, 4 + kk, lo:lo + 512], ZT[:, 4 + kk, :], gains[:, 25 + kk:26 + kk], ZT[:, 9, :], ALU.mult, ALU.mult,
                    ['z%d' % (4 + kk), 'gains', 'z9'], ['A%d_%d' % (4 + kk, sb)])

    def gla_mixer(l, hf):
        Lb = ZT[:, 0:4, :].rearrange("p (a b) t -> p a (b t)", a=2)
        kL = [['z0', 'z1'], ['z2', 'z3']]
        kq = lambda p2: ['qT%d_0' % p2, 'qT%d_1' % p2]
        kk_ = lambda p2: ['kT%d_0' % p2, 'kT%d_1' % p2]
        ktok = ktok_t
        PT = uT[:, 0, :].rearrange("p (s c) -> p s c", c=128)
        Sbf = uT[:, 1, :].rearrange("p (s q c) -> p s q c", q=2, c=128)
        osq = rgT[:].rearrange("p a (s t) -> p (a s) t", t=512)
        cnt = 0
        for h in range(4):
            for sb in range(2):
                gk_ = 'gT%d_%d' % (h, sb)
                ACT(gT[:, h, sb * 512:(sb + 1) * 512], gT[:, h, sb * 512:(sb + 1) * 512], AF.Silu, [gk_], [gk_])
        for p2 in range(2):
            for sb in range(2):
                b = cnt % 4
                cnt += 1
                MM(ps[:, b, :], wgk[0:16, p2 * 128:(p2 + 1) * 128], gkl[0:16, sb * 512:(sb + 1) * 512], True, True,
                   ['wgk', 'gkl_%d' % sb], [PS(b)])
                Lv = Lb[:, p2, sb * 512:(sb + 1) * 512]
                ACT(Lv, ps[:, b, :], AF.Exp, [PS(b), 'rgc'], [kL[p2][sb]], scale=-1.0, bias=rgc[:, 4 + p2:5 + p2])
                ACT(Lv, Lv, AF.Ln, [kL[p2][sb]], [kL[p2][sb]], bias=1.0)
            SCAN(Lb[:, p2, :], segb[:], Lb[:, p2, :], 0.0, kL[p2] + ['segb'], kL[p2])
            Lc = Lb[:, p2, :].rearrange("p (n c) -> p n c", c=64)
            Lmid, Llast = Lc[:, :, 31], Lc[:, :, 63]
            ACT(gsc[:, 0, p2, :], Llast, AF.Exp, kL[p2], ['gsc'], scale=-1.0 / 16)
            ACT(gsc[:, 2, p2, :], Lmid, AF.Exp, kL[p2], ['gsc'], scale=-1.0 / 16)
            TT(gsc[:, 1, p2, :], Llast, Lmid, ALU.subtract, kL[p2], ['gsc'])
            ACT(gsc[:, 1, p2, :], gsc[:, 1, p2, :], AF.Exp, ['gsc'], ['gsc'], scale=-1.0 / 16)
            import os
            gcut = int(os.environ.get('GLACUT', '99'))
            if gcut <= 0:
                continue
            D1 = rx[:, p2, 3:3 + HALF]
            kD = ['rx%d_0' % p2, 'rx%d_1' % p2]
            TT(D1.rearrange("p (n c) -> p n c", c=64), Lc, Lmid.unsqueeze(2).broadcast_to([128, 16, 64]), ALU.subtract, kL[p2], kD)
            eq = ZT[:, 4:6, :].rearrange("p a t -> p (a t)")
            ek = ZT[:, 6:8, :].rearrange("p a t -> p (a t)")
            ACT(eq, D1, AF.Exp, kD, ['z4', 'z5'], scale=-1.0 / 16, bias=lneighth[:])
            ACT(ek, D1, AF.Exp, kD, ['z6', 'z7'], scale=1.0 / 16)
            TT(qT[:, p2, :], qT[:, p2, :], eq, ALU.mult, kq(p2) + ['z4', 'z5'], kq(p2))
            TT(ek, kT[:, p2, :], ek, ALU.mult, kk_(p2) + ['z6', 'z7'], ['z6', 'z7'])
            CP(kT[:, p2, :], ek, ['z6', 'z7'], kk_(p2), eng='pool')
            if gcut <= 1:
                continue
            for g4 in range(2):
                bk = 4 + g4
                tks = ['scb%d' % g4]
                for i_ in range(4):
                    blk = 4 * g4 + i_
                    TR(ps[:, bk, i_ * 128:(i_ + 1) * 128], ek[:, blk * 128:(blk + 1) * 128], ident, ['z6', 'z7', 'cst'], tks,
                       last=(i_ == 3))
                CP(ktok[:, 4 * g4:4 * g4 + 4, p2, :], ps[:, bk, :].rearrange("p (b c) -> p b c", c=128), tks, ['z8', 'z9'],
                   eng=('act' if g4 else 'dve'))
        scn = 0
        if gcut <= 2:
            return
        for sbk in range(2):
            for blk in range(4):
                bg = 4 * sbk + blk
                for h in range(4):
                    p2, hl = h // 2, h % 2
                    hs = slice(hl * 64, (hl + 1) * 64)
                    MM(ps[:, 4 + hl, p2 * 128:(p2 + 1) * 128], kT[hs, p2, bg * 128:(bg + 1) * 128], qT[hs, p2, bg * 128:(bg + 1) * 128],
                       True, True, kk_(p2) + kq(p2), ['scb%d' % hl])
                pbase = (bg % 2) * 4
                for hl in range(2):
                    TT(PT[:, pbase + hl:pbase + hl + 3:2, :], ps[:, 4 + hl, 0:256].rearrange("p (q c) -> p q c", c=128),
                       cmask.unsqueeze(1).broadcast_to([128, 2, 128]), ALU.mult, ['scb%d' % hl, 'cst'],
                       ['PT%d' % (pbase + hl), 'PT%d' % (pbase + hl + 2)])
                for h in range(4):
                    pl = pbase + h
                    MM(ps[:, h, blk * 128:(blk + 1) * 128], vtok[:, bg, h * 128:(h + 1) * 128], PT[:, pl, :], blk == 0, False,
                       ['vtok%d' % bg, 'PT%d' % pl], [PS(h)], sgc=True)
            if gcut <= 3:
                return
            for n in range(8):
                ng = 8 * sbk + n
                bg, hh = ng // 2, ng % 2
                ts_ = slice(hh * 64, (hh + 1) * 64)
                ssl = ng % 4
                kvs = ng % 2
                for p2 in range(2):
                    for hl in range(2):
                        h = 2 * p2 + hl
                        MM(ps[hl * 64:(hl + 1) * 64, 6 + kvs, p2 * 128:(p2 + 1) * 128],
                           ktok[ts_, bg, p2, hl * 64:(hl + 1) * 64], vtok[ts_, bg, h * 128:(h + 1) * 128], True, True,
                           ['z8', 'z9', 'vtok%d' % bg], ['kv%d' % kvs], last=(p2 == 1 and hl == 1))
                TT(Sbf[:, ssl, :, :], Sst[:], gsc[:, 2, :, ng].unsqueeze(2).broadcast_to([128, 2, 128]), ALU.mult,
                   ['Sst', 'gsc'], ['Sbf%d' % ssl])
                for h in range(4):
                    p2, hl = h // 2, h % 2
                    hs = slice(hl * 64, (hl + 1) * 64)
                    tcol = (4 * sbk) * 128 + n * 64
                    MM(ps[:, h, n * 64:(n + 1) * 64], Sbf[hs, ssl, p2, :], qT[hs, p2, tcol:tcol + 64], False, n == 7,
                       ['Sbf%d' % ssl] + kq(p2), [PS(h)], sgc=True)
                kvv = ps[:, 6 + kvs, 0:256].rearrange("p (q c) -> p q c", c=128)
                tkv = ZT[:, 4, 0:256].rearrange("p (q c) -> p q c", c=128)
                TT(tkv, kvv, gsc[:, 1, :, ng].unsqueeze(2).broadcast_to([128, 2, 128]), ALU.mult, ['kv%d' % kvs, 'gsc'], ['z4'])
                TT(Sst[:], Sst[:], gsc[:, 0, :, ng].unsqueeze(2).broadcast_to([128, 2, 128]), ALU.mult, ['Sst', 'gsc'], ['Sst'])
                TT(Sst[:], Sst[:], tkv, ALU.add, ['Sst', 'z4'], ['Sst'])
            if gcut <= 4:
                return
            lo = sbk * 512
            for h in range(4):
                oq = osq[:, h % 4, :]
                ACT(oq, ps[:, h, :], AF.Square, [PS(h)], ['rgT%d_%d' % (h // 2, h % 2)])
                MM(ps[:, 4 + h % 2, :], onesb[:], oq, True, True, ['onesb', 'rgT%d_%d' % (h // 2, h % 2)], ['scb%d' % (h % 2)])
                za, zb = (5, 6) if h % 2 == 0 else (7, 0)
                rsqrt_from_sum(ZT[:, za, :], ps[:, 4 + h % 2, :], 128, ['scb%d' % (h % 2), 'epst'], ['z%d' % za])
                STT(ZT[:, zb, :], ps[:, h, :], gains[:, 24:25], ZT[:, za, :], ALU.mult, ALU.mult, [PS(h), 'gains', 'z%d' % za], ['z%d' % zb])
                TT(A[:, h, lo:lo + 512], ZT[:, zb, :], gT[:, h, lo:lo + 512], ALU.mult, ['z%d' % zb, 'gT%d_%d' % (h, sbk)],
                   ['A%d_%d' % (h, sbk)])

    def outproj(l, hf):
        W = wout_d[l]
        cnt = 0

        def ld(i):
            P.dma('pool', 'wct%dL%d' % (i % 3, l), wct[i % 3][:], W[:, i * 128:(i + 1) * 128].rearrange("(k p) c -> p k c", p=128),
                  writes=['wct%d' % (i % 3)])
        ld(0)
        ld(1)
        for i in range(8):
            slot = i % 3
            if i + 2 < 8:
                ld(i + 2)
            for sb in range(2):
                b = cnt % 4
                cnt += 1
                t0 = hf * HALF + sb * 512
                for k in range(8):
                    MM(ps[:, b, :], wct[slot][:, k, :], A[:, k, sb * 512:(sb + 1) * 512], k == 0, k == 7,
                       ['wct%d' % slot, 'A%d_%d' % (k, sb)], [PS(b)], last=(k == 7))
                xk = 'x%d_%d' % (i, t0 // 512)
                TT(xT[:, i, t0:t0 + 512], xT[:, i, t0:t0 + 512], ps[:, b, :], ALU.add, [xk, PS(b)], [xk])

    def ffn_prefetch(l, hf):
        Wu = wup_d[l]
        P.dma('pool', 'upgv2L%d' % l, upgv[2][:, :, 0:128], Wu[:, 0:128].rearrange("(k p) c -> p k c", p=128), writes=['upgv2'])
        P.dma('pool', 'upgv2L%d' % l, upgv[2][:, :, 128:256], Wu[:, DFF:DFF + 128].rearrange("(k p) c -> p k c", p=128), writes=['upgv2'])

    def ffn(l, hf):
        Wu, Wd = wup_d[l], wdn_d[l]
        cnt = 0
        def ldu(j):
            sk_ = 'upgv%d' % ((j + 2) % 3)
            P.dma('pool', sk_ + 'L%d' % l, upgv[(j + 2) % 3][:, :, 0:128], Wu[:, j * 128:(j + 1) * 128].rearrange("(k p) c -> p k c", p=128), writes=[sk_])
            P.dma('pool', sk_ + 'L%d' % l, upgv[(j + 2) % 3][:, :, 128:256],
                  Wu[:, DFF + j * 128:DFF + (j + 1) * 128].rearrange("(k p) c -> p k c", p=128), writes=[sk_])

        def ldd(i):
            P.dma('pool', 'wdn%dL%d' % (i % 2, l), wdn[i % 2][:], Wd[:, i * 128:(i + 1) * 128].rearrange("(j p) c -> p j c", p=128),
                  writes=['wdn%d' % (i % 2)])
        ldu(1)
        pend = []
        import os
        FFN_TS_ENG = 'dve'
        for j in range(NJ):
            slot = (j + 2) % 3
            sk = 'upgv%d' % slot
            if j + 2 < NJ:
                ldu(j + 2)
            elif j + 2 == NJ:
                ldd(0)
            elif j + 1 == NJ:
                ldd(1)
            for sb in range(2):
                q = cnt % 3
                qp = (cnt - 1) % 3
                cnt += 1
                bu, bg_ = 2 * q, 2 * q + 1
                for k in range(8):
                    MM(ps[:, bu, :], upgv[slot][:, k, 0:128], A[:, k, sb * 512:(sb + 1) * 512], k == 0, k == 7,
                       [sk, 'A%d_%d' % (k, sb)], [PS(bu)], last=(k == 7))
                for k in range(8):
                    MM(ps[:, bg_, :], upgv[slot][:, k, 128:256], A[:, k, sb * 512:(sb + 1) * 512], k == 0, k == 7,
                       [sk, 'A%d_%d' % (k, sb)], [PS(bg_)], last=(k == 7))
                us, uk = upsb[q], 'us%d' % q
                if sb == 0:
                    CP(us[:, 0:2], carry[:, j, :], ['carry'], [uk], eng='act')
                else:
                    CP(us[:, 0:2], upsb[qp][:, 512:514], ['us%d' % qp], [uk], eng='act')
                CP(us[:, 2:514], ps[:, bu, :], [PS(bu)], [uk], eng='act')
                if sb == 1:
                    CP(carry[:, j, :], us[:, 512:514], [uk], ['carry'], eng='act')
                cw = lambda tap: pp[:, PP_MCW + j * 3 + tap:PP_MCW + j * 3 + tap + 1]
                cv, ck = fcv[q], 'cv%d' % q
                TS(cv[:], us[:, 2:514], cw(2), pp[:, PP_MCB + j:PP_MCB + j + 1], ALU.mult, ALU.add, [uk, 'pp'], [ck], eng=FFN_TS_ENG)
                STT(cv[:], us[:, 1:513], cw(1), cv[:], ALU.mult, ALU.add, [uk, 'pp', ck], [ck])
                STT(cv[:], us[:, 0:512], cw(0), cv[:], ALU.mult, ALU.add, [uk, 'pp', ck], [ck])
                if pend:
                    pend.pop()()
                ACT(cv[:], cv[:], AF.Gelu_apprx_tanh, [ck], [ck])
                pend.append(lambda j=j, sb=sb, cv=cv, ck=ck, bg_=bg_: TT(interm[:, j, sb * 512:(sb + 1) * 512], cv[:], ps[:, bg_, :],
                                                                      ALU.mult, [ck, PS(bg_)], ['im%d_%d' % (j, sb)]))
        while pend:
            pend.pop()()
        cnt = 0
        for i in range(8):
            slot = i % 2
            sk = 'wdn%d' % slot
            if i >= 1 and i + 1 < 8:
                ldd(i + 1)
            for sb in range(2):
                b = 6 + cnt % 2
                cnt += 1
                t0 = hf * HALF + sb * 512
                for j in range(NJ):
                    MM(ps[:, b, :], wdn[slot][:, j, :], interm[:, j, sb * 512:(sb + 1) * 512], j == 0, j == NJ - 1,
                       [sk, 'im%d_%d' % (j, sb)], [PS(b)], last=(j == NJ - 1))
                xk = 'x%d_%d' % (i, t0 // 512)
                TT(xT[:, i, t0:t0 + 512], xT[:, i, t0:t0 + 512], ps[:, b, :], ALU.add, [xk, PS(b)], [xk])

    def final_out():
        for blk in range(4):
            t0 = blk * 512
            sqv = sqbuf[:]
            for k in range(8):
                ACT(sqv[:, k, :], xT[:, k, t0:t0 + 512], AF.Square, ['x%d_%d' % (k, blk)], ['z%d' % (k // 2)])
            for k in range(8):
                MM(ps[:, 6, :], onesb[:], sqv[:, k, :], k == 0, k == 7, ['onesb', 'z%d' % (k // 2)], [PS(6)], last=(k == 7))
            rsqrt_from_sum(rstd_n, ps[:, 6, :], 1024, [PS(6), 'epst'], ['z4'])
            for k in range(8):
                STT(xo[:, k, :], xT[:, k, t0:t0 + 512], gains[:, 16 + k:17 + k], rstd_n, ALU.mult, ALU.mult,
                    ['x%d_%d' % (k, blk), 'gains', 'z4'], ['xo%d' % k])
            for t4 in range(4):
                tt = blk * 4 + t4
                s, sk = xs[tt % 2], 'xs%d' % (tt % 2)
                for kq_ in range(2):
                    b = (tt * 2 + kq_) % 4
                    for kk in range(4):
                        k = kq_ * 4 + kk
                        TR(ps[:, b, kk * 128:(kk + 1) * 128], xo[:, k, t4 * 128:(t4 + 1) * 128], ident, ['xo%d' % k, 'cst'], [PS(b)],
                           last=(kk == 3))
                    CP(s[:, kq_ * 512:(kq_ + 1) * 512], ps[:, b, :], [PS(b)], [sk], eng=('act' if kq_ == 0 else 'dve'))
                P.dma('sp', 'outd%d' % (tt % 2), out_d[tt * 128:(tt + 1) * 128, :], s[:], reads=[sk])

    import os
    stop_hf = int(os.environ.get('STOPHF', '0'))
    done = False
    for l in range(nlayers):
        layer_setup(l)
        hooks = [lambda q_=q_: load_x(range(4 * q_, 4 * q_ + 4)) for q_ in range(4)] if l == 0 else []
        s5_setup(l, hooks)
        P.barrier()
        MSET(rxhist[:], 0.0, ['rxhist'])
        for hf in range(2):
            rmsnorm_to_A(l, hf, 0)
            if stop == 'norm1' and hf == stop_hf:
                done = True
                break
            inproj(l, hf)
            if stop == 'inproj' and hf == stop_hf:
                done = True
                break
            rg_post(l, hf)
            if stop == 'rg' and hf == stop_hf:
                done = True
                break
            s5_mixer(l, hf)
            if stop == 's5' and hf == stop_hf:
                done = True
                break
            gla_mixer(l, hf)
            if stop == 'gla' and hf == stop_hf:
                done = True
                break
            outproj(l, hf)
            if stop == 'outproj' and hf == stop_hf:
                done = True
                break
            ffn_prefetch(l, hf)
            rmsnorm_to_A(l, hf, 8)
            P.barrier()
            ffn(l, hf)
            P.barrier()
            if stop == 'ffn' and hf == stop_hf:
                done = True
                break
        if done:
            break
    if not done:
        P.barrier()
        final_out()

    avail = dict(A=(A, [128, 8, HALF], BF16), qT=(qT, [128, 2, HALF], BF16), kT=(kT, [128, 2, HALF], BF16),
                 gT=(gT, [128, 4, HALF], BF16), uT=(uT, [128, 2, HALF], BF16), rx=(rx, [128, 2, 3 + HALF], F32),
                 rgT=(rgT, [128, 2, HALF], BF16), vtok=(vtok, [128, 8, 512], BF16), gkl=(gkl, [128, HALF], BF16),
                 xT=(xT, [128, 8, NT], F32), ZT=(ZT, [128, 10, 512], F32))
    for n in dbg_names:
        t, shp, dt = avail[n]
        d = nc.dram_tensor("dbg_" + n, shp, dt, kind="ExternalOutput").ap()
        P.barrier()
        P.dma('sp', 'dbg', d, t[:])
    P.emit()
    return nc


def host_prep(inputs):
    f = lambda n: np.ascontiguousarray(np.asarray(inputs[n], dtype=np.float32))
    cc = np.zeros((128, NCC), np.float32)
    cc[:, CC_ID:CC_ID + 128] = np.eye(128, dtype=np.float32)
    s = np.arange(128)[:, None]
    c = np.arange(128)[None, :]
    cc[:, CC_MASK:CC_MASK + 128] = ((s // 64 == c // 64) & (s <= c)).astype(np.float32)
    cc[:, CC_SEG:CC_SEG + HALF] = (np.arange(HALF) % 64 != 0).astype(np.float32)[None, :]
    cc[:, CC_CIDX:CC_CIDX + 256] = np.arange(256, dtype=np.float32)[None, :]
    cc[:, CC_IDX9:CC_IDX9 + 9] = np.arange(9, dtype=np.float32)[None, :]
    q = np.arange(128)
    glp = (q // 16) % 2
    cc[:, CC_MASKT + 0] = (glp == 0)
    cc[:, CC_MASKT + 1] = (glp == 1)
    pp = np.zeros((DEPTH, 128, NPP), np.float32)

    def tile_vec(v, nt):
        return v.reshape(DEPTH, nt, 128).transpose(0, 2, 1)

    pp[:, :, PP_AN:PP_AN + 8] = tile_vec(f('attn_norm'), 8)
    pp[:, :, PP_MN:PP_MN + 8] = tile_vec(f('mlp_norm'), 8)
    pp[:, :, PP_FN:PP_FN + 8] = np.broadcast_to(f('final_norm').reshape(8, 128).T[None], (DEPTH, 128, 8))
    pp[:, :, PP_BGK:PP_BGK + 2] = tile_vec(f('gla_b_gk'), 2)
    pp[:, :, PP_GN] = f('gla_norm')
    pp[:, :, PP_S5D:PP_S5D + 2] = tile_vec(f('s5_d'), 2)
    pp[:, :, PP_BGLU:PP_BGLU + 2] = tile_vec(f('s5_b_glu'), 2)
    pp[:, :, PP_S5N:PP_S5N + 2] = tile_vec(f('s5_norm'), 2)
    rcw = f('rg_conv_w').reshape(DEPTH, 4, 2, 128).transpose(0, 3, 2, 1)
    pp[:, :, PP_RCW:PP_RCW + 8] = rcw.reshape(DEPTH, 128, 8)
    pp[:, :, PP_RCB:PP_RCB + 2] = tile_vec(f('rg_conv_b'), 2)
    pp[:, :, PP_RBA:PP_RBA + 2] = tile_vec(f('rg_b_a'), 2)
    pp[:, :, PP_RBX:PP_RBX + 2] = tile_vec(f('rg_b_x'), 2)
    pp[:, :, PP_RLAM:PP_RLAM + 2] = tile_vec(f('rg_lambda'), 2)
    pp[:, :, PP_RN:PP_RN + 2] = tile_vec(f('rg_norm'), 2)
    mcw = f('mlp_conv_w').reshape(DEPTH, 3, NJ, 128).transpose(0, 3, 2, 1)
    pp[:, :, PP_MCW:PP_MCW + 66] = mcw.reshape(DEPTH, 128, 66)
    pp[:, :, PP_MCB:PP_MCB + NJ] = tile_vec(f('mlp_conv_b'), NJ)
    def SL(a):
        sh = a.shape
        a = a.reshape((DEPTH, 8, 2, 64) + sh[3:])
        a = np.moveaxis(a, 1, 3)
        return a.reshape((DEPTH, 128, 8) + sh[3:])
    lre, lim = f('s5_lambda_re'), f('s5_lambda_im')
    ldt = np.broadcast_to(f('s5_log_dt')[:, :, None], (DEPTH, 16, 64))
    pp[:, :, PP_LRE:PP_LRE + 8] = SL(lre)
    pp[:, :, PP_LIM:PP_LIM + 8] = SL(lim)
    pp[:, :, PP_LDT:PP_LDT + 8] = SL(np.ascontiguousarray(ldt))
    pp[:, :, PP_BRE:PP_BRE + 128] = SL(f('s5_b_re')).reshape(DEPTH, 128, 128)
    pp[:, :, PP_BIM:PP_BIM + 128] = SL(f('s5_b_im')).reshape(DEPTH, 128, 128)
    pp[:, :, PP_CRE:PP_CRE + 128] = SL(f('s5_c_re').transpose(0, 1, 3, 2)).reshape(DEPTH, 128, 128)
    pp[:, :, PP_CIM:PP_CIM + 128] = SL(f('s5_c_im').transpose(0, 1, 3, 2)).reshape(DEPTH, 128, 128)
    def TL(a, per_i):
        if not per_i:
            a = np.broadcast_to(a[..., None], a.shape + (16,))
        a = a.reshape(DEPTH, 2, 4, 2, 64, 16)
        a = a.transpose(0, 2, 3, 5, 1, 4)
        return a.reshape(DEPTH, 128, 128)
    pp[:, :, PP_LRET:PP_LRET + 128] = TL(lre, False)
    pp[:, :, PP_LIMT:PP_LIMT + 128] = TL(lim, False)
    pp[:, :, PP_LDTT:PP_LDTT + 128] = TL(np.ascontiguousarray(ldt), False)
    pp[:, :, PP_BRET:PP_BRET + 128] = TL(f('s5_b_re'), True)
    pp[:, :, PP_BIMT:PP_BIMT + 128] = TL(f('s5_b_im'), True)
    shared = dict(cc=cc, pp=pp, w_in=f('w_in'), w_out=f('w_out'), w_up=f('w_up'), w_down=f('w_down'),
                  wgk=f('gla_w_gk_up'), rwa=f('rg_w_a'), rwx=f('rg_w_x'), wglu=f('s5_w_glu'))
    return shared


_CACHE = {}


def kernel(**inputs):
    x = np.ascontiguousarray(np.asarray(inputs['x'], dtype=np.float32))
    shared = host_prep(inputs)
    if 'nc' not in _CACHE:
        _CACHE['nc'] = build_program()
    nc = _CACHE['nc']
    in_maps = [dict(shared, x=x[b]) for b in range(8)]
    res = run_bass_kernel_spmd(nc, in_maps, core_ids=list(range(8)))
    return np.stack([res.results[b]['out'] for b in range(8)], axis=0).astype(np.float32)
```

```python
import math
import numpy as np
import concourse.bass as bass
import concourse.mybir as mybir
from concourse.bass_utils import run_bass_kernel_spmd

F32 = mybir.dt.float32
BF16 = mybir.dt.bfloat16
I32 = mybir.dt.int32
ALU = mybir.AluOpType
AF = mybir.ActivationFunctionType

ENG = ['pe', 'act', 'dve', 'pool', 'sp']
DEPTH = 4
NT = 2048
HALF = 1024
DM = 1024
DFF = 2816
NJ = 22
EPS = 1e-6
PI = math.pi

CC_ID, CC_MASK, CC_SEG, CC_CIDX, CC_IDX9, CC_MASKT, NCC = 0, 128, 256, 1280, 1536, 1545, 1548
PP_AN, PP_MN, PP_FN, PP_BGK, PP_GN = 0, 8, 16, 24, 26
PP_S5D, PP_BGLU, PP_S5N = 27, 29, 31
PP_RCW, PP_RCB, PP_RBA, PP_RBX, PP_RLAM, PP_RN = 33, 41, 43, 45, 47, 49
PP_MCW, PP_MCB = 51, 117
PP_LRE, PP_LIM, PP_LDT = 139, 147, 155
PP_LRET, PP_LIMT, PP_LDTT = 163, 291, 419
PP_BRE, PP_BIM, PP_BRET, PP_BIMT, PP_CRE, PP_CIM, NPP = 547, 675, 803, 931, 1059, 1187, 1316


class Prog:
    def __init__(self, nc):
        self.nc = nc
        self.streams = {e: [] for e in ENG}
        self.cnt = {e: 0 for e in ENG}
        self.known = {e: {} for e in ENG}
        self.snap = {}
        self.lastw = {}
        self.readers = {}
        self.dcnt = {}
        self.pe_pending = []
        self.pe_reads = []
        self.pe_writes = []
        self.ekeys = set()

    def _need(self, E, ev, waits):
        s, v = ev
        if self.known[E].get(s, 0) >= v:
            return
        if '#' in s:
            e0, ep = s.split('#')
            for s2 in self.known[E]:
                if s2.startswith(e0 + '#') and int(s2.split('#')[1]) > int(ep):
                    return
        waits[s] = max(waits.get(s, 0), v)

    def _collect(self, E, reads, writes):
        waits = {}
        for k in reads:
            w = self.lastw.get(k)
            if w is not None:
                self._need(E, w, waits)
        for k in writes:
            w = self.lastw.get(k)
            if w is not None and (E != 'pe' or w[0].split('#')[0] != E):
                self._need(E, w, waits)
            for r in self.readers.get(k, ()):
                if E != 'pe' or r[0].split('#')[0] != E:
                    self._need(E, r, waits)
        kn = self.known[E]
        for s, v in waits.items():
            self.streams[E].append(('wait', s, v))
            if kn.get(s, 0) < v:
                kn[s] = v
            sn = self.snap.get((s, v))
            if sn:
                for s2, v2 in sn.items():
                    if kn.get(s2, 0) < v2:
                        kn[s2] = v2

    def _register(self, ev, reads, writes):
        for k in writes:
            self.lastw[k] = ev
            self.readers[k] = []
        for k in reads:
            self.readers.setdefault(k, []).append(ev)

    EPOCH = 1500

    def _tick(self, E):
        self.cnt[E] += 1
        ep = (self.cnt[E] - 1) // self.EPOCH
        key = '%s#%d' % (E, ep)
        self.ekeys.add(key)
        return (key, self.cnt[E] - ep * self.EPOCH)

    def op(self, E, fn, reads=(), writes=()):
        self._collect(E, reads, writes)
        ev = self._tick(E)
        self.snap[ev] = dict(self.known[E])
        self.streams[E].append(('op', fn, ev[0], 1))
        self._register(ev, reads, writes)

    def mm(self, fn, reads=(), writes=(), last=True):
        self.pe_pending.append(fn)
        self.pe_reads += list(reads)
        self.pe_writes += list(writes)
        if last:
            self._collect('pe', self.pe_reads, self.pe_writes)
            ev = self._tick('pe')
            self.snap[ev] = dict(self.known['pe'])
            for f in self.pe_pending[:-1]:
                self.streams['pe'].append(('op', f, None, 0))
            self.streams['pe'].append(('op', self.pe_pending[-1], ev[0], 1))
            self._register(ev, self.pe_reads, self.pe_writes)
            self.pe_pending, self.pe_reads, self.pe_writes = [], [], []

    def dma(self, Q, sem, out, in_, reads=(), writes=()):
        self._collect(Q, reads, writes)
        self.dcnt[sem] = self.dcnt.get(sem, 0) + 16
        ev = ('d:' + sem, self.dcnt[sem])
        self.snap[ev] = dict(self.known[Q])
        self.streams[Q].append(('op', lambda e: e.dma_start(out=out, in_=in_), 'd:' + sem, 16))
        self._register(ev, reads, writes)

    def barrier(self):
        assert not self.pe_pending
        evs = []
        for e in ENG:
            if self.cnt[e] > 0:
                ep = (self.cnt[e] - 1) // self.EPOCH
                evs.append(('%s#%d' % (e, ep), self.cnt[e] - ep * self.EPOCH))
        evs += [('d:' + s, v) for s, v in self.dcnt.items()]
        for E in ENG:
            waits = {}
            for ev in evs:
                if ev[0].split('#')[0] != E:
                    self._need(E, ev, waits)
            for s, v in waits.items():
                self.streams[E].append(('wait', s, v))
                self.known[E][s] = v

    def emit(self):
        import contextlib
        nc = self.nc
        self.barrier()
        names = sorted(self.ekeys) + ['d:' + s for s in self.dcnt]
        with contextlib.ExitStack() as st:
            sems = {}
            for i, n in enumerate(names):
                sems[n] = st.enter_context(nc.semaphore('s%d' % i))
            block = st.enter_context(nc.Block())

            def run(E):
                def body(eng):
                    for it in self.streams[E]:
                        if it[0] == 'wait':
                            eng.wait_ge(sems[it[1]], it[2])
                        else:
                            ins = it[1](eng)
                            if it[2] is not None:
                                ins.then_inc(sems[it[2]], it[3])
                return body

            block.tensor(run('pe'))
            block.scalar(run('act'))
            block.vector(run('dve'))
            block.gpsimd(run('pool'))
            block.sync(run('sp'))


def build_program(nlayers=DEPTH, stop=None, dbg_names=()):
    nc = bass.Bass("TRN2", target_bir_lowering=False)
    P = Prog(nc)

    def dram(name, shape, dt=F32, kind="ExternalInput"):
        return nc.dram_tensor(name, shape, dt, kind=kind).ap()

    x_d = dram("x", [NT, DM])
    out_d = dram("out", [NT, DM], kind="ExternalOutput")
    cc_d = dram("cc", [128, NCC])
    pp_d = dram("pp", [DEPTH, 128, NPP])
    win_d = dram("w_in", [DEPTH, DM, 2320])
    wout_d = dram("w_out", [DEPTH, DM, DM])
    wup_d = dram("w_up", [DEPTH, DM, 2 * DFF])
    wdn_d = dram("w_down", [DEPTH, DFF, DM])
    wgk_d = dram("wgk", [DEPTH, 16, 256])
    rwa_d = dram("rwa", [DEPTH, 4, 64, 64])
    rwx_d = dram("rwx", [DEPTH, 4, 64, 64])
    wglu_d = dram("wglu", [DEPTH, 256, 256])

    cur = [16512]
    SB_TOP = 229376
    uid = [0]

    def alloc(shape, dt, at=None):
        size = int(np.prod(shape[1:])) * (4 if dt in (F32, I32) else 2)
        off = ((cur[0] if at is None else at) + 31) // 32 * 32
        assert off + size <= SB_TOP, (shape, off, size)
        uid[0] += 1
        t = nc.alloc_sbuf_tensor_at("t%d" % uid[0], list(shape), dt, offset=off)
        if at is None:
            cur[0] = off + size
        return t, off + size

    def A_(shape, dt):
        return alloc(shape, dt)[0]

    xT = A_([128, 8, NT], F32)
    A = A_([128, 8, HALF], BF16)
    cst = A_([128, NCC], F32)
    pp = A_([128, NPP], F32)
    identb = A_([128, 128], BF16)
    onesb = A_([128, 128], BF16)
    segb = A_([128, HALF], BF16)
    gains = A_([128, 40], F32)
    rgc = A_([128, 8], F32)
    E5 = A_([128, 8, 9, 2, 32], BF16)
    WH = A_([128, 2, 8, 2, 128], BF16)
    KT = A_([128, 2, 8, 128], BF16)
    s5sm = A_([128, 24], F32)
    S5S = A_([128, 8, 2, 129], BF16)
    Gsl = A_([128, 8, 2], F32)
    wgk = A_([128, 256], BF16)
    rgw = A_([128, 2, 2, 64], BF16)
    wglu = A_([128, 2, 256], BF16)
    Sst = A_([128, 2, 128], F32)
    hlast = A_([128, 2], F32)
    rxhist = A_([128, 2, 3], F32)
    carry = A_([128, NJ, 2], F32)
    gsc = A_([128, 3, 2, 16], F32)
    wb0 = cur[0]
    upgv = [A_([128, 8, 256], BF16) for _ in range(2)]
    wdn = [A_([128, NJ, 128], BF16) for _ in range(2)]
    wb1 = cur[0]
    wct = []
    o = wb0
    for _ in range(3):
        t, o = alloc([128, 8, 128], BF16, at=o)
        wct.append(t)
    wv, o = alloc([128, 8, 512], BF16, at=o)
    yrg, o = alloc([128, 2, HALF], BF16, at=o)
    assert o <= wb1
    r0 = cur[0]
    qT = A_([128, 2, HALF], BF16)
    kT = A_([128, 2, HALF], BF16)
    gT = A_([128, 4, HALF], BF16)
    vtok = A_([128, 8, 512], BF16)
    gkl = A_([128, HALF], BF16)
    uT = A_([128, 2, HALF], BF16)
    rx = A_([128, 2, 3 + HALF], F32)
    rgT = A_([128, 2, HALF], BF16)
    ZT = A_([128, 10, 512], F32)
    r_end = cur[0]
    print("SBUF used", r_end, "of", SB_TOP, "R size", r_end - r0)
    o = r0
    interm, o = alloc([128, NJ, HALF], BF16, at=o)
    upsb = []
    for _ in range(3):
        t, o = alloc([128, 2 + 512], F32, at=o)
        upsb.append(t)
    fcv = []
    for _ in range(3):
        t, o = alloc([128, 512], F32, at=o)
        fcv.append(t)
    assert o <= r_end, (o, r_end)
    o = r0
    xs = []
    for _ in range(2):
        t, o = alloc([128, DM], F32, at=o)
        xs.append(t)
    xo, o = alloc([128, 8, 512], F32, at=o)
    assert o <= r_end
    o = r0
    st_ = {}

    def SA(name, shape, dt=F32):
        nonlocal o
        t, o2 = alloc(shape, dt, at=o)
        o = o2
        st_[name] = t
        return t

    for n in ['dt', 'ar', 'th', 'nr', 'ni', 'den', 'kr', 'ki', 'w8a', 'w8b']:
        SA(n, [128, 8])
    for n in ['tA', 'tB', 'sn9', 'cs9', 'Pr', 'Pi', 'w9a', 'w9b']:
        SA(n, [128, 8, 9])
    SA('w9i', [128, 8, 9], I32)
    SA('w8i', [128, 8], I32)
    for n in ['Bbr', 'Bbi', 'w16a', 'w16b']:
        SA(n, [128, 8, 16])
    SA('t1', [128, 8, 9, 16]); SA('t2', [128, 8, 9, 16])
    SA('Bw', [128, 8, 2, 128], BF16)
    for n in ['dtT', 'arT', 'thT', 'nrT', 'niT', 'denT', 'krT', 'kiT', 'wTa', 'wTb', 'BbrT', 'BbiT']:
        SA(n, [128, 2, 64])
    for n in ['angT', 'wkf', 'snT', 'csT', 'mgT', 'WHr', 'WHi', 'wk2']:
        SA(n, [128, 2, 8, 64])
    SA('wki', [128, 2, 8, 64], I32)
    assert o <= r_end, (o, r_end)

    ps = nc.alloc_psum_tensor("ps", [128, 8, 512], F32)
    psb = ps[:, 7, :].bitcast(BF16).rearrange("p (s c) -> p s c", c=128)

    def PS(b):
        return 'ps%d' % b

    def ACT(out, in_, func, r, w, bias=None, scale=None):
        kw = {}
        if bias is not None:
            kw['bias'] = bias
        if scale is not None:
            kw['scale'] = scale
        P.op('act', lambda e: e.activation(out=out, in_=in_, func=func, **kw), r, w)

    def TT(out, in0, in1, op, r, w, eng='dve'):
        P.op(eng, lambda e: e.tensor_tensor(out=out, in0=in0, in1=in1, op=op), r, w)

    def TS(out, in0, s1, s2, op0, op1, r, w, eng='dve'):
        if s2 is None:
            P.op(eng, lambda e: e.tensor_scalar(out=out, in0=in0, scalar1=s1, scalar2=None, op0=op0), r, w)
        else:
            P.op(eng, lambda e: e.tensor_scalar(out=out, in0=in0, scalar1=s1, scalar2=s2, op0=op0, op1=op1), r, w)

    def STT(out, in0, scalar, in1, op0, op1, r, w, eng='dve'):
        P.op(eng, lambda e: e.scalar_tensor_tensor(out=out, in0=in0, scalar=scalar, in1=in1, op0=op0, op1=op1), r, w)

    def CP(out, in_, r, w, eng='dve'):
        if eng == 'act':
            P.op('act', lambda e: e.copy(out=out, in_=in_), r, w)
        else:
            P.op(eng, lambda e: e.tensor_copy(out=out, in_=in_), r, w)

    def MSET(ap, val, w, eng='dve'):
        P.op(eng, lambda e: e.memset(ap, val), (), w)

    def SCAN(out, d0, d1, init, r, w):
        P.op('dve', lambda e: e.tensor_tensor_scan(out=out, data0=d0, data1=d1, initial=init, op0=ALU.mult, op1=ALU.add), r, w)

    def MM(out, lhsT, rhs, start, stop, r=(), w=(), last=True, tp=None, sgc=False):
        kw = {}
        if tp is not None:
            kw['tile_position'] = tp
        if sgc:
            kw['skip_group_check'] = True
        P.mm(lambda e: e.matmul(out, lhsT=lhsT, rhs=rhs, start=start, stop=stop, **kw), r, w, last)

    def TR(out, in_, ident, r, w, last=True):
        P.mm(lambda e: e.transpose(out, in_, ident), r, w, last)

    def bc(ap, shape):
        return ap.broadcast_to(list(shape))

    def rsqrt_from_sum(out, ss_ps, n, r, w):
        ACT(out, ss_ps, AF.Ln, r, w, bias=epsb[n])
        ACT(out, out, AF.Exp, w, w, scale=-0.5)

    dbg = {}

    P.dma('sp', 'cst', cst[:], cc_d, writes=['cst'])
    P.dma('pool', 'idb', identb[:], cc_d[:, CC_ID:CC_ID + 128], writes=['identb'])
    P.dma('pool', 'sgb', segb[:], cc_d[:, CC_SEG:CC_SEG + HALF], writes=['segb'])
    MSET(onesb[:], 1.0, ['onesb'])
    MSET(E5[:], 0.0, ['E5'], eng='pool')
    epst = A_([128, 4], F32)
    MSET(epst[:, 0:1], 128 * EPS, ['epst'])
    MSET(epst[:, 1:2], 256 * EPS, ['epst'])
    MSET(epst[:, 2:3], 1024 * EPS, ['epst'])
    epsb = {128: epst[:, 0:1], 256: epst[:, 1:2], 1024: epst[:, 2:3]}
    upgv.append(A_([128, 8, 256], BF16))
    hpi = A_([128, 1], F32)
    lneighth = A_([128, 1], F32)
    MSET(hpi[:], PI / 2, ['hpi'])
    MSET(lneighth[:], math.log(0.125), ['hpi'])
    ident = cst[:, CC_ID:CC_ID + 128]
    cmask = cst[:, CC_MASK:CC_MASK + 128]

    xs_in = [alloc([128, DM], F32, at=r_end - 10 * 2048 + i_ * 4096)[0] for i_ in range(2)]

    def load_x(tts):
        for tt in tts:
            s = xs_in[tt % 2]
            sk = 'xsi%d' % (tt % 2)
            P.dma('sp', sk, s[:], x_d[tt * 128:(tt + 1) * 128, :], writes=[sk])
            for kq in range(2):
                b = 4 + (tt * 2 + kq) % 4
                for kk in range(4):
                    k = kq * 4 + kk
                    TR(ps[:, b, kk * 128:(kk + 1) * 128], s[:, k * 128:(k + 1) * 128], ident,
                       [sk, 'cst'], [PS(b)], last=(kk == 3))
                CP(xT[:, kq * 4:(kq + 1) * 4, tt * 128:(tt + 1) * 128],
                   ps[:, b, :].rearrange("p (k t) -> p k t", k=4),
                   [PS(b)], ['x%d_%d' % (kq * 4 + kk_, tt // 4) for kk_ in range(4)], eng=('act' if kq == 0 else 'dve'))

    def rmsnorm_to_A(l, hf, gcol):
        for sb in range(2):
            t0 = hf * HALF + sb * 512
            sqv = sqbuf[:]
            for k in range(8):
                ACT(sqv[:, k, :], xT[:, k, t0:t0 + 512], AF.Square, ['x%d_%d' % (k, t0 // 512)], ['z%d' % (k // 2)])
            for k in range(8):
                MM(ps[:, 6, :], onesb[:], sqv[:, k, :], k == 0, k == 7, ['onesb', 'z%d' % (k // 2)], [PS(6)], last=(k == 7))
            rsqrt_from_sum(rstd_n, ps[:, 6, :], 1024, [PS(6), 'epst'], ['z4'])
            for k in range(8):
                STT(A[:, k, sb * 512:(sb + 1) * 512], xT[:, k, t0:t0 + 512], gains[:, gcol + k:gcol + k + 1],
                    rstd_n, ALU.mult, ALU.mult, ['x%d_%d' % (k, t0 // 512), 'gains', 'z4'], ['A%d_%d' % (k, sb)])

    sqbuf_t, _ = alloc([128, 8, 512], BF16, at=r_end - 10 * 2048)
    sqbuf = sqbuf_t
    rstd_n = ZT[:, 4, :]
    ktok_t, _ = alloc([128, 8, 2, 128], BF16, at=r_end - 2 * 2048)

    def layer_setup(l):
        P.dma('sp', 'pp', pp[:], pp_d[l], writes=['pp'])
        P.dma('pool', 'wgk', wgk[0:16, :], wgk_d[l], writes=['wgk'])
        for gi, wd in enumerate((rwa_d, rwx_d)):
            src = wd[l].rearrange("(k hl) i j -> hl i k j", hl=2)
            for hl in range(2):
                P.dma('pool', 'rgw', rgw[hl * 64:(hl + 1) * 64, gi, :, :], src[hl], writes=['rgw'])
        P.dma('pool', 'wglu', wglu[:], wglu_d[l].rearrange("(k p) c -> p k c", p=128), writes=['wglu'])
        TS(gains[:, 0:24], pp[:, PP_AN:PP_AN + 24], 32.0, None, ALU.mult, None, ['pp'], ['gains'])
        TS(gains[:, 24:25], pp[:, PP_GN:PP_GN + 1], math.sqrt(128.0), None, ALU.mult, None, ['pp'], ['gains'])
        TS(gains[:, 25:27], pp[:, PP_S5N:PP_S5N + 2], 16.0, None, ALU.mult, None, ['pp'], ['gains'])
        TS(gains[:, 27:29], pp[:, PP_RN:PP_RN + 2], 16.0, None, ALU.mult, None, ['pp'], ['gains'])
        ACT(rgc[:, 0:2], pp[:, PP_RLAM:PP_RLAM + 2], AF.Exp, ['pp'], ['rgc'], scale=-1.0)
        ACT(rgc[:, 0:2], rgc[:, 0:2], AF.Ln, ['rgc'], ['rgc'], bias=1.0)
        TS(rgc[:, 2:4], rgc[:, 0:2], -16.0, None, ALU.mult, None, ['rgc'], ['rgc'])
        TS(rgc[:, 0:2], rgc[:, 0:2], -8.0, None, ALU.mult, None, ['rgc'], ['rgc'])
        TS(rgc[:, 4:6], pp[:, PP_BGK:PP_BGK + 2], -1.0, None, ALU.mult, None, ['pp'], ['rgc'])
        MSET(Sst[:], 0.0, ['Sst'])
        MSET(hlast[:], 0.0, ['hlast'])
        MSET(carry[:], 0.0, ['carry'])
        MSET(S5S[:, :, :, 0:1], 0.0, ['S5S'])
        MSET(Gsl[:], 0.0, ['Gsl'])

    def inproj(l, hf):
        W = win_d[l]

        def load_ct(slot, c0, ncols=128):
            P.dma('pool', 'wct%dL%d' % (slot, l), wct[slot][:, :, 0:ncols],
                  W[:, c0:c0 + ncols].rearrange("(k p) c -> p k c", p=128), writes=['wct%d' % slot])

        tiles = []
        for t in range(2):
            tiles.append((lambda sb, t=t: rx[:, t, 3 + sb * 512:3 + (sb + 1) * 512], 1808 + t * 128, 'rx%d' % t))
        for t in range(2):
            tiles.append((lambda sb, t=t: rgT[:, t, sb * 512:(sb + 1) * 512], 2064 + t * 128, 'rgT%d' % t))
        for t in range(2):
            tiles.append((lambda sb, t=t: qT[:, t, sb * 512:(sb + 1) * 512], 0 + t * 128, 'qT%d' % t))
        for t in range(2):
            tiles.append((lambda sb, t=t: kT[:, t, sb * 512:(sb + 1) * 512], 256 + t * 128, 'kT%d' % t))
        for t in range(4):
            tiles.append((lambda sb, t=t: gT[:, t, sb * 512:(sb + 1) * 512], 1024 + t * 128, 'gT%d' % t))
        for t in range(2):
            tiles.append((lambda sb, t=t: uT[:, t, sb * 512:(sb + 1) * 512], 1552 + t * 128, 'uT%d' % t))
        cnt = 0
        P.dma('pool', 'wvL%d' % l, wv[:], W[:, 512:1024].rearrange("(k p) c -> p k c", p=128), writes=['wv'])
        load_ct(0, tiles[0][1])
        load_ct(1, tiles[1][1])
        for i, (dst, c0, key) in enumerate(tiles):
            slot = i % 3
            if i + 2 < len(tiles):
                load_ct((i + 2) % 3, tiles[i + 2][1])
            elif i + 2 == len(tiles):
                load_ct((i + 2) % 3, 1536, 16)
            for sb in range(2):
                b = cnt % 4
                for k in range(8):
                    MM(ps[:, b, :], wct[slot][:, k, :], A[:, k, sb * 512:(sb + 1) * 512], k == 0, k == 7,
                       ['wct%d' % slot, 'A%d_%d' % (k, sb)], [PS(b)], last=(k == 7))
                CP(dst(sb), ps[:, b, :], [PS(b)], ['%s_%d' % (key, sb)], eng=('act' if cnt % 2 == 0 else 'dve'))
                cnt += 1
            if i == 3:
                rg_pre(l, hf)
                chain = rg_chain(l, hf)
            elif i > 3:
                next(chain, None)
        gs = len(tiles) % 3
        for sb in range(2):
            b = cnt % 4
            for k in range(8):
                MM(ps[0:16, b, :], wct[gs][:, k, 0:16], A[:, k, sb * 512:(sb + 1) * 512], k == 0, k == 7,
                   ['wct%d' % gs, 'A%d_%d' % (k, sb)], [PS(b)], last=(k == 7))
            CP(gkl[0:16, sb * 512:(sb + 1) * 512], ps[0:16, b, :], [PS(b)], ['gkl_%d' % sb], eng='act')
            cnt += 1
            next(chain, None)
        for tb in range(8):
            b = cnt % 4
            sb = tb // 4
            for k in range(8):
                MM(ps[:, b, :], A[:, k, tb * 128:(tb + 1) * 128], wv[:, k, :], k == 0, k == 7,
                   ['wv', 'A%d_%d' % (k, sb)], [PS(b)], last=(k == 7))
            CP(vtok[:, tb, :], ps[:, b, :], [PS(b)], ['vtok%d' % tb], eng=('act' if cnt % 2 == 0 else 'dve'))
            cnt += 1
            next(chain, None)
        for _ in chain:
            pass

    def rg_pre(l, hf):
        for k in range(2):
            CP(rx[:, k, 0:3], rxhist[:, k, :], ['rxhist'], ['rxh'], eng='pool')
        for k in range(2):
            for sb in range(2):
                gk_ = 'rgT%d_%d' % (k, sb)
                ACT(rgT[:, k, sb * 512:(sb + 1) * 512], rgT[:, k, sb * 512:(sb + 1) * 512], AF.Gelu_apprx_tanh, [gk_], [gk_])

    def rg_iter(l, hf, sb, k):
        z = lambda i: ZT[:, i, :]
        lo = sb * 512
        xc, rb, ib, ab, hb = z(0), z(1), z(2), z(3), z(4)
        xcb = ZT[:, 5, 0:256].bitcast(BF16)
        rxk = ['rx%d_%d' % (k, sb), 'rxh'] + (['rx%d_%d' % (k, sb - 1)] if sb else [])
        w = lambda tap: pp[:, PP_RCW + k * 4 + tap:PP_RCW + k * 4 + tap + 1]
        TS(xc, rx[:, k, 3 + lo:3 + lo + 512], w(3), pp[:, PP_RCB + k:PP_RCB + k + 1], ALU.mult, ALU.add,
           rxk + ['pp'], ['z0'])
        for tap in range(3):
            STT(xc, rx[:, k, tap + lo:tap + lo + 512], w(tap), xc, ALU.mult, ALU.add, rxk + ['pp', 'z0'], ['z0'])
        CP(xcb, xc, ['z0'], ['z5'], eng='act')
        yield
        for gi in range(2):
            b = 4 + gi
            for hl in range(2):
                MM(ps[hl * 64:(hl + 1) * 64, b, :], rgw[hl * 64:(hl + 1) * 64, gi, k, :],
                   xcb[hl * 64:(hl + 1) * 64, :], True, True, ['rgw', 'z5'], [PS(b)], last=(hl == 1))
        yield
        for gi, (dst, bcol) in enumerate(((rb, PP_RBA), (ib, PP_RBX))):
            b = 4 + gi
            ACT(dst, ps[:, b, :], AF.Sigmoid, [PS(b), 'pp'], ['z%d' % (1 + gi)], bias=pp[:, bcol + k:bcol + k + 1])
        ACT(ab, rb, AF.Exp, ['z1', 'rgc'], ['z3'], scale=rgc[:, k:k + 1])
        ACT(rb, rb, AF.Exp, ['z1', 'rgc'], ['z1'], scale=rgc[:, 2 + k:3 + k])
        TS(rb, rb, 0.999999, None, ALU.min, None, ['z1'], ['z1'])
        ACT(rb, rb, AF.Ln, ['z1'], ['z1'], scale=-1.0, bias=1.0)
        ACT(rb, rb, AF.Exp, ['z1'], ['z1'], scale=0.5)
        yield
        TT(ib, ib, xc, ALU.mult, ['z2', 'z0'], ['z2'])
        TT(ib, ib, rb, ALU.mult, ['z2', 'z1'], ['z2'])
        SCAN(hb, ab, ib, hlast[:, k:k + 1], ['z3', 'z2', 'hlast'], ['z4'])
        CP(hlast[:, k:k + 1], hb[:, 511:512], ['z4'], ['hlast'], eng='pool')
        TT(ZT[:, 6 + k, :], hb, rgT[:, k, lo:lo + 512], ALU.mult, ['z4', 'rgT%d_%d' % (k, sb)], ['z%d' % (6 + k)])
        ACT(ZT[:, 8, k * 256:(k + 1) * 256].bitcast(BF16), ZT[:, 6 + k, :], AF.Square, ['z%d' % (6 + k)], ['z8'])
        yield

    def rg_chain(l, hf):
        for sb in range(2):
            for k in range(2):
                yield from rg_iter(l, hf, sb, k)
            rg_norm(l, hf, sb)
            yield

    def rg_norm(l, hf, sb):
        lo = sb * 512
        sqv = ZT[:, 8, :].bitcast(BF16)
        for k in range(2):
            MM(ps[:, 6, :], onesb[:], sqv[:, k * 512:(k + 1) * 512], k == 0, k == 1, ['onesb', 'z8'], [PS(6)], last=(k == 1))
        rsqrt_from_sum(ZT[:, 9, :], ps[:, 6, :], 256, [PS(6), 'epst'], ['z9'])
        for k in range(2):
            STT(yrg[:, k, lo:lo + 512], ZT[:, 6 + k, :], gains[:, 27 + k:28 + k], ZT[:, 9, :], ALU.mult, ALU.mult,
                ['z%d' % (6 + k), 'gains', 'z9'], ['yrg%d_%d' % (k, sb)])

    def rg_post(l, hf):
        for k in range(2):
            CP(rxhist[:, k, :], rx[:, k, HALF:HALF + 3], ['rx%d_1' % k], ['rxhist'], eng='pool')
            for sb in range(2):
                CP(A[:, 6 + k, sb * 512:(sb + 1) * 512], yrg[:, k, sb * 512:(sb + 1) * 512], ['yrg%d_%d' % (k, sb)],
                   ['A%d_%d' % (6 + k, sb)], eng='pool')


    TWO_PI = 2.0 * PI

    def bcl(ap, n):
        return ap.unsqueeze(2).broadcast_to([ap.shape[0], ap.shape[1], n])

    def sincos(X, sn, cs, wf, wi, r, w):
        TS(wi, X, 1.0 / TWO_PI, None, ALU.mult, None, r, w)
        CP(wf, wi, w, w)
        STT(wf, wf, -TWO_PI, X, ALU.mult, ALU.add, r + w, w)
        TS(wf, wf, -3.14159, 3.14159, ALU.max, ALU.min, w, w)
        ACT(sn, wf, AF.Sin, w, w)
        STT(wf, wf, -1.0, wf, ALU.mult, ALU.max, w, w)
        ACT(cs, wf, AF.Sin, w, w, scale=-1.0, bias=hpi[:])

    def kappa(P1r, P1i, lre, lim, nr, den, kr, ki, wa, r, w):
        TS(nr, P1r, -1.0, None, ALU.add, None, r, w)
        TT(den, lre, lre, ALU.mult, r, w)
        TT(wa, lim, lim, ALU.mult, r, w)
        TT(den, den, wa, ALU.add, w, w)
        P.op('dve', lambda e: e.reciprocal(out=den, in_=den), w, w)
        TT(kr, nr, lre, ALU.mult, r + w, w)
        TT(wa, P1i, lim, ALU.mult, r + w, w)
        TT(kr, kr, wa, ALU.add, w, w)
        TT(kr, kr, den, ALU.mult, w, w)
        TT(ki, P1i, lre, ALU.mult, r + w, w)
        TT(wa, nr, lim, ALU.mult, r + w, w)
        TT(ki, ki, wa, ALU.subtract, w, w)
        TT(ki, ki, den, ALU.mult, w, w)

    def s5_setup(l, hooks=()):
        hooks = list(hooks)

        def hook():
            if hooks:
                hooks.pop(0)()
        T = st_
        hook()
        r_ = ['pp', 'cst']
        w_ = ['s5t']
        idx9 = cst[:, CC_IDX9:CC_IDX9 + 9]
        idx8 = cst[:, CC_IDX9:CC_IDX9 + 8]
        lre, lim = pp[:, PP_LRE:PP_LRE + 8], pp[:, PP_LIM:PP_LIM + 8]
        ACT(T['dt'][:], pp[:, PP_LDT:PP_LDT + 8], AF.Exp, r_, w_)
        TT(T['ar'][:], lre, T['dt'][:], ALU.mult, r_ + w_, w_)
        TT(T['th'][:], lim, T['dt'][:], ALU.mult, r_ + w_, w_)
        i9b = idx9.unsqueeze(1).broadcast_to([128, 8, 9])
        TT(T['tA'][:], bcl(T['ar'][:], 9), i9b, ALU.mult, r_ + w_, w_)
        TT(T['tB'][:], bcl(T['th'][:], 9), i9b, ALU.mult, r_ + w_, w_)
        ACT(T['tA'][:], T['tA'][:], AF.Exp, w_, w_)
        sincos(T['tB'][:], T['sn9'][:], T['cs9'][:], T['w9a'][:], T['w9i'][:], w_, w_)
        TT(T['Pr'][:], T['tA'][:], T['cs9'][:], ALU.mult, w_, w_)
        TT(T['Pi'][:], T['tA'][:], T['sn9'][:], ALU.mult, w_, w_)
        hook()
        kappa(T['Pr'][:, :, 1], T['Pi'][:, :, 1], lre, lim, T['nr'][:], T['den'][:], T['kr'][:], T['ki'][:], T['w8a'][:], r_ + w_, w_)
        Bre = pp[:, PP_BRE:PP_BRE + 128].rearrange("p (a i) -> p a i", i=16)
        Bim = pp[:, PP_BIM:PP_BIM + 128].rearrange("p (a i) -> p a i", i=16)
        krb, kib = bcl(T['kr'][:], 16), bcl(T['ki'][:], 16)
        TT(T['Bbr'][:], krb, Bre, ALU.mult, r_ + w_, w_)
        TT(T['w16a'][:], kib, Bim, ALU.mult, r_ + w_, w_)
        TT(T['Bbr'][:], T['Bbr'][:], T['w16a'][:], ALU.subtract, w_, w_)
        TT(T['Bbi'][:], krb, Bim, ALU.mult, r_ + w_, w_)
        TT(T['w16a'][:], kib, Bre, ALU.mult, r_ + w_, w_)
        TT(T['Bbi'][:], T['Bbi'][:], T['w16a'][:], ALU.add, w_, w_)
        MSET(T['Bw'][:], 0.0, w_, eng='pool')
        hook()
        for gl in range(2):
            for ri, Bb in enumerate((T['Bbr'], T['Bbi'])):
                for k in range(2):
                    dst = bass.AP(T['Bw'], (gl * 64) * 2048 + (4 * k) * 256 + ri * 128 + gl * 16,
                                  [[2048, 64], [256 + 32, 4], [1, 16]])
                    CP(dst, Bb[gl * 64:(gl + 1) * 64, 4 * k:4 * k + 4, :], w_, w_)
        Cre = pp[:, PP_CRE:PP_CRE + 128].rearrange("p (a j) -> p a j", j=16)
        Cim = pp[:, PP_CIM:PP_CIM + 128].rearrange("p (a j) -> p a j", j=16)
        Cb = lambda C: C.unsqueeze(2).broadcast_to([128, 8, 9, 16])
        Pb = lambda Pt: Pt[:].unsqueeze(3).broadcast_to([128, 8, 9, 16])
        TT(T['t1'][:], Cb(Cre), Pb(T['Pr']), ALU.mult, r_ + w_, w_)
        TT(T['t2'][:], Cb(Cim), Pb(T['Pi']), ALU.mult, r_ + w_, w_)
        TT(T['t1'][:], T['t1'][:], T['t2'][:], ALU.subtract, w_, w_)
        for gl in range(2):
            CP(E5[gl * 64:(gl + 1) * 64, :, :, 0, gl * 16:(gl + 1) * 16], T['t1'][gl * 64:(gl + 1) * 64], w_, ['E5'])
        TT(T['t1'][:], Cb(Cre), Pb(T['Pi']), ALU.mult, r_ + w_, w_)
        TT(T['t2'][:], Cb(Cim), Pb(T['Pr']), ALU.mult, r_ + w_, w_)
        TT(T['t1'][:], T['t1'][:], T['t2'][:], ALU.add, w_, w_)
        for gl in range(2):
            TS(E5[gl * 64:(gl + 1) * 64, :, :, 1, gl * 16:(gl + 1) * 16], T['t1'][gl * 64:(gl + 1) * 64], -1.0, None,
               ALU.mult, None, w_, ['E5'])
        hook()
        while hooks:
            hook()
        for k in range(2):
            for a in range(4):
                pr = 4 * k + a
                for tau in range(8):
                    out = ps[:, 2 * k + tau // 4, (tau % 4) * 128 + 32 * a:(tau % 4) * 128 + 32 * a + 32]
                    MM(out, T['Bw'][:, pr, 0, :], E5[:, pr, tau, 0, :], True, False, ['s5t', 'E5'],
                       [PS(2 * k + tau // 4)], last=False, sgc=True)
                    MM(out, T['Bw'][:, pr, 1, :], E5[:, pr, tau, 1, :], False, True, [], [], last=(tau == 7), sgc=True)
            CP(KT[:, k, :, :], ps[:, 2 * k:2 * k + 2, :].rearrange("p b (t c) -> p (b t) c", c=128),
               [PS(2 * k), PS(2 * k + 1)], ['KT'], eng='act')
        qo = lambda ap: ap.rearrange("p (k a) -> p a k", k=2)
        ACT(s5sm[:, 0:8].rearrange("p (a k) -> p a k", k=2), qo(T['ar'][:]), AF.Exp, w_, ['s5sm'], scale=8.0)
        TS(T['w8i'][:], T['th'][:], 8.0 / TWO_PI, None, ALU.mult, None, w_, w_)
        CP(T['w8a'][:], T['w8i'][:], w_, w_)
        TS(T['w8b'][:], T['th'][:], 8.0, None, ALU.mult, None, w_, w_)
        STT(s5sm[:, 8:16].rearrange("p (a k) -> p a k", k=2), qo(T['w8a'][:]), -TWO_PI, qo(T['w8b'][:]), ALU.mult, ALU.add, w_, ['s5sm'])
        wT_ = ['s5tT']
        v3 = lambda c0: pp[:, c0:c0 + 128].rearrange("p (k q) -> p k q", q=64)
        lreT, limT = v3(PP_LRET), v3(PP_LIMT)
        ACT(T['dtT'][:], v3(PP_LDTT), AF.Exp, r_, wT_)
        TT(T['arT'][:], lreT, T['dtT'][:], ALU.mult, r_ + wT_, wT_)
        TT(T['thT'][:], limT, T['dtT'][:], ALU.mult, r_ + wT_, wT_)
        i8b = idx8.unsqueeze(1).unsqueeze(3).broadcast_to([128, 2, 8, 64])
        eb = lambda t: t.unsqueeze(2).broadcast_to([128, 2, 8, 64])
        TT(T['angT'][:], eb(T['thT'][:]), i8b, ALU.mult, r_ + wT_, wT_)
        TT(T['mgT'][:], eb(T['arT'][:]), i8b, ALU.mult, r_ + wT_, wT_)
        ACT(T['mgT'][:], T['mgT'][:], AF.Exp, wT_, wT_)
        sincos(T['angT'][:], T['snT'][:], T['csT'][:], T['wkf'][:], T['wki'][:], wT_, wT_)
        TT(T['csT'][:], T['csT'][:], T['mgT'][:], ALU.mult, wT_, wT_)
        TT(T['snT'][:], T['snT'][:], T['mgT'][:], ALU.mult, wT_, wT_)
        kappa(T['csT'][:, :, 1, :], T['snT'][:, :, 1, :], lreT, limT, T['nrT'][:], T['denT'][:], T['krT'][:], T['kiT'][:],
              T['wTa'][:], r_ + wT_, wT_)
        BreT, BimT = v3(PP_BRET), v3(PP_BIMT)
        TT(T['BbrT'][:], T['krT'][:], BreT, ALU.mult, r_ + wT_, wT_)
        TT(T['wTa'][:], T['kiT'][:], BimT, ALU.mult, r_ + wT_, wT_)
        TT(T['BbrT'][:], T['BbrT'][:], T['wTa'][:], ALU.subtract, wT_, wT_)
        TT(T['BbiT'][:], T['krT'][:], BimT, ALU.mult, r_ + wT_, wT_)
        TT(T['wTa'][:], T['kiT'][:], BreT, ALU.mult, r_ + wT_, wT_)
        TT(T['BbiT'][:], T['BbiT'][:], T['wTa'][:], ALU.add, wT_, wT_)
        TT(T['WHr'][:], T['csT'][:], eb(T['BbrT'][:]), ALU.mult, wT_, wT_)
        TT(T['wk2'][:], T['snT'][:], eb(T['BbiT'][:]), ALU.mult, wT_, wT_)
        TT(T['WHr'][:], T['WHr'][:], T['wk2'][:], ALU.subtract, wT_, wT_)
        TT(T['WHi'][:], T['csT'][:], eb(T['BbiT'][:]), ALU.mult, wT_, wT_)
        TT(T['wk2'][:], T['snT'][:], eb(T['BbrT'][:]), ALU.mult, wT_, wT_)
        TT(T['WHi'][:], T['WHi'][:], T['wk2'][:], ALU.add, wT_, wT_)
        mT = cst[:, CC_MASKT:CC_MASKT + 2].unsqueeze(1).unsqueeze(3).broadcast_to([128, 8, 2, 64])
        for k in range(2):
            for ri, Wx in enumerate((T['WHr'], T['WHi'])):
                out = WH[:, k, :, ri, :].rearrange("p e (g q) -> p e g q", g=2)
                TT(out, Wx[:, k, :, :].unsqueeze(2).broadcast_to([128, 8, 2, 64]), mT, ALU.mult, r_ + wT_, ['WH'])

    def s5_mixer(l, hf):
        c0 = hf * 128
        v8 = lambda ap: ap.rearrange("p (a c) -> p a c", c=128)
        Tc = ZT[:, 0:2, :].rearrange("p a (b c) -> p (a b) c", c=128)
        Ts = ZT[:, 2:4, :].rearrange("p a (b c) -> p (a b) c", c=128)
        Gr = ZT[:, 4:6, :].rearrange("p a (b c) -> p (a b) c", c=128)
        Gi = ZT[:, 6:8, :].rearrange("p a (b c) -> p (a b) c", c=128)
        Gsi = ZT[:, 8:10, :].rearrange("p a (b c) -> p (a b) c", c=128)
        tmp = v8(rx[:, 0, 3:3 + HALF])
        Gsr = v8(rx[:, 1, 3:3 + HALF])
        wi = rgT[:].rearrange("p a t -> p (a t)").bitcast(I32).rearrange("p (a c) -> p a c", c=128)
        kTc, kTs, kGr, kGi, kGsi = ['z0', 'z1'], ['z2', 'z3'], ['z4', 'z5'], ['z6', 'z7'], ['z8', 'z9']
        ktmp, kGsr = ['rx0_0', 'rx0_1'], ['rx1_0', 'rx1_1']
        kwi = ['rgT0_0', 'rgT0_1', 'rgT1_0', 'rgT1_1']
        cidx = cst[:, CC_CIDX + c0:CC_CIDX + c0 + 128].unsqueeze(1).broadcast_to([128, 8, 128])
        TT(Tc, bcl(s5sm[:, 8:16], 128), cidx, ALU.mult, ['s5sm', 'cst'], kTc)
        TS(wi, Tc, 1.0 / TWO_PI, None, ALU.mult, None, kTc, kwi)
        CP(Gr, wi, kwi, kGr)
        STT(Gr, Gr, -TWO_PI, Tc, ALU.mult, ALU.add, kGr + kTc, kGr)
        TS(Gr, Gr, -3.14159, 3.14159, ALU.max, ALU.min, kGr, kGr)
        ACT(Ts, Gr, AF.Sin, kGr, kTs)
        STT(Gr, Gr, -1.0, Gr, ALU.mult, ALU.max, kGr, kGr)
        ACT(Tc, Gr, AF.Sin, kGr, kTc, scale=-1.0, bias=hpi[:])
        for k in range(2):
            uv = uT[:, k, :].rearrange("p (c s) -> p s c", s=8)
            for a in range(4):
                pr = 4 * k + a
                for ri in range(2):
                    off = k * 256 + ri * 128
                    for e in range(8):
                        MM(ps[:, a, off:off + 128], WH[32 * a:32 * a + 32, k, e, ri, :], uv[32 * a:32 * a + 32, 7 - e, :],
                           e == 0, e == 7, ['WH', 'uT%d_0' % k, 'uT%d_1' % k], [PS(a)], last=(e == 7),
                           tp=(32 * a, 0), sgc=True)
        import os
        cut = int(os.environ.get('S5CUT', '99'))
        if cut <= 0:
            return
        hv = ps[:, 0:4, :].rearrange("p b (q r c) -> p (b q) r c", r=2, c=128)
        hr, hi = hv[:, :, 0, :], hv[:, :, 1, :]
        kh = [PS(0), PS(1), PS(2), PS(3)]
        TT(tmp, hr, Tc, ALU.mult, kh + kTc, ktmp)
        TT(Gr, hi, Ts, ALU.mult, kh + kTs, kGr)
        TT(Gr, Gr, tmp, ALU.add, kGr + ktmp, kGr)
        TT(tmp, hr, Ts, ALU.mult, kh + kTs, ktmp)
        TT(Gi, hi, Tc, ALU.mult, kh + kTc, kGi)
        TT(Gi, Gi, tmp, ALU.subtract, kGi + ktmp, kGi)
        for pr in range(8):
            d0 = s5sm[:, pr:pr + 1].broadcast_to([128, 128])
            SCAN(Gsr[:, pr, :], d0, Gr[:, pr, :], Gsl[:, pr, 0:1], kGr + ['s5sm', 'Gsl'], kGsr)
            SCAN(Gsi[:, pr, :], d0, Gi[:, pr, :], Gsl[:, pr, 1:2], kGi + ['s5sm', 'Gsl'], kGsi)
        CP(Gsl[:, :, 0], Gsr[:, :, 127], kGsr, ['Gsl'], eng='pool')
        CP(Gsl[:, :, 1], Gsi[:, :, 127], kGsi, ['Gsl'], eng='pool')
        TT(tmp, Gsr, Tc, ALU.mult, kGsr + kTc, ktmp)
        TT(Gr, Gsi, Ts, ALU.mult, kGsi + kTs, kGr)
        TT(S5S[:, :, 0, 1:129], tmp, Gr, ALU.subtract, ktmp + kGr, ['S5S'])
        TT(tmp, Gsr, Ts, ALU.mult, kGsr + kTs, ktmp)
        TT(Gr, Gsi, Tc, ALU.mult, kGsi + kTc, kGr)
        TT(S5S[:, :, 1, 1:129], tmp, Gr, ALU.add, ktmp + kGr, ['S5S'])
        if cut <= 1:
            return
        ybank = {0: (4, 5), 1: (0, 1)}
        for k in range(2):
            uv = uT[:, k, :].rearrange("p (c s) -> p s c", s=8)
            bks = ybank[k]
            for tau in range(8):
                for b2 in range(2):
                    t_lo, t_hi = max(tau, 4 * b2), 4 * b2 + 4
                    if t_lo >= t_hi:
                        continue
                    out = ps[:, bks[b2], (t_lo - 4 * b2) * 128:(t_hi - 4 * b2) * 128]
                    MM(out, KT[:, k, tau, :], uv[:, t_lo - tau:t_hi - tau, :], tau == 0, False,
                       ['KT', 'uT%d_0' % k, 'uT%d_1' % k], [PS(bks[b2])], last=False, sgc=True)
            for a in range(4):
                pr = 4 * k + a
                for t in range(8):
                    for ri in range(2):
                        lastmm = (a == 3 and t == 7 and ri == 1)
                        out = ps[32 * a:32 * a + 32, bks[t // 4], (t % 4) * 128:(t % 4) * 128 + 128]
                        MM(out, E5[:, pr, t + 1, ri, :], S5S[:, 2 * a + k, ri, 0:128], False, lastmm,
                           ['E5', 'S5S'], [PS(bks[0]), PS(bks[1])], last=lastmm, tp=(0, 32 * a), sgc=True)
        if cut <= 2:
            return
        CP(S5S[:, :, :, 0:1], S5S[:, :, :, 128:129], ['S5S'], ['S5S'], eng='pool')
        for sb in range(2):
            lo = sb * 512
            y2b = ZT[:, 6, :].bitcast(BF16).rearrange("p (k t) -> p k t", k=2)
            for k in range(2):
                bks = ybank[k]
                yv = ps[:, bks[0]:bks[0] + 2, :].rearrange("p b (t c) -> p (b t) c", c=128)[:, :, 64 * sb:64 * sb + 64]
                yv = yv.rearrange("p t c -> p c t")
                y1 = ZT[:, 4 + k, :]
                STT(y1.rearrange("p (c t) -> p c t", t=8), uT[:, k, lo:lo + 512].rearrange("p (c t) -> p c t", t=8),
                    pp[:, PP_S5D + k:PP_S5D + k + 1], yv, ALU.mult, ALU.add,
                    ['uT%d_%d' % (k, sb), 'pp', PS(bks[0]), PS(bks[1])], ['z%d' % (4 + k)])
                ACT(y1, y1, AF.Gelu_apprx_tanh, ['z%d' % (4 + k)], ['z%d' % (4 + k)])
                CP(y2b[:, k, :], y1, ['z%d' % (4 + k)], ['z6'], eng='pool')
            for kk in range(2):
                for k in range(2):
                    MM(ps[:, 2 + kk, :], wglu[:, k, kk * 128:(kk + 1) * 128], y2b[:, k, :], k == 0, k == 1, ['wglu', 'z6'],
                       [PS(2 + kk)], last=(k == 1))
                ACT(ZT[:, 7, :], ps[:, 2 + kk, :], AF.Sigmoid, [PS(2 + kk), 'pp'], ['z7'], bias=pp[:, PP_BGLU + kk:PP_BGLU + kk + 1])
                TT(ZT[:, 4 + kk, :], ZT[:, 4 + kk, :], ZT[:, 7, :], ALU.mult, ['z%d' % (4 + kk), 'z7'], ['z%d' % (4 + kk)])
                ACT(ZT[:, 8, kk * 256:(kk + 1) * 256].bitcast(BF16), ZT[:, 4 + kk, :], AF.Square, ['z%d' % (4 + kk)], ['z8'])
            sqv = ZT[:, 8, :].bitcast(BF16)
            for kk in range(2):
                MM(ps[:, 6, :], onesb[:], sqv[:, kk * 512:(kk + 1) * 512], kk == 0, kk == 1, ['onesb', 'z8'], [PS(6)], last=(kk == 1))
            rsqrt_from_sum(ZT[:, 9, :], ps[:, 6, :], 256, [PS(6), 'epst'], ['z9'])
            for kk in range(2):
                STT(A[:, 4 + kk, lo:lo + 512], ZT[:, 4 + kk, :], gains[:, 25 + kk:26 + kk], ZT[:, 9, :], ALU.mult, ALU.mult,
                    ['z%d' % (4 + kk), 'gains', 'z9'], ['A%d_%d' % (4 + kk, sb)])

    def gla_mixer(l, hf):
        Lb = ZT[:, 0:4, :].rearrange("p (a b) t -> p a (b t)", a=2)
        kL = [['z0', 'z1'], ['z2', 'z3']]
        kq = lambda p2: ['qT%d_0' % p2, 'qT%d_1' % p2]
        kk_ = lambda p2: ['kT%d_0' % p2, 'kT%d_1' % p2]
        ktok = ktok_t
        PT = uT[:, 0, :].rearrange("p (s c) -> p s c", c=128)
        Sbf = uT[:, 1, :].rearrange("p (s q c) -> p s q c", q=2, c=128)
        osq = rgT[:].rearrange("p a (s t) -> p (a s) t", t=512)
        cnt = 0
        for h in range(4):
            for sb in range(2):
                gk_ = 'gT%d_%d' % (h, sb)
                ACT(gT[:, h, sb * 512:(sb + 1) * 512], gT[:, h, sb * 512:(sb + 1) * 512], AF.Silu, [gk_], [gk_])
        for p2 in range(2):
            for sb in range(2):
                b = cnt % 4
                cnt += 1
                MM(ps[:, b, :], wgk[0:16, p2 * 128:(p2 + 1) * 128], gkl[0:16, sb * 512:(sb + 1) * 512], True, True,
                   ['wgk', 'gkl_%d' % sb], [PS(b)])
                Lv = Lb[:, p2, sb * 512:(sb + 1) * 512]
                ACT(Lv, ps[:, b, :], AF.Exp, [PS(b), 'rgc'], [kL[p2][sb]], scale=-1.0, bias=rgc[:, 4 + p2:5 + p2])
                ACT(Lv, Lv, AF.Ln, [kL[p2][sb]], [kL[p2][sb]], bias=1.0)
            SCAN(Lb[:, p2, :], segb[:], Lb[:, p2, :], 0.0, kL[p2] + ['segb'], kL[p2])
            Lc = Lb[:, p2, :].rearrange("p (n c) -> p n c", c=64)
            Lmid, Llast = Lc[:, :, 31], Lc[:, :, 63]
            ACT(gsc[:, 0, p2, :], Llast, AF.Exp, kL[p2], ['gsc'], scale=-1.0 / 16)
            ACT(gsc[:, 2, p2, :], Lmid, AF.Exp, kL[p2], ['gsc'], scale=-1.0 / 16)
            TT(gsc[:, 1, p2, :], Llast, Lmid, ALU.subtract, kL[p2], ['gsc'])
            ACT(gsc[:, 1, p2, :], gsc[:, 1, p2, :], AF.Exp, ['gsc'], ['gsc'], scale=-1.0 / 16)
            import os
            gcut = int(os.environ.get('GLACUT', '99'))
            if gcut <= 0:
                continue
            D1 = rx[:, p2, 3:3 + HALF]
            kD = ['rx%d_0' % p2, 'rx%d_1' % p2]
            TT(D1.rearrange("p (n c) -> p n c", c=64), Lc, Lmid.unsqueeze(2).broadcast_to([128, 16, 64]), ALU.subtract, kL[p2], kD)
            eq = ZT[:, 4:6, :].rearrange("p a t -> p (a t)")
            ek = ZT[:, 6:8, :].rearrange("p a t -> p (a t)")
            ACT(eq, D1, AF.Exp, kD, ['z4', 'z5'], scale=-1.0 / 16, bias=lneighth[:])
            ACT(ek, D1, AF.Exp, kD, ['z6', 'z7'], scale=1.0 / 16)
            TT(qT[:, p2, :], qT[:, p2, :], eq, ALU.mult, kq(p2) + ['z4', 'z5'], kq(p2))
            TT(ek, kT[:, p2, :], ek, ALU.mult, kk_(p2) + ['z6', 'z7'], ['z6', 'z7'])
            CP(kT[:, p2, :], ek, ['z6', 'z7'], kk_(p2), eng='pool')
            if gcut <= 1:
                continue
            for g4 in range(2):
                bk = 4 + g4
                tks = ['scb%d' % g4]
                for i_ in range(4):
                    blk = 4 * g4 + i_
                    TR(ps[:, bk, i_ * 128:(i_ + 1) * 128], ek[:, blk * 128:(blk + 1) * 128], ident, ['z6', 'z7', 'cst'], tks,
                       last=(i_ == 3))
                CP(ktok[:, 4 * g4:4 * g4 + 4, p2, :], ps[:, bk, :].rearrange("p (b c) -> p b c", c=128), tks, ['z8', 'z9'],
                   eng=('act' if g4 else 'dve'))
        scn = 0
        if gcut <= 2:
            return
        for sbk in range(2):
            for blk in range(4):
                bg = 4 * sbk + blk
                for h in range(4):
                    p2, hl = h // 2, h % 2
                    hs = slice(hl * 64, (hl + 1) * 64)
                    MM(ps[:, 4 + hl, p2 * 128:(p2 + 1) * 128], kT[hs, p2, bg * 128:(bg + 1) * 128], qT[hs, p2, bg * 128:(bg + 1) * 128],
                       True, True, kk_(p2) + kq(p2), ['scb%d' % hl], last=(h == 3))
                pbase = (bg % 2) * 4
                for hl in range(2):
                    TT(PT[:, pbase + hl:pbase + hl + 3:2, :], ps[:, 4 + hl, 0:256].rearrange("p (q c) -> p q c", c=128),
                       cmask.unsqueeze(1).broadcast_to([128, 2, 128]), ALU.mult, ['scb%d' % hl, 'cst'],
                       ['PT%d' % (pbase + hl), 'PT%d' % (pbase + hl + 2)])
                for h in range(4):
                    pl = pbase + h
                    MM(ps[:, h, blk * 128:(blk + 1) * 128], vtok[:, bg, h * 128:(h + 1) * 128], PT[:, pl, :], blk == 0, False,
                       ['vtok%d' % bg, 'PT%d' % pl], [PS(h)], last=(h == 3), sgc=True)
            if gcut <= 3:
                return
            for n in range(8):
                ng = 8 * sbk + n
                bg, hh = ng // 2, ng % 2
                ts_ = slice(hh * 64, (hh + 1) * 64)
                ssl = ng % 4
                kvs = ng % 2
                for p2 in range(2):
                    for hl in range(2):
                        h = 2 * p2 + hl
                        MM(ps[hl * 64:(hl + 1) * 64, 6 + kvs, p2 * 128:(p2 + 1) * 128],
                           ktok[ts_, bg, p2, hl * 64:(hl + 1) * 64], vtok[ts_, bg, h * 128:(h + 1) * 128], True, True,
                           ['z8', 'z9', 'vtok%d' % bg], ['kv%d' % kvs], last=(p2 == 1 and hl == 1))
                TT(Sbf[:, ssl, :, :], Sst[:], gsc[:, 2, :, ng].unsqueeze(2).broadcast_to([128, 2, 128]), ALU.mult,
                   ['Sst', 'gsc'], ['Sbf%d' % ssl])
                for h in range(4):
                    p2, hl = h // 2, h % 2
                    hs = slice(hl * 64, (hl + 1) * 64)
                    tcol = (4 * sbk) * 128 + n * 64
                    MM(ps[:, h, n * 64:(n + 1) * 64], Sbf[hs, ssl, p2, :], qT[hs, p2, tcol:tcol + 64], False, n == 7,
                       ['Sbf%d' % ssl] + kq(p2), [PS(h)], last=(h == 3), sgc=True)
                kvv = ps[:, 6 + kvs, 0:256].rearrange("p (q c) -> p q c", c=128)
                tkv = ZT[:, 4, 0:256].rearrange("p (q c) -> p q c", c=128)
                TT(tkv, kvv, gsc[:, 1, :, ng].unsqueeze(2).broadcast_to([128, 2, 128]), ALU.mult, ['kv%d' % kvs, 'gsc'], ['z4'])
                TT(Sst[:], Sst[:], gsc[:, 0, :, ng].unsqueeze(2).broadcast_to([128, 2, 128]), ALU.mult, ['Sst', 'gsc'], ['Sst'])
                TT(Sst[:], Sst[:], tkv, ALU.add, ['Sst', 'z4'], ['Sst'])
            if gcut <= 4:
                return
            lo = sbk * 512
            for h in range(4):
                oq = osq[:, h % 4, :]
                ACT(oq, ps[:, h, :], AF.Square, [PS(h)], ['rgT%d_%d' % (h // 2, h % 2)])
                MM(ps[:, 4 + h % 2, :], onesb[:], oq, True, True, ['onesb', 'rgT%d_%d' % (h // 2, h % 2)], ['scb%d' % (h % 2)])
                za, zb = (5, 6) if h % 2 == 0 else (7, 0)
                rsqrt_from_sum(ZT[:, za, :], ps[:, 4 + h % 2, :], 128, ['scb%d' % (h % 2), 'epst'], ['z%d' % za])
                STT(ZT[:, zb, :], ps[:, h, :], gains[:, 24:25], ZT[:, za, :], ALU.mult, ALU.mult, [PS(h), 'gains', 'z%d' % za], ['z%d' % zb])
                TT(A[:, h, lo:lo + 512], ZT[:, zb, :], gT[:, h, lo:lo + 512], ALU.mult, ['z%d' % zb, 'gT%d_%d' % (h, sbk)],
                   ['A%d_%d' % (h, sbk)])

    def outproj(l, hf):
        W = wout_d[l]
        cnt = 0

        def ld(i):
            P.dma('pool', 'wct%dL%d' % (i % 3, l), wct[i % 3][:], W[:, i * 128:(i + 1) * 128].rearrange("(k p) c -> p k c", p=128),
                  writes=['wct%d' % (i % 3)])
        ld(0)
        ld(1)
        for i in range(8):
            slot = i % 3
            if i + 2 < 8:
                ld(i + 2)
            for sb in range(2):
                b = cnt % 4
                cnt += 1
                t0 = hf * HALF + sb * 512
                for k in range(8):
                    MM(ps[:, b, :], wct[slot][:, k, :], A[:, k, sb * 512:(sb + 1) * 512], k == 0, k == 7,
                       ['wct%d' % slot, 'A%d_%d' % (k, sb)], [PS(b)], last=(k == 7))
                xk = 'x%d_%d' % (i, t0 // 512)
                TT(xT[:, i, t0:t0 + 512], xT[:, i, t0:t0 + 512], ps[:, b, :], ALU.add, [xk, PS(b)], [xk])

    def ffn_prefetch(l, hf):
        Wu = wup_d[l]
        P.dma('pool', 'upgv2L%d' % l, upgv[2][:, :, 0:128], Wu[:, 0:128].rearrange("(k p) c -> p k c", p=128), writes=['upgv2'])
        P.dma('pool', 'upgv2L%d' % l, upgv[2][:, :, 128:256], Wu[:, DFF:DFF + 128].rearrange("(k p) c -> p k c", p=128), writes=['upgv2'])

    def ffn(l, hf):
        Wu, Wd = wup_d[l], wdn_d[l]
        cnt = 0
        def ldu(j):
            sk_ = 'upgv%d' % ((j + 2) % 3)
            P.dma('pool', sk_ + 'L%d' % l, upgv[(j + 2) % 3][:, :, 0:128], Wu[:, j * 128:(j + 1) * 128].rearrange("(k p) c -> p k c", p=128), writes=[sk_])
            P.dma('pool', sk_ + 'L%d' % l, upgv[(j + 2) % 3][:, :, 128:256],
                  Wu[:, DFF + j * 128:DFF + (j + 1) * 128].rearrange("(k p) c -> p k c", p=128), writes=[sk_])

        def ldd(i):
            P.dma('pool', 'wdn%dL%d' % (i % 2, l), wdn[i % 2][:], Wd[:, i * 128:(i + 1) * 128].rearrange("(j p) c -> p j c", p=128),
                  writes=['wdn%d' % (i % 2)])
        ldu(1)
        pend = []
        import os
        FFN_TS_ENG = 'dve'
        for j in range(NJ):
            slot = (j + 2) % 3
            sk = 'upgv%d' % slot
            if j + 2 < NJ:
                ldu(j + 2)
            elif j + 2 == NJ:
                ldd(0)
            elif j + 1 == NJ:
                ldd(1)
            for sb in range(2):
                q = cnt % 3
                qp = (cnt - 1) % 3
                cnt += 1
                bu, bg_ = 2 * q, 2 * q + 1
                for k in range(8):
                    MM(ps[:, bu, :], upgv[slot][:, k, 0:128], A[:, k, sb * 512:(sb + 1) * 512], k == 0, k == 7,
                       [sk, 'A%d_%d' % (k, sb)], [PS(bu)], last=(k == 7))
                for k in range(8):
                    MM(ps[:, bg_, :], upgv[slot][:, k, 128:256], A[:, k, sb * 512:(sb + 1) * 512], k == 0, k == 7,
                       [sk, 'A%d_%d' % (k, sb)], [PS(bg_)], last=(k == 7))
                us, uk = upsb[q], 'us%d' % q
                if sb == 0:
                    CP(us[:, 0:2], carry[:, j, :], ['carry'], [uk], eng='act')
                else:
                    CP(us[:, 0:2], upsb[qp][:, 512:514], ['us%d' % qp], [uk], eng='act')
                CP(us[:, 2:514], ps[:, bu, :], [PS(bu)], [uk], eng='act')
                if sb == 1:
                    CP(carry[:, j, :], us[:, 512:514], [uk], ['carry'], eng='act')
                cw = lambda tap: pp[:, PP_MCW + j * 3 + tap:PP_MCW + j * 3 + tap + 1]
                cv, ck = fcv[q], 'cv%d' % q
                TS(cv[:], us[:, 2:514], cw(2), pp[:, PP_MCB + j:PP_MCB + j + 1], ALU.mult, ALU.add, [uk, 'pp'], [ck], eng=FFN_TS_ENG)
                STT(cv[:], us[:, 1:513], cw(1), cv[:], ALU.mult, ALU.add, [uk, 'pp', ck], [ck])
                STT(cv[:], us[:, 0:512], cw(0), cv[:], ALU.mult, ALU.add, [uk, 'pp', ck], [ck])
                if pend:
                    pend.pop()()
                ACT(cv[:], cv[:], AF.Gelu_apprx_tanh, [ck], [ck])
                pend.append(lambda j=j, sb=sb, cv=cv, ck=ck, bg_=bg_: TT(interm[:, j, sb * 512:(sb + 1) * 512], cv[:], ps[:, bg_, :],
                                                                      ALU.mult, [ck, PS(bg_)], ['im%d_%d' % (j, sb)]))
        while pend:
            pend.pop()()
        cnt = 0
        for i in range(8):
            slot = i % 2
            sk = 'wdn%d' % slot
            if i >= 1 and i + 1 < 8:
                ldd(i + 1)
            for sb in range(2):
                b = 6 + cnt % 2
                cnt += 1
                t0 = hf * HALF + sb * 512
                for j in range(NJ):
                    MM(ps[:, b, :], wdn[slot][:, j, :], interm[:, j, sb * 512:(sb + 1) * 512], j == 0, j == NJ - 1,
                       [sk, 'im%d_%d' % (j, sb)], [PS(b)], last=(j == NJ - 1))
                xk = 'x%d_%d' % (i, t0 // 512)
                TT(xT[:, i, t0:t0 + 512], xT[:, i, t0:t0 + 512], ps[:, b, :], ALU.add, [xk, PS(b)], [xk])

    def final_out():
        for blk in range(4):
            t0 = blk * 512
            sqv = sqbuf[:]
            for k in range(8):
                ACT(sqv[:, k, :], xT[:, k, t0:t0 + 512], AF.Square, ['x%d_%d' % (k, blk)], ['z%d' % (k // 2)])
            for k in range(8):
                MM(ps[:, 6, :], onesb[:], sqv[:, k, :], k == 0, k == 7, ['onesb', 'z%d' % (k // 2)], [PS(6)], last=(k == 7))
            rsqrt_from_sum(rstd_n, ps[:, 6, :], 1024, [PS(6), 'epst'], ['z4'])
            for k in range(8):
                STT(xo[:, k, :], xT[:, k, t0:t0 + 512], gains[:, 16 + k:17 + k], rstd_n, ALU.mult, ALU.mult,
                    ['x%d_%d' % (k, blk), 'gains', 'z4'], ['xo%d' % k])
            for t4 in range(4):
                tt = blk * 4 + t4
                s, sk = xs[tt % 2], 'xs%d' % (tt % 2)
                for kq_ in range(2):
                    b = (tt * 2 + kq_) % 4
                    for kk in range(4):
                        k = kq_ * 4 + kk
                        TR(ps[:, b, kk * 128:(kk + 1) * 128], xo[:, k, t4 * 128:(t4 + 1) * 128], ident, ['xo%d' % k, 'cst'], [PS(b)],
                           last=(kk == 3))
                    CP(s[:, kq_ * 512:(kq_ + 1) * 512], ps[:, b, :], [PS(b)], [sk], eng=('act' if kq_ == 0 else 'dve'))
                P.dma('sp', 'outd%d' % (tt % 2), out_d[tt * 128:(tt + 1) * 128, :], s[:], reads=[sk])

    import os
    stop_hf = int(os.environ.get('STOPHF', '0'))
    done = False
    for l in range(nlayers):
        layer_setup(l)
        hooks = [lambda q_=q_: load_x(range(4 * q_, 4 * q_ + 4)) for q_ in range(4)] if l == 0 else []
        s5_setup(l, hooks)
        P.barrier()
        MSET(rxhist[:], 0.0, ['rxhist'])
        for hf in range(2):
            rmsnorm_to_A(l, hf, 0)
            if stop == 'norm1' and hf == stop_hf:
                done = True
                break
            inproj(l, hf)
            if stop == 'inproj' and hf == stop_hf:
                done = True
                break
            rg_post(l, hf)
            if stop == 'rg' and hf == stop_hf:
                done = True
                break
            s5_mixer(l, hf)
            if stop == 's5' and hf == stop_hf:
                done = True
                break
            gla_mixer(l, hf)
            if stop == 'gla' and hf == stop_hf:
                done = True
                break
            outproj(l, hf)
            if stop == 'outproj' and hf == stop_hf:
                done = True
                break
            ffn_prefetch(l, hf)
            rmsnorm_to_A(l, hf, 8)
            P.barrier()
            ffn(l, hf)
            P.barrier()
            if stop == 'ffn' and hf == stop_hf:
                done = True
                break
        if done:
            break
    if not done:
        P.barrier()
        final_out()

    avail = dict(A=(A, [128, 8, HALF], BF16), qT=(qT, [128, 2, HALF], BF16), kT=(kT, [128, 2, HALF], BF16),
                 gT=(gT, [128, 4, HALF], BF16), uT=(uT, [128, 2, HALF], BF16), rx=(rx, [128, 2, 3 + HALF], F32),
                 rgT=(rgT, [128, 2, HALF], BF16), vtok=(vtok, [128, 8, 512], BF16), gkl=(gkl, [128, HALF], BF16),
                 xT=(xT, [128, 8, NT], F32), ZT=(ZT, [128, 10, 512], F32))
    for n in dbg_names:
        t, shp, dt = avail[n]
        d = nc.dram_tensor("dbg_" + n, shp, dt, kind="ExternalOutput").ap()
        P.barrier()
        P.dma('sp', 'dbg', d, t[:])
    P.emit()
    return nc


def host_prep(inputs):
    f = lambda n: np.ascontiguousarray(np.asarray(inputs[n], dtype=np.float32))
    cc = np.zeros((128, NCC), np.float32)
    cc[:, CC_ID:CC_ID + 128] = np.eye(128, dtype=np.float32)
    s = np.arange(128)[:, None]
    c = np.arange(128)[None, :]
    cc[:, CC_MASK:CC_MASK + 128] = ((s // 64 == c // 64) & (s <= c)).astype(np.float32)
    cc[:, CC_SEG:CC_SEG + HALF] = (np.arange(HALF) % 64 != 0).astype(np.float32)[None, :]
    cc[:, CC_CIDX:CC_CIDX + 256] = np.arange(256, dtype=np.float32)[None, :]
    cc[:, CC_IDX9:CC_IDX9 + 9] = np.arange(9, dtype=np.float32)[None, :]
    q = np.arange(128)
    glp = (q // 16) % 2
    cc[:, CC_MASKT + 0] = (glp == 0)
    cc[:, CC_MASKT + 1] = (glp == 1)
    pp = np.zeros((DEPTH, 128, NPP), np.float32)

    def tile_vec(v, nt):
        return v.reshape(DEPTH, nt, 128).transpose(0, 2, 1)

    pp[:, :, PP_AN:PP_AN + 8] = tile_vec(f('attn_norm'), 8)
    pp[:, :, PP_MN:PP_MN + 8] = tile_vec(f('mlp_norm'), 8)
    pp[:, :, PP_FN:PP_FN + 8] = np.broadcast_to(f('final_norm').reshape(8, 128).T[None], (DEPTH, 128, 8))
    pp[:, :, PP_BGK:PP_BGK + 2] = tile_vec(f('gla_b_gk'), 2)
    pp[:, :, PP_GN] = f('gla_norm')
    pp[:, :, PP_S5D:PP_S5D + 2] = tile_vec(f('s5_d'), 2)
    pp[:, :, PP_BGLU:PP_BGLU + 2] = tile_vec(f('s5_b_glu'), 2)
    pp[:, :, PP_S5N:PP_S5N + 2] = tile_vec(f('s5_norm'), 2)
    rcw = f('rg_conv_w').reshape(DEPTH, 4, 2, 128).transpose(0, 3, 2, 1)
    pp[:, :, PP_RCW:PP_RCW + 8] = rcw.reshape(DEPTH, 128, 8)
    pp[:, :, PP_RCB:PP_RCB + 2] = tile_vec(f('rg_conv_b'), 2)
    pp[:, :, PP_RBA:PP_RBA + 2] = tile_vec(f('rg_b_a'), 2)
    pp[:, :, PP_RBX:PP_RBX + 2] = tile_vec(f('rg_b_x'), 2)
    pp[:, :, PP_RLAM:PP_RLAM + 2] = tile_vec(f('rg_lambda'), 2)
    pp[:, :, PP_RN:PP_RN + 2] = tile_vec(f('rg_norm'), 2)
    mcw = f('mlp_conv_w').reshape(DEPTH, 3, NJ, 128).transpose(0, 3, 2, 1)
    pp[:, :, PP_MCW:PP_MCW + 66] = mcw.reshape(DEPTH, 128, 66)
    pp[:, :, PP_MCB:PP_MCB + NJ] = tile_vec(f('mlp_conv_b'), NJ)
    def SL(a):
        sh = a.shape
        a = a.reshape((DEPTH, 8, 2, 64) + sh[3:])
        a = np.moveaxis(a, 1, 3)
        return a.reshape((DEPTH, 128, 8) + sh[3:])
    lre, lim = f('s5_lambda_re'), f('s5_lambda_im')
    ldt = np.broadcast_to(f('s5_log_dt')[:, :, None], (DEPTH, 16, 64))
    pp[:, :, PP_LRE:PP_LRE + 8] = SL(lre)
    pp[:, :, PP_LIM:PP_LIM + 8] = SL(lim)
    pp[:, :, PP_LDT:PP_LDT + 8] = SL(np.ascontiguousarray(ldt))
    pp[:, :, PP_BRE:PP_BRE + 128] = SL(f('s5_b_re')).reshape(DEPTH, 128, 128)
    pp[:, :, PP_BIM:PP_BIM + 128] = SL(f('s5_b_im')).reshape(DEPTH, 128, 128)
    pp[:, :, PP_CRE:PP_CRE + 128] = SL(f('s5_c_re').transpose(0, 1, 3, 2)).reshape(DEPTH, 128, 128)
    pp[:, :, PP_CIM:PP_CIM + 128] = SL(f('s5_c_im').transpose(0, 1, 3, 2)).reshape(DEPTH, 128, 128)
    def TL(a, per_i):
        if not per_i:
            a = np.broadcast_to(a[..., None], a.shape + (16,))
        a = a.reshape(DEPTH, 2, 4, 2, 64, 16)
        a = a.transpose(0, 2, 3, 5, 1, 4)
        return a.reshape(DEPTH, 128, 128)
    pp[:, :, PP_LRET:PP_LRET + 128] = TL(lre, False)
    pp[:, :, PP_LIMT:PP_LIMT + 128] = TL(lim, False)
    pp[:, :, PP_LDTT:PP_LDTT + 128] = TL(np.ascontiguousarray(ldt), False)
    pp[:, :, PP_BRET:PP_BRET + 128] = TL(f('s5_b_re'), True)
    pp[:, :, PP_BIMT:PP_BIMT + 128] = TL(f('s5_b_im'), True)
    shared = dict(cc=cc, pp=pp, w_in=f('w_in'), w_out=f('w_out'), w_up=f('w_up'), w_down=f('w_down'),
                  wgk=f('gla_w_gk_up'), rwa=f('rg_w_a'), rwx=f('rg_w_x'), wglu=f('s5_w_glu'))
    return shared


_CACHE = {}


def kernel(**inputs):
    x = np.ascontiguousarray(np.asarray(inputs['x'], dtype=np.float32))
    shared = host_prep(inputs)
    if 'nc' not in _CACHE:
        _CACHE['nc'] = build_program()
    nc = _CACHE['nc']
    in_maps = [dict(shared, x=x[b]) for b in range(8)]
    res = run_bass_kernel_spmd(nc, in_maps, core_ids=list(range(8)))
    return np.stack([res.results[b]['out'] for b in range(8)], axis=0).astype(np.float32)
```
